# Optimizing a Trainium2 kernel written in Bass

```python
import math
import jax
import jax.numpy as jnp
from jax import lax
import numpy as np

D_MODEL = 2048
BATCH = 8
SEQ = 2048
DEPTH = 2

EPS = 1e-6
Q_BLOCK = 128
REL_BUCKETS = 32
REL_MAX_DIST = 128
REL_HEADS = 8

A_HEADS = 8
A_DH = 64
A_DV = 2 * A_DH

B_HEADS = 8
B_DK = 64
B_DV = 128
RET_CHUNK = 128
ROPE_BASE = 10000.0

C_DINNER = D_MODEL
C_HEADDIM = 64
C_HEADS = C_DINNER // C_HEADDIM
C_GROUPS = 4
C_DSTATE = 128
C_CONV = 4
C_CONV_CH = C_DINNER + 2 * C_GROUPS * C_DSTATE
SSD_CHUNK = 128

D_HEADS = 8
D_DH = 128
D_LATENT = 256
IDX_HEADS = 16
IDX_DIM = 64
IDX_TOPK_MAX = 256

PEER_HEADS = 8
PEER_NKEYS = 128
PEER_NEXPERTS = PEER_NKEYS * PEER_NKEYS
PEER_DQ = 256
PEER_TOPK = 16
PEER_CHUNK = 128

AB_WIDTHS = (A_HEADS * 2 * A_DH, A_HEADS * 2 * A_DH, A_HEADS * A_DV,
             B_HEADS * B_DK, B_HEADS * B_DK, B_HEADS * B_DV, B_HEADS * B_DV)
AB_IN = sum(AB_WIDTHS)
AB_MIX = A_HEADS * A_DV + B_HEADS * B_DV
CD_WIDTHS = (C_DINNER, C_CONV_CH, C_HEADS,
             D_HEADS * D_DH, D_LATENT, IDX_HEADS * IDX_DIM, IDX_DIM, IDX_HEADS)
CD_IN = sum(CD_WIDTHS)
CD_MIX = C_DINNER + D_HEADS * D_DH

kernel_name = 'hybrid_diffret_ssd_dsa_peer'


def rms_f32(x, g):
    xf = x.astype(jnp.float32)
    return xf * lax.rsqrt(jnp.mean(xf * xf, axis=-1, keepdims=True) + EPS) * g.astype(jnp.float32)


def rmsnorm(x, g):
    return rms_f32(x, g).astype(x.dtype)


def split_cols(a, widths):
    offs = np.cumsum(widths)[:-1].tolist()
    return jnp.split(a, offs, axis=-1)


def to_blocks(a, size):
    return a.reshape(a.shape[0], a.shape[1] // size, size, *a.shape[2:]).swapaxes(0, 1)


def from_blocks(a):
    a = a.swapaxes(0, 1)
    return a.reshape(a.shape[0], a.shape[1] * a.shape[2], *a.shape[3:])


def rel_bucket(dist):
    n = jnp.maximum(dist, 0)
    max_exact = REL_BUCKETS // 2
    nf = jnp.maximum(n, 1).astype(jnp.float32)
    large = max_exact + (jnp.log(nf / max_exact) / math.log(REL_MAX_DIST / max_exact)
                         * (REL_BUCKETS - max_exact)).astype(jnp.int32)
    large = jnp.minimum(large, REL_BUCKETS - 1)
    return jnp.where(n < max_exact, n, large)


def rope(x, pos):
    d = x.shape[-1]
    inv = ROPE_BASE ** (-jnp.arange(0, d, 2, dtype=jnp.float32) / d)
    ang = pos.astype(jnp.float32)[:, None] * inv[None, :]
    cos = jnp.cos(ang)[None, :, None, :]
    sin = jnp.sin(ang)[None, :, None, :]
    xf = x.astype(jnp.float32)
    x1, x2 = xf[..., 0::2], xf[..., 1::2]
    return jnp.stack([x1 * cos - x2 * sin, x1 * sin + x2 * cos], axis=-1).reshape(x.shape).astype(x.dtype)


def diff_attention(q, k, v, lam, rel_table):
    bsz, s_len = q.shape[0], q.shape[1]
    nb = s_len // Q_BLOCK
    kpos = jnp.arange(s_len)
    scale = A_DH ** -0.5

    def block(args):
        qblk, i = args
        qpos = i * Q_BLOCK + jnp.arange(Q_BLOCK)
        dist = qpos[:, None] - kpos[None, :]
        bias = rel_table[rel_bucket(dist)].astype(jnp.float32).transpose(2, 0, 1)
        bias = jnp.where((dist >= 0)[None], bias, -jnp.inf)
        logits = jnp.einsum('bqhmd,bshmd->bmhqs', qblk, k).astype(jnp.float32) * scale + bias[None, None]
        p = jax.nn.softmax(logits, axis=-1)
        w = p[:, 0] - lam * p[:, 1]
        return jnp.einsum('bhqs,bshd->bqhd', w.astype(v.dtype), v)

    out = lax.map(block, (to_blocks(q, Q_BLOCK), jnp.arange(nb)))
    return from_blocks(out)


def retention(q, k, v):
    bsz, s_len, n_h, dk = q.shape
    dv = v.shape[-1]
    c = RET_CHUNK
    log_gamma = jnp.log(1.0 - 2.0 ** (-5.0 - jnp.arange(n_h, dtype=jnp.float32)))
    idx = jnp.arange(c, dtype=jnp.float32)
    rel = idx[:, None] - idx[None, :]
    inner_decay = jnp.where(rel[None] >= 0, jnp.exp(rel[None] * log_gamma[:, None, None]), 0.0)
    q_decay = jnp.exp((idx[:, None] + 1.0) * log_gamma[None, :])
    k_decay = jnp.exp((c - 1.0 - idx[:, None]) * log_gamma[None, :])
    chunk_decay = jnp.exp(c * log_gamma)
    qc = to_blocks(q.astype(jnp.float32) * dk ** -0.5, c)
    kc = to_blocks(k.astype(jnp.float32), c)
    vc = to_blocks(v.astype(jnp.float32), c)

    def step(state, inp):
        qi, ki, vi = inp
        scores = jnp.einsum('bqhd,bshd->bhqs', qi, ki) * inner_decay[None]
        o = jnp.einsum('bhqs,bshv->bqhv', scores, vi)
        o = o + jnp.einsum('bqhd,bhdv->bqhv', qi * q_decay[None, :, :, None], state)
        state = state * chunk_decay[None, :, None, None] + jnp.einsum(
            'bshd,bshv->bhdv', ki * k_decay[None, :, :, None], vi)
        return state, o

    _, o = lax.scan(step, jnp.zeros((bsz, n_h, dk, dv), jnp.float32), (qc, kc, vc))
    return from_blocks(o)


def causal_dwconv(x, w, b):
    k_w = w.shape[0]
    out = lax.conv_general_dilated(x, w[:, None, :], window_strides=(1,), padding=[(k_w - 1, 0)],
                                   dimension_numbers=('NWC', 'WIO', 'NWC'),
                                   feature_group_count=x.shape[-1])
    return out + b


def ssd_scan(x, dt, a, bm, cm):
    bsz, s_len, n_h, p = x.shape
    g, n = bm.shape[2], bm.shape[3]
    hg = n_h // g
    qn = SSD_CHUNK
    f32 = jnp.float32
    xs = to_blocks(x.astype(f32).reshape(bsz, s_len, g, hg, p), qn)
    dts = to_blocks(dt.reshape(bsz, s_len, g, hg), qn)
    bs = to_blocks(bm.astype(f32), qn)
    cs_in = to_blocks(cm.astype(f32), qn)
    ag = a.reshape(g, hg)
    causal = jnp.tril(jnp.ones((qn, qn), bool))[None, :, :, None, None]

    def step(state, inp):
        xc, dtc, bc, cc = inp
        cs = jnp.cumsum(dtc * ag, axis=1)
        seg = cs[:, :, None] - cs[:, None, :]
        lmat = jnp.exp(jnp.where(causal, seg, -jnp.inf))
        cb = jnp.einsum('blgn,bsgn->blsg', cc, bc)
        xdt = xc * dtc[..., None]
        y = jnp.einsum('blsg,blsgh,bsghp->blghp', cb, lmat, xdt)
        y = y + jnp.einsum('blgn,bghpn->blghp', cc, state) * jnp.exp(cs)[..., None]
        last = cs[:, -1]
        to_end = jnp.exp(last[:, None] - cs)
        state = state * jnp.exp(last)[..., None, None] + jnp.einsum(
            'bsgn,bsghp->bghpn', bc, xdt * to_end[..., None])
        return state, y

    _, y = lax.scan(step, jnp.zeros((bsz, g, hg, p, n), f32), (xs, dts, bs, cs_in))
    return from_blocks(y).reshape(bsz, s_len, n_h, p)


def mamba2_mixer(z, xbc, dt_raw, conv_w, conv_b, dt_bias, a_log, d_skip, norm_g):
    bsz, s_len = z.shape[0], z.shape[1]
    xbc = jax.nn.silu(causal_dwconv(xbc, conv_w, conv_b))
    xs, bm, cm = split_cols(xbc, (C_DINNER, C_GROUPS * C_DSTATE, C_GROUPS * C_DSTATE))
    dt = jax.nn.softplus(dt_raw.astype(jnp.float32) + dt_bias.astype(jnp.float32))
    a = -jnp.exp(a_log.astype(jnp.float32))
    xh = xs.reshape(bsz, s_len, C_HEADS, C_HEADDIM)
    y = ssd_scan(xh, dt, a,
                 bm.reshape(bsz, s_len, C_GROUPS, C_DSTATE),
                 cm.reshape(bsz, s_len, C_GROUPS, C_DSTATE))
    y = y + xh.astype(jnp.float32) * d_skip.astype(jnp.float32)[:, None]
    y = y.reshape(bsz, s_len, C_DINNER) * jax.nn.silu(z.astype(jnp.float32))
    yg = y.reshape(bsz, s_len, C_GROUPS, C_DINNER // C_GROUPS)
    yg = yg * lax.rsqrt(jnp.mean(yg * yg, axis=-1, keepdims=True) + EPS)
    return (yg.reshape(bsz, s_len, C_DINNER) * norm_g.astype(jnp.float32)).astype(z.dtype)


def dsa_mixer(q, ckv, q_idx, k_idx, w_idx, kv_norm, w_uk, w_uv, rel_table):
    bsz, s_len = q.shape[0], q.shape[1]
    topk = min(IDX_TOPK_MAX, s_len // 4)
    nb = s_len // Q_BLOCK
    ckv = rmsnorm(ckv, kv_norm)
    q_abs = jnp.einsum('bshd,hdc->bshc', q, w_uk)
    kpos = jnp.arange(s_len)
    bidx = jnp.arange(bsz)[:, None, None]
    idx_scale = (IDX_HEADS * IDX_DIM) ** -0.5

    def block(args):
        qa, qi, wi, i = args
        qpos = i * Q_BLOCK + jnp.arange(Q_BLOCK)
        causal = qpos[:, None] >= kpos[None, :]
        rel = jax.nn.relu(jnp.einsum('bqhd,bsd->bqhs', qi, k_idx).astype(jnp.float32))
        score = jnp.einsum('bqh,bqhs->bqs', wi.astype(jnp.float32) * idx_scale, rel)
        score = jnp.where(causal[None], score, -jnp.inf)
        _, sel = lax.top_k(score, topk)
        kv_sel = ckv[bidx, sel]
        dist = qpos[None, :, None] - sel
        valid = dist >= 0
        bias = rel_table[rel_bucket(dist)].astype(jnp.float32)
        logits = jnp.einsum('bqhc,bqkc->bqhk', qa, kv_sel).astype(jnp.float32) * D_DH ** -0.5
        logits = logits + bias.transpose(0, 1, 3, 2)
        logits = jnp.where(valid[:, :, None, :], logits, -jnp.inf)
        p = jax.nn.softmax(logits, axis=-1)
        ctx = jnp.einsum('bqhk,bqkc->bqhc', p.astype(kv_sel.dtype), kv_sel)
        return jnp.einsum('bqhc,hcd->bqhd', ctx, w_uv)

    out = lax.map(block, (to_blocks(q_abs, Q_BLOCK), to_blocks(q_idx, Q_BLOCK),
                          to_blocks(w_idx, Q_BLOCK), jnp.arange(nb)))
    return from_blocks(out)


def ab_mixer(h, w_in, w_out, lam_p, a_norm, b_norm, rel_table, layer):
    bsz, s_len, _ = h.shape
    qa, ka, va, qb, kb, vb, gb = split_cols(h @ w_in, AB_WIDTHS)
    lam_init = 0.8 - 0.6 * math.exp(-0.3 * layer)
    lp = lam_p.astype(jnp.float32)
    lam = jnp.exp(jnp.sum(lp[0] * lp[1])) - jnp.exp(jnp.sum(lp[2] * lp[3])) + lam_init
    oa = diff_attention(qa.reshape(bsz, s_len, A_HEADS, 2, A_DH),
                        ka.reshape(bsz, s_len, A_HEADS, 2, A_DH),
                        va.reshape(bsz, s_len, A_HEADS, A_DV), lam, rel_table)
    oa = rms_f32(oa, a_norm) * (1.0 - lam_init)
    pos = jnp.arange(s_len)
    ob = retention(rope(qb.reshape(bsz, s_len, B_HEADS, B_DK), pos),
                   rope(kb.reshape(bsz, s_len, B_HEADS, B_DK), pos),
                   vb.reshape(bsz, s_len, B_HEADS, B_DV))
    ob = rms_f32(ob, b_norm) * jax.nn.silu(gb.astype(jnp.float32)).reshape(bsz, s_len, B_HEADS, B_DV)
    mixed = jnp.concatenate([oa.reshape(bsz, s_len, -1), ob.reshape(bsz, s_len, -1)], axis=-1)
    return mixed.astype(h.dtype) @ w_out


def cd_mixer(h, w_in, w_out, conv_w, conv_b, dt_bias, a_log, d_skip, ssm_norm,
             kv_norm, w_uk, w_uv, rel_table):
    bsz, s_len, _ = h.shape
    z, xbc, dt, q, ckv, q_idx, k_idx, w_idx = split_cols(h @ w_in, CD_WIDTHS)
    yc = mamba2_mixer(z, xbc, dt, conv_w, conv_b, dt_bias, a_log, d_skip, ssm_norm)
    yd = dsa_mixer(q.reshape(bsz, s_len, D_HEADS, D_DH), ckv,
                   q_idx.reshape(bsz, s_len, IDX_HEADS, IDX_DIM), k_idx, w_idx,
                   kv_norm, w_uk, w_uv, rel_table)
    mixed = jnp.concatenate([yc, yd.reshape(bsz, s_len, -1).astype(yc.dtype)], axis=-1)
    return mixed.astype(h.dtype) @ w_out


def peer(h, w_q, sub_keys, u, v):
    bsz, s_len, d = h.shape
    t = bsz * s_len
    ht = h.reshape(t, d)
    q = (ht @ w_q).reshape(t, PEER_HEADS, 2, PEER_DQ // 2)
    s = jnp.einsum('thcd,hckd->thck', q, sub_keys).astype(jnp.float32)
    top_s, top_i = lax.top_k(s, PEER_TOPK)
    cand = (top_s[:, :, 0, :, None] + top_s[:, :, 1, None, :]).reshape(t, PEER_HEADS, -1)
    cand_id = (top_i[:, :, 0, :, None] * PEER_NKEYS + top_i[:, :, 1, None, :]).reshape(t, PEER_HEADS, -1)
    best, pos = lax.top_k(cand, PEER_TOPK)
    ids = jnp.take_along_axis(cand_id, pos, axis=-1)
    gate = jax.nn.softmax(best, axis=-1)
    nch = t // PEER_CHUNK

    def chunk(args):
        xc, idc, gc = args
        act = jax.nn.gelu(jnp.einsum('td,thkd->thk', xc, u[idc]).astype(jnp.float32))
        return jnp.einsum('thk,thkd->td', (gc * act).astype(v.dtype), v[idc])

    out = lax.map(chunk, (ht.reshape(nch, PEER_CHUNK, d),
                          ids.reshape(nch, PEER_CHUNK, PEER_HEADS, PEER_TOPK),
                          gate.reshape(nch, PEER_CHUNK, PEER_HEADS, PEER_TOPK)))
    return out.reshape(bsz, s_len, d).astype(h.dtype)


def setup_inputs(seed: int = 0) -> dict:
    key = jax.random.key(seed)
    keys = list(jax.random.split(key, 32))
    f32 = jnp.float32
    ne = (DEPTH + 1) // 2
    no = DEPTH // 2

    def nrm(shape, scale):
        return jax.random.normal(keys.pop(), shape, f32) * scale

    def gain(shape):
        return 1.0 + nrm(shape, 0.02)

    dt = jnp.exp(jax.random.uniform(keys.pop(), (no, C_HEADS), f32, math.log(1e-3), math.log(1e-1)))
    a_init = jax.random.uniform(keys.pop(), (no, C_HEADS), f32, 1.0, 16.0)
    return {
        'x': nrm((BATCH, SEQ, D_MODEL), 1.0),
        'rel_table': nrm((REL_BUCKETS, REL_HEADS), 0.2),
        'ab_w_in': nrm((ne, D_MODEL, AB_IN), D_MODEL ** -0.5),
        'ab_w_out': nrm((ne, AB_MIX, D_MODEL), AB_MIX ** -0.5),
        'ab_lambda': nrm((ne, 4, A_DH), 0.1),
        'ab_a_norm': gain((ne, A_DV)),
        'ab_b_norm': gain((ne, B_DV)),
        'cd_w_in': nrm((no, D_MODEL, CD_IN), D_MODEL ** -0.5),
        'cd_w_out': nrm((no, CD_MIX, D_MODEL), CD_MIX ** -0.5),
        'cd_conv_w': nrm((no, C_CONV, C_CONV_CH), C_CONV ** -0.5),
        'cd_conv_b': nrm((no, C_CONV_CH), 0.02),
        'cd_dt_bias': dt + jnp.log(-jnp.expm1(-dt)),
        'cd_a_log': jnp.log(a_init),
        'cd_d_skip': gain((no, C_HEADS)),
        'cd_ssm_norm': gain((no, C_DINNER)),
        'cd_kv_norm': gain((no, D_LATENT)),
        'cd_w_uk': nrm((no, D_HEADS, D_DH, D_LATENT), D_DH ** -0.5),
        'cd_w_uv': nrm((no, D_HEADS, D_LATENT, D_DH), D_LATENT ** -0.5),
        'norm_mix': gain((DEPTH, D_MODEL)),
        'norm_ffn': gain((DEPTH, D_MODEL)),
        'peer_w_q': nrm((DEPTH, D_MODEL, PEER_HEADS * PEER_DQ), D_MODEL ** -0.5),
        'peer_keys': nrm((DEPTH, PEER_HEADS, 2, PEER_NKEYS, PEER_DQ // 2), (PEER_DQ // 2) ** -0.5),
        'peer_u': nrm((DEPTH, PEER_NEXPERTS, D_MODEL), D_MODEL ** -0.5),
        'peer_v': nrm((DEPTH, PEER_NEXPERTS, D_MODEL), (PEER_HEADS * PEER_TOPK) ** -0.5),
        'norm_final': gain((D_MODEL,)),
    }


def reference(x, rel_table, ab_w_in, ab_w_out, ab_lambda, ab_a_norm, ab_b_norm,
              cd_w_in, cd_w_out, cd_conv_w, cd_conv_b, cd_dt_bias, cd_a_log, cd_d_skip,
              cd_ssm_norm, cd_kv_norm, cd_w_uk, cd_w_uv, norm_mix, norm_ffn,
              peer_w_q, peer_keys, peer_u, peer_v, norm_final):
    h = x
    for layer in range(DEPTH):
        hn = rmsnorm(h, norm_mix[layer])
        i = layer // 2
        if layer % 2 == 0:
            mix = ab_mixer(hn, ab_w_in[i], ab_w_out[i], ab_lambda[i], ab_a_norm[i], ab_b_norm[i],
                           rel_table, layer)
        else:
            mix = cd_mixer(hn, cd_w_in[i], cd_w_out[i], cd_conv_w[i], cd_conv_b[i], cd_dt_bias[i],
                           cd_a_log[i], cd_d_skip[i], cd_ssm_norm[i], cd_kv_norm[i],
                           cd_w_uk[i], cd_w_uv[i], rel_table)
        h = h + mix.astype(h.dtype)
        h = h + peer(rmsnorm(h, norm_ffn[layer]), peer_w_q[layer], peer_keys[layer],
                     peer_u[layer], peer_v[layer])
    return rmsnorm(h, norm_final)
```

```python
from contextlib import ExitStack
import os
import numpy as np
import concourse.bass as bass
import concourse.mybir as mybir

F32 = mybir.dt.float32
BF16 = mybir.dt.bfloat16
I32 = mybir.dt.int32
AF = mybir.ActivationFunctionType
ALU = mybir.AluOpType
AX = mybir.AxisListType

NDMA = 24
COMPUTE = ("pe", "act", "dve", "pool")


class Tok:
    __slots__ = ("key", "val", "clock")

    def __init__(self, key, val, clock):
        self.key, self.val, self.clock = key, val, clock


class Prog:
    def __init__(self):
        self.nc = bass.Bass("TRN2", target_bir_lowering=False)
        nc = self.nc
        self.es = ExitStack()
        self.engs = {"pe": nc.tensor, "act": nc.scalar, "dve": nc.vector,
                     "pool": nc.gpsimd, "sp": nc.sync}
        self.sem = {e: self.es.enter_context(nc.semaphore("s_" + e)) for e in COMPUTE}
        self.cnt = {e: 0 for e in COMPUTE}
        self.dsem = [self.es.enter_context(nc.semaphore("d%d" % i)) for i in range(NDMA)]
        self.dval = [0] * NDMA
        self.dtok = [None] * NDMA
        self.dn = 0
        self.known = {e: {} for e in self.engs}
        self.snap = {e: None for e in self.engs}
        self.last_w = {}
        self.readers = {}
        self.nwaits = 0
        self.ninst = 0
        self.psum_names = set()
        self.dram_in = {}

    def sb(self, name, shape, dt, stack=None):
        self.uid = getattr(self, "uid", 0) + 1
        return (stack or self.es).enter_context(self.nc.sbuf_tensor("%s_u%d" % (name, self.uid), list(shape), dt))

    def ps(self, name, shape, dt=F32, stack=None):
        self.uid = getattr(self, "uid", 0) + 1
        self.psum_names.add(name)
        return (stack or self.es).enter_context(self.nc.psum_tensor("%s_u%d" % (name, self.uid), list(shape), dt))

    def dram(self, name, shape, dt, kind="Internal"):
        return self.nc.dram_tensor(name, list(shape), dt, kind=kind)

    def _snapshot(self, eng):
        s = self.snap[eng]
        if s is None:
            s = dict(self.known[eng])
            self.snap[eng] = s
        return s

    def _semof(self, key):
        return self.sem[key] if isinstance(key, str) else self.dsem[key[1]]

    def _wait(self, eng, toks):
        kn = self.known[eng]
        best = {}
        for t in toks:
            if t is None:
                continue
            if eng == "pe" and t.key == "pe":
                continue
            if kn.get(t.key, 0) >= t.val:
                continue
            if best.get(t.key, (0, None))[0] < t.val:
                best[t.key] = (t.val, t)
        for key, (val, t) in best.items():
            if kn.get(key, 0) >= val:
                continue
            self.engs[eng].wait_ge(self._semof(key), val)
            self.nwaits += 1
            for k2, v2 in t.clock.items():
                if kn.get(k2, 0) < v2:
                    kn[k2] = v2
            kn[key] = val
            self.snap[eng] = None

    def _is_psum(self, b):
        n = b if isinstance(b, str) else b[0]
        return isinstance(n, str) and (n in self.psum_names or n.startswith("ss_bk"))

    def _deps(self, r, w, eng=None):
        need = []
        for b in r:
            t = self.last_w.get(b)
            if t is not None:
                need.append(t)
            if self._is_psum(b):
                need.extend(t2 for t2 in self.readers.get(b, ()) if t2.key != eng)
        for b in w:
            t = self.last_w.get(b)
            if t is not None:
                need.append(t)
            need.extend(self.readers.get(b, ()))
        return need

    def _record(self, tok, r, w):
        for b in w:
            self.last_w[b] = tok
            self.readers[b] = []
        for b in r:
            if b in w:
                continue
            self.readers.setdefault(b, []).append(tok)

    def op(self, eng, fn, r=(), w=()):
        self._wait(eng, self._deps(r, w, eng))
        ins = fn(self.engs[eng])
        self.cnt[eng] += 1
        ins.then_inc(self.sem[eng], 1)
        self.ninst += 1
        tok = Tok(eng, self.cnt[eng], self._snapshot(eng))
        self._record(tok, r, w)
        return tok

    def dma(self, out, in_, r=(), w=(), q="sp", **kw):
        i = self.dn % NDMA
        self.dn += 1
        need = self._deps(r, w)
        need.append(self.dtok[i])
        self._wait(q, need)
        self.dval[i] += 16
        self.engs[q].dma_start(out=out, in_=in_, **kw).then_inc(self.dsem[i], 16)
        self.ninst += 1
        tok = Tok(("d", i), self.dval[i], self._snapshot(q))
        self.dtok[i] = tok
        self._record(tok, r, w)
        return tok

    def barrier(self):
        toks = []
        for e in COMPUTE:
            if self.cnt[e]:
                toks.append(Tok(e, self.cnt[e], {}))
        toks += [t for t in self.dtok if t is not None]
        for e in self.engs:
            self._wait(e, toks)
        self.last_w.clear()
        self.readers.clear()

    def finish(self):
        toks = [t for t in self.dtok if t is not None]
        for e in COMPUTE:
            if self.cnt[e]:
                toks.append(Tok(e, self.cnt[e], {}))
        self._wait("sp", toks)


S_LEN = 2048
DM = 2048
NT = 16
EPS = 1e-6
LAM_INIT0 = 0.8 - 0.6 * float(np.exp(-0.3 * 0))


class MK:
    def __init__(self, dbg=()):
        self.P = Prog()
        self.dbg = set(dbg)
        self.inputs = {}
        self.rr = 0
        P = self.P
        self.ident = P.sb("ident", [128, 128], BF16)
        self.eps_t = P.sb("eps_t", [128, 1], F32)

    def inp(self, name, shape, dt=F32):
        t = self.P.dram(name, shape, dt, kind="ExternalInput")
        self.inputs[name] = t
        return t

    def scratch(self, name, shape, dt):
        kind = "ExternalOutput" if name in self.dbg else "Internal"
        return self.P.dram(name, shape, dt, kind=kind)

    def alt(self, *engs):
        self.rr += 1
        return engs[self.rr % len(engs)]

    def copy(self, eng, out, in_, r, w):
        if eng == "act":
            return self.P.op("act", lambda e: e.activation(out=out, in_=in_, func=AF.Copy), r=r, w=w)
        return self.P.op(eng, lambda e: e.tensor_copy(out=out, in_=in_), r=r, w=w)

    def setup_consts(self, c_ident):
        P = self.P
        with ExitStack() as st:
            idf = P.sb("idf", [128, 128], F32, st)
            P.dma(idf[:], c_ident.ap(), w=["idf"])
            self.copy("dve", self.ident[:], idf[:], ["idf"], ["ident"])
            P.op("pool", lambda e: e.memset(self.eps_t[:], EPS), w=["eps_t"])
            P.barrier()

    def rstd_from_ss(self, rstd, ss, n, rid, sid):
        P = self.P
        P.op("act", lambda e: e.activation(out=rstd, in_=ss, func=AF.Sqrt, bias=self.eps_t[:, 0:1], scale=1.0 / n),
             r=[sid, "eps_t"], w=[rid])
        P.op("dve", lambda e: e.reciprocal(out=rstd, in_=rstd), r=[rid], w=[rid])

    def loadT(self, src, K, dstT, dst_id, gain=None, src_dt=F32):
        P = self.P
        KC = K // 128
        with ExitStack() as st:
            xts = [P.sb("lt_x%d" % i, [128, K], src_dt, st) for i in range(2)]
            need_cast = (gain is not None) or (src_dt != BF16)
            xns = [P.sb("lt_n%d" % i, [128, K], BF16, st) for i in range(2)] if need_cast else None
            pts = [P.ps("lt_p%d" % i, [128, 512], BF16, st) for i in range(2)]
            if gain is not None:
                gb = P.sb("lt_g", [128, K], F32, st)
                junk = P.sb("lt_j", [128, K], BF16, st)
                ss = P.sb("lt_ss", [128, 2], F32, st)
                rs = P.sb("lt_rs", [128, 2], F32, st)
                P.dma(gb[:], gain.partition_broadcast(128), w=["lt_g"])
            ip = 0
            for t in range(NT):
                b = t % 2
                xt = xts[b]
                P.dma(xt[:], src[t * 128:(t + 1) * 128, :], w=["lt_x%d" % b])
                if gain is not None:
                    P.op("act", lambda e: e.activation(out=junk[:], in_=xt[:], func=AF.Square, accum_out=ss[:, b:b + 1]),
                         r=["lt_x%d" % b], w=["lt_j", "lt_ss%d" % b])
                    self.rstd_from_ss(rs[:, b:b + 1], ss[:, b:b + 1], K, "lt_rs%d" % b, "lt_ss%d" % b)
                    P.op("dve", lambda e: e.scalar_tensor_tensor(out=xns[b][:], in0=xt[:], scalar=rs[:, b:b + 1], in1=gb[:],
                                                                 op0=ALU.mult, op1=ALU.mult),
                         r=["lt_x%d" % b, "lt_rs%d" % b, "lt_g"], w=["lt_n%d" % b])
                    xn, xid = xns[b], "lt_n%d" % b
                elif need_cast:
                    self.copy("pool", xns[b][:], xt[:], ["lt_x%d" % b], ["lt_n%d" % b])
                    xn, xid = xns[b], "lt_n%d" % b
                else:
                    xn, xid = xt, "lt_x%d" % b
                for k4 in range(KC // 4):
                    pt = pts[ip % 2]
                    pid = "lt_p%d" % (ip % 2)
                    ip += 1
                    for kk in range(4):
                        k = k4 * 4 + kk
                        P.op("pe", lambda e: e.transpose(out=pt[:, kk * 128:(kk + 1) * 128], in_=xn[:, k * 128:(k + 1) * 128],
                                                         identity=self.ident[:]), r=[xid, "ident"], w=[pid])
                    self.copy(self.alt("act", "dve"), dstT[:, k4 * 4:(k4 + 1) * 4, t * 128:(t + 1) * 128],
                              pt[:].rearrange("p (a b) -> p a b", b=128), [pid], [dst_id])
            P.barrier()

    def linear(self, xT, x_id, KC, W, col0, ncols, mode, sink, slab=512, swap=False, nps=2):
        P = self.P
        Wv = W.rearrange("(kc p) n -> p kc n", p=128)
        with ExitStack() as st:
            w32 = [P.sb("ln_w%d" % i, [128, KC, slab], F32, st) for i in range(2)]
            wb = [P.sb("ln_b%d" % i, [128, KC, slab], BF16, st) for i in range(2)]
            ws = [P.sb("ln_s%d" % i, [128, KC, slab], BF16, st) for i in range(2)] if swap else None
            npsum = nps * (2 if swap else 1)
            pss = [P.ps("ln_p%d" % i, [128, 512], F32, st) for i in range(npsum)]
            ip = 0
            for si, s0 in enumerate(range(col0, col0 + ncols, slab)):
                n = min(slab, col0 + ncols - s0)
                b = si % 2
                P.dma(w32[b][:, :, 0:n], Wv[:, :, s0:s0 + n], w=["ln_w%d" % b])
                half = KC // 2
                self.copy("act", wb[b][:, 0:half, 0:n], w32[b][:, 0:half, 0:n], ["ln_w%d" % b], [("ln_b%d" % b, 0)])
                self.copy("pool", wb[b][:, half:KC, 0:n], w32[b][:, half:KC, 0:n], ["ln_w%d" % b], [("ln_b%d" % b, 1)])
                wid = [("ln_b%d" % b, 0), ("ln_b%d" % b, 1)]
                if swap:
                    self.copy("pool", ws[b][:, :, 0:n:2], w32[b][:, :, 1:n:2], ["ln_w%d" % b], [("ln_s%d" % b, 0)])
                    self.copy("dve", ws[b][:, :, 1:n:2], w32[b][:, :, 0:n:2], ["ln_w%d" % b], [("ln_s%d" % b, 1)])
                    sid = [("ln_s%d" % b, 0), ("ln_s%d" % b, 1)]
                if mode == "tok":
                    for t in range(NT):
                        ps = pss[ip % npsum]
                        pid = "ln_p%d" % (ip % npsum)
                        ip += 1
                        for k in range(KC):
                            P.op("pe", lambda e: e.matmul(ps[:, 0:n], lhsT=xT[:, k, t * 128:(t + 1) * 128], rhs=wb[b][:, k, 0:n],
                                                          start=(k == 0), stop=(k == KC - 1)), r=[x_id] + wid, w=[pid])
                        sink(t, s0 - col0, n, ps, pid)
                else:
                    for c in range(n // 128):
                        for tg in range(4):
                            ps = pss[ip % npsum]
                            pid = "ln_p%d" % (ip % npsum)
                            ip += 1
                            for k in range(KC):
                                P.op("pe", lambda e: e.matmul(ps[:, :], lhsT=wb[b][:, k, c * 128:(c + 1) * 128],
                                                              rhs=xT[:, k, tg * 512:(tg + 1) * 512],
                                                              start=(k == 0), stop=(k == KC - 1)), r=[x_id] + wid, w=[pid])
                            if swap:
                                ps2 = pss[ip % npsum]
                                pid2 = "ln_p%d" % (ip % npsum)
                                ip += 1
                                for k in range(KC):
                                    P.op("pe", lambda e: e.matmul(ps2[:, :], lhsT=ws[b][:, k, c * 128:(c + 1) * 128],
                                                                  rhs=xT[:, k, tg * 512:(tg + 1) * 512],
                                                                  start=(k == 0), stop=(k == KC - 1)), r=[x_id] + sid, w=[pid2])
                                sink((s0 - col0) // 128 + c, tg, ps, pid, ps2, pid2)
                            else:
                                sink((s0 - col0) // 128 + c, tg, ps, pid)
            P.barrier()

    def make_store_sink(self, st, dst, mode, dt, func=None, nstage=3, tag="sk"):
        P = self.P
        stages = [P.sb("%s_st%d" % (tag, i), [128, 512], dt, st) for i in range(nstage)]
        cnt = [0]

        def sink(a, b, *rest):
            i = cnt[0] % nstage
            cnt[0] += 1
            sid = "%s_st%d" % (tag, i)
            if mode == "tok":
                n, ps, pid = rest
                if func is not None:
                    P.op("act", lambda e: e.activation(out=stages[i][:, 0:n], in_=ps[:, 0:n], func=func), r=[pid], w=[sid])
                else:
                    self.copy(self.alt("act", "dve"), stages[i][:, 0:n], ps[:, 0:n], [pid], [sid])
                P.dma(dst[a * 128:(a + 1) * 128, b:b + n], stages[i][:, 0:n], r=[sid], w=[tag + "_d"], q="act")
            else:
                ps, pid = rest
                self.copy(self.alt("act", "dve"), stages[i][:, :], ps[:, :], [pid], [sid])
                P.dma(dst[a * 128:(a + 1) * 128, b * 512:(b + 1) * 512], stages[i][:, :], r=[sid], w=[tag + "_d"], q="act")
        return sink


def host_consts():
    c = {}
    c["c_ident"] = np.eye(128, dtype=np.float32)
    s = np.arange(128)[:, None]
    cidx = np.arange(256)[None, :]
    dist = cidx - s
    n = np.maximum(dist, 0)
    nf = np.maximum(n, 1).astype(np.float32)
    large = 16 + (np.log(nf / np.float32(16)) / np.float32(np.log(128 / 16)) * np.float32(16)).astype(np.int32)
    large = np.minimum(large, 31)
    bucket = np.where(n < 16, n, large)
    oh = np.zeros((128, 32, 256), np.float32)
    for b in range(32):
        oh[:, b, :] = (bucket == b)
    c["c_boh"] = oh
    c["c_bmask"] = (dist >= 0).astype(np.float32)
    pos = np.arange(S_LEN, dtype=np.float32)
    inv = (np.float32(10000.0) ** (-np.arange(0, 64, 2, dtype=np.float32) / np.float32(64))).astype(np.float32)
    ang = pos[:, None] * inv[None, :]
    cos = np.cos(ang).astype(np.float64)
    sin = np.sin(ang).astype(np.float64)
    rq = np.zeros((4, 2, 128, S_LEN), np.float32)
    rk = np.zeros((4, 2, 128, S_LEN), np.float32)
    t64 = np.arange(S_LEN, dtype=np.float64)
    for ch in range(4):
        for p in range(128):
            h = 2 * ch + p // 64
            d = p % 64
            i = d // 2
            sign = -1.0 if d % 2 == 0 else 1.0
            lg = np.log(1.0 - 2.0 ** (-5.0 - h))
            dq = np.exp(t64 * lg) * 64 ** -0.5
            dk = np.exp(-t64 * lg)
            rq[ch, 0, p] = cos[:, i] * dq
            rq[ch, 1, p] = sign * sin[:, i] * dq
            rk[ch, 0, p] = cos[:, i] * dk
            rk[ch, 1, p] = sign * sin[:, i] * dk
    c["c_ropeq"] = rq
    c["c_ropek"] = rk
    return c


class MK0(MK):
    def build_bias_tables(self, rel_table, c_boh, c_bmask):
        P = self.P
        self.tbl_bc = P.sb("tbl_bc", [128, 256], F32)
        self.EB = P.sb("EB", [128, 8, 256], F32)
        self.mask01 = P.sb("mask01", [128, 256], F32)
        with ExitStack() as st:
            oh = P.sb("bt_oh", [128, 32, 256], F32, st)
            P.dma(self.tbl_bc[:], rel_table.ap().rearrange("b h -> (b h)").unsqueeze(0).partition_broadcast(128) if False else
                  rel_table.ap().rearrange("(o b) h -> o (b h)", o=1).partition_broadcast(128), w=["tbl_bc"])
            P.dma(oh[:], c_boh.ap(), w=["bt_oh"])
            P.dma(self.mask01[:], c_bmask.ap(), w=["mask01"])
            for h in range(8):
                for b in range(32):
                    if b == 0:
                        P.op("dve", lambda e: e.tensor_scalar(out=self.EB[:, h, :], in0=oh[:, 0, :], scalar1=self.tbl_bc[:, h:h + 1],
                                                              scalar2=None, op0=ALU.mult), r=["bt_oh", "tbl_bc"], w=[("EB", h)])
                    else:
                        P.op("dve", lambda e: e.scalar_tensor_tensor(out=self.EB[:, h, :], in0=oh[:, b, :],
                                                                     scalar=self.tbl_bc[:, b * 8 + h:b * 8 + h + 1],
                                                                     in1=self.EB[:, h, :], op0=ALU.mult, op1=ALU.add),
                             r=["bt_oh", "tbl_bc", ("EB", h)], w=[("EB", h)])
                P.op("act", lambda e: e.activation(out=self.EB[:, h, :], in_=self.EB[:, h, :], func=AF.Exp), r=[("EB", h)], w=[("EB", h)])
                P.op("dve", lambda e: e.tensor_tensor(out=self.EB[:, h, :], in0=self.EB[:, h, :], in1=self.mask01[:], op=ALU.mult),
                     r=[("EB", h), "mask01"], w=[("EB", h)])
            P.barrier()

    def lam_scalar(self, ab_lambda):
        P = self.P
        self.neglam = P.sb("neglam", [128, 1], F32)
        with ExitStack() as st:
            lp = P.sb("lm_lp", [128, 256], F32, st)
            pr = P.sb("lm_pr", [128, 2, 64], F32, st)
            sm = P.sb("lm_sm", [128, 2], F32, st)
            P.dma(lp[:], ab_lambda.ap().rearrange("o a d -> o (a d)").partition_broadcast(128), w=["lm_lp"])
            P.op("dve", lambda e: e.tensor_tensor(out=pr[:, 0, :], in0=lp[:, 0:64], in1=lp[:, 64:128], op=ALU.mult), r=["lm_lp"], w=["lm_pr"])
            P.op("dve", lambda e: e.tensor_tensor(out=pr[:, 1, :], in0=lp[:, 128:192], in1=lp[:, 192:256], op=ALU.mult), r=["lm_lp", "lm_pr"], w=["lm_pr"])
            P.op("dve", lambda e: e.tensor_reduce(out=sm[:], in_=pr[:], axis=AX.X, op=ALU.add), r=["lm_pr"], w=["lm_sm"])
            P.op("act", lambda e: e.activation(out=sm[:], in_=sm[:], func=AF.Exp), r=["lm_sm"], w=["lm_sm"])
            P.op("dve", lambda e: e.tensor_tensor(out=self.neglam[:], in0=sm[:, 1:2], in1=sm[:, 0:1], op=ALU.subtract), r=["lm_sm"], w=["neglam"])
            P.op("dve", lambda e: e.tensor_scalar(out=self.neglam[:], in0=self.neglam[:], scalar1=-LAM_INIT0, scalar2=None, op0=ALU.add),
                 r=["neglam"], w=["neglam"])
            P.barrier()

    def attention(self, kind, KT_d, QT_d, V_d, nheads, norm_gain, out_d, out_col0, gate_d=None):
        P = self.P
        nmap = 2 if kind == "diff" else 1
        dvp = 129 if (kind == "diff" or os.environ.get("RET_V129", "0") == "1") else 128
        dvo = 129 if kind == "diff" else 128
        with ExitStack() as st:
            KT = [P.sb("at_k%d" % i, [128, S_LEN], BF16, st) for i in range(2)]
            QT = [P.sb("at_q%d" % i, [128, S_LEN], BF16, st) for i in range(2)]
            Vt = [P.sb("at_v%d" % i, [128, NT, dvp], BF16, st) for i in range(2)]
            gn = P.sb("at_gn", [128, 128], F32, st)
            PTs = [P.sb("at_pt%d" % i, [128, 512], BF16, st) for i in range(3)]
            ex = [P.sb("at_ex%d" % i, [128, 256], F32, st) for i in range(2)]
            pS = [P.ps("at_ps%d" % i, [128, 512], F32, st) for i in range(2)]
            pO = [P.ps("at_po%d" % i, [128, 4, 256], F32, st) for i in range(nmap)]
            sm = P.sb("at_sm", [128, 8], F32, st)
            t0 = [P.sb("at_t0%d" % i, [128, 128], F32, st) for i in range(2)]
            junk = P.sb("at_jk", [128, 128], F32, st)
            og = [P.sb("at_og%d" % i, [128, 128], BF16, st) for i in range(2)]
            gt = [P.sb("at_gt%d" % i, [128, 128], F32, st) for i in range(2)] if gate_d is not None else None
            P.dma(gn[:], norm_gain.partition_broadcast(128), w=["at_gn"])
            if kind == "diff":
                P.op("dve", lambda e: e.tensor_scalar(out=gn[:], in0=gn[:], scalar1=1.0 - LAM_INIT0, scalar2=None, op0=ALU.mult),
                     r=["at_gn"], w=["at_gn"])
                for i in range(2):
                    P.op("pool", lambda e: e.memset(Vt[i][:, :, 128:129], 1.0), w=["at_v%d" % i])
            ipt = 0
            isx = 0
            ifin = 0
            for h in range(nheads if kind == "diff" else int(os.environ.get("RET_H", "8"))):
                hb = h % 2
                if kind == "diff":
                    if True:
                        P.dma(KT[hb][:], KT_d[h * 128:(h + 1) * 128, :], w=["at_k%d" % hb])
                        P.dma(QT[hb][:], QT_d[h * 128:(h + 1) * 128, :], w=["at_q%d" % hb])
                    kq_b, kbase = hb, None
                else:
                    if h % 2 == 0:
                        cb = (h // 2) % 2
                        P.dma(KT[cb][:], KT_d[(h // 2) * 128:(h // 2 + 1) * 128, :], w=["at_k%d" % cb])
                        P.dma(QT[cb][:], QT_d[(h // 2) * 128:(h // 2 + 1) * 128, :], w=["at_q%d" % cb])
                    kq_b = (h // 2) % 2
                P.dma(Vt[hb][:, :, 0:128], V_d[:, h * 128:(h + 1) * 128].rearrange("(j p) d -> p j d", p=128), w=["at_v%d" % hb])
                kid, qid, vid = "at_k%d" % kq_b, "at_q%d" % kq_b, "at_v%d" % hb
                for g in range(4):
                    for m in range(nmap):
                        pb = 64 * m if kind == "diff" else 64 * (h % 2)
                        Km = KT[kq_b][pb:pb + 64, :]
                        Qm = QT[kq_b][pb:pb + 64, :]
                        po = pO[m]
                        poid = "at_po%d" % m
                        for j in range(4 * g + 4):
                            c0 = max(0, j - 4 * g)
                            ps = pS[isx % 2]
                            psid = "at_ps%d" % (isx % 2)
                            isx += 1
                            P.op("pe", lambda e: e.matmul(ps[:, c0 * 128:512], lhsT=Km[:, j * 128:(j + 1) * 128],
                                                          rhs=Qm[:, g * 512 + c0 * 128:(g + 1) * 512], start=True, stop=True),
                                 r=[kid, qid], w=[psid])
                            pt = PTs[ipt % 3]
                            ptid = "at_pt%d" % (ipt % 3)
                            ipt += 1
                            near_lo = c0 * 128
                            if j >= 4 * g:
                                near_hi = min(512, near_lo + 256)
                                eb_lo = 0
                            else:
                                near_hi = 128 if j == 4 * g - 1 else 0
                                eb_lo = 128
                            nw = near_hi - near_lo if near_hi > near_lo else 0
                            if kind == "diff":
                                if nw > 0:
                                    exb = ex[ipt % 2]
                                    exid = "at_ex%d" % (ipt % 2)
                                    P.op("act", lambda e: e.activation(out=exb[:, 0:nw], in_=ps[:, near_lo:near_hi], func=AF.Exp, scale=0.125),
                                         r=[psid], w=[exid])
                                    P.op("dve", lambda e: e.tensor_tensor(out=pt[:, near_lo:near_hi], in0=exb[:, 0:nw],
                                                                          in1=self.EB[:, h, eb_lo:eb_lo + nw], op=ALU.mult),
                                         r=[exid, ("EB", h)], w=[(ptid, 0)])
                                far_lo = max(near_hi, near_lo)
                                if far_lo < 512:
                                    P.op("act", lambda e: e.activation(out=pt[:, far_lo:512], in_=ps[:, far_lo:512], func=AF.Exp,
                                                                       bias=self.tbl_bc[:, 31 * 8 + h:31 * 8 + h + 1], scale=0.125),
                                         r=[psid, "tbl_bc"], w=[(ptid, 1)])
                            else:
                                far_lo = near_lo
                                if j >= 4 * g:
                                    if os.environ.get("RET_MASK2", "1") == "1":
                                        exb = ex[ipt % 2]
                                        exid = "at_ex%d" % (ipt % 2)
                                        self.copy("act", exb[:, 0:128], ps[:, near_lo:near_lo + 128], [psid], [exid])
                                        P.op("dve", lambda e: e.tensor_tensor(out=pt[:, near_lo:near_lo + 128], in0=exb[:, 0:128],
                                                                              in1=self.mask01[:, 0:128], op=ALU.mult), r=[exid, "mask01"], w=[(ptid, 0)])
                                    else:
                                        P.op("dve", lambda e: e.tensor_tensor(out=pt[:, near_lo:near_lo + 128], in0=ps[:, near_lo:near_lo + 128],
                                                                              in1=self.mask01[:, 0:128], op=ALU.mult), r=[psid, "mask01"], w=[(ptid, 0)])
                                    far_lo = near_lo + 128
                                if far_lo < 512:
                                    self.copy(self.alt("act", "dve"), pt[:, far_lo:512], ps[:, far_lo:512], [psid], [(ptid, 1)])
                            for il in range(c0, 4):
                                P.op("pe", lambda e: e.matmul(po[:, il, 0:dvo], lhsT=pt[:, il * 128:(il + 1) * 128], rhs=Vt[hb][:, j, 0:dvo],
                                                              start=(j == 0 and il % 2 == 0), stop=(j == 4 * g + il),
                                                              skip_group_check=True), r=[(ptid, 0), (ptid, 1), vid], w=[poid])
                    for il in range(4 if int(os.environ.get("RET_FIN", "1")) or kind == "diff" else 0):
                        qb = 4 * g + il
                        f = ifin % 2
                        ifin += 1
                        tt, ttid = t0[f], "at_t0%d" % f
                        if kind == "diff":
                            P.op("dve", lambda e: e.reciprocal(out=sm[:, 0:1], in_=pO[0][:, il, 128:129]), r=["at_po0"], w=["at_sm"])
                            P.op("dve", lambda e: e.reciprocal(out=sm[:, 1:2], in_=pO[1][:, il, 128:129]), r=["at_po1", "at_sm"], w=["at_sm"])
                            P.op("dve", lambda e: e.tensor_tensor(out=sm[:, 2:3], in0=sm[:, 1:2], in1=self.neglam[:], op=ALU.mult),
                                 r=["at_sm", "neglam"], w=["at_sm"])
                            P.op("act", lambda e: e.activation(out=tt[:], in_=pO[0][:, il, 0:128], func=AF.Copy, scale=sm[:, 0:1]),
                                 r=["at_po0", "at_sm"], w=[ttid])
                            P.op("dve", lambda e: e.scalar_tensor_tensor(out=tt[:], in0=pO[1][:, il, 0:128], scalar=sm[:, 2:3], in1=tt[:],
                                                                         op0=ALU.mult, op1=ALU.add), r=["at_po1", "at_sm", ttid], w=[ttid])
                        else:
                            self.copy("act", tt[:], pO[0][:, il, 0:128], ["at_po0"], [ttid])
                        P.op("act", lambda e: e.activation(out=junk[:], in_=tt[:], func=AF.Square, accum_out=sm[:, 4:5]),
                             r=[ttid, "at_sm"], w=["at_jk", "at_sm"])
                        self.rstd_from_ss(sm[:, 5:6], sm[:, 4:5], 128, "at_sm", "at_sm")
                        if gate_d is None:
                            P.op("dve", lambda e: e.scalar_tensor_tensor(out=og[f][:], in0=tt[:], scalar=sm[:, 5:6], in1=gn[:],
                                                                         op0=ALU.mult, op1=ALU.mult), r=[ttid, "at_sm", "at_gn"], w=["at_og%d" % f])
                        else:
                            P.dma(gt[f][:], gate_d[qb * 128:(qb + 1) * 128, h * 128:(h + 1) * 128], w=["at_gt%d" % f])
                            P.op("dve", lambda e: e.scalar_tensor_tensor(out=tt[:], in0=tt[:], scalar=sm[:, 5:6], in1=gn[:],
                                                                         op0=ALU.mult, op1=ALU.mult), r=[ttid, "at_sm", "at_gn"], w=[ttid])
                            P.op("dve", lambda e: e.tensor_tensor(out=og[f][:], in0=tt[:], in1=gt[f][:], op=ALU.mult),
                                 r=[ttid, "at_gt%d" % f], w=["at_og%d" % f])
                        P.dma(out_d[qb * 128:(qb + 1) * 128, out_col0 + h * 128:out_col0 + (h + 1) * 128], og[f][:],
                              r=["at_og%d" % f], w=["at_out"], q="act")
            P.barrier()


class MK1(MK0):
    def layer0_mixer(self, x_ap, h_out_ap, w):
        P = self.P
        qaT = self.scratch("qaT", [1024, S_LEN], BF16)
        kaT = self.scratch("kaT", [1024, S_LEN], BF16)
        va = self.scratch("va", [S_LEN, 1024], BF16)
        qbT = self.scratch("qbT", [512, S_LEN], BF16)
        kbT = self.scratch("kbT", [512, S_LEN], BF16)
        vb = self.scratch("vb", [S_LEN, 1024], BF16)
        gbs = self.scratch("gbs", [S_LEN, 1024], F32)
        mixed = self.scratch("mixed0", [S_LEN, 2048], BF16)
        Win = w["ab_w_in"].ap()[0]
        with ExitStack() as st:
            hT = P.sb("hT", [128, 16, S_LEN], BF16, st)
            self.loadT(x_ap, DM, hT, "hT", gain=w["norm_mix"].ap()[0:1, :])
            if getattr(self, "stop", 99) <= 1:
                return
            with ExitStack() as s2:
                self.linear(hT, "hT", 16, Win, 0, 1024, "feat", self.make_store_sink(s2, qaT.ap(), "feat", BF16, tag="sqa"))
            if getattr(self, "stop", 99) <= 2:
                return
            with ExitStack() as s2:
                self.linear(hT, "hT", 16, Win, 1024, 1024, "feat", self.make_store_sink(s2, kaT.ap(), "feat", BF16, tag="ska"))
            with ExitStack() as s2:
                self.linear(hT, "hT", 16, Win, 2048, 1024, "tok", self.make_store_sink(s2, va.ap(), "tok", BF16, tag="sva"))
            if getattr(self, "stop", 99) <= 3:
                return
            for (c0, dst, tab, tg_) in ((3072, qbT, w["c_ropeq"], "sqb"), (3584, kbT, w["c_ropek"], "skb")):
                with ExitStack() as s2:
                    cs = [P.sb("%s_c%d" % (tg_, i), [128, 2, 512], F32, s2) for i in range(2)]
                    t1 = [P.sb("%s_a%d" % (tg_, i), [128, 512], F32, s2) for i in range(2)]
                    t2 = [P.sb("%s_b%d" % (tg_, i), [128, 512], F32, s2) for i in range(2)]
                    so = [P.sb("%s_o%d" % (tg_, i), [128, 512], BF16, s2) for i in range(2)]
                    cnt = [0]

                    def rsink(chunk, tg, ps, pid, ps2, pid2, dst=dst, tab=tab, tg_=tg_, cs=cs, t1=t1, t2=t2, so=so, cnt=cnt):
                        i = cnt[0] % 2
                        cnt[0] += 1
                        P.dma(cs[i][:], tab.ap()[chunk, :, :, tg * 512:(tg + 1) * 512].rearrange("a p t -> p a t"), w=["%s_c%d" % (tg_, i)])
                        P.op("dve", lambda e: e.tensor_tensor(out=t1[i][:], in0=ps[:, :], in1=cs[i][:, 0, :], op=ALU.mult),
                             r=[pid, "%s_c%d" % (tg_, i)], w=["%s_a%d" % (tg_, i)])
                        P.op("dve", lambda e: e.tensor_tensor(out=t2[i][:], in0=ps2[:, :], in1=cs[i][:, 1, :], op=ALU.mult),
                             r=[pid2, "%s_c%d" % (tg_, i)], w=["%s_b%d" % (tg_, i)])
                        P.op("pool", lambda e: e.tensor_tensor(out=so[i][:], in0=t1[i][:], in1=t2[i][:], op=ALU.add),
                             r=["%s_a%d" % (tg_, i), "%s_b%d" % (tg_, i)], w=["%s_o%d" % (tg_, i)])
                        P.dma(dst.ap()[chunk * 128:(chunk + 1) * 128, tg * 512:(tg + 1) * 512], so[i][:], r=["%s_o%d" % (tg_, i)], w=[tg_ + "_d"], q="act")
                    self.linear(hT, "hT", 16, Win, c0, 512, "feat", rsink, slab=256, swap=True)
            with ExitStack() as s2:
                self.linear(hT, "hT", 16, Win, 4096, 1024, "tok", self.make_store_sink(s2, vb.ap(), "tok", BF16, tag="svb"))
            with ExitStack() as s2:
                self.linear(hT, "hT", 16, Win, 5120, 1024, "tok", self.make_store_sink(s2, gbs.ap(), "tok", F32, func=AF.Silu, tag="sgb"))
        P.barrier()
        if getattr(self, "stop", 99) <= 4:
            return
        if getattr(self, "stop", 99) != 7:
            self.attention("diff", kaT.ap(), qaT.ap(), va.ap(), 8, w["ab_a_norm"].ap()[0:1, :], mixed.ap(), 0)
        if getattr(self, "stop", 99) <= 5:
            return
        if os.environ.get("RET_DATA", "b") == "a":
            kbT, qbT, vb = kaT, qaT, va
        self.attention("ret", kbT.ap(), qbT.ap(), vb.ap(), 8, w["ab_b_norm"].ap()[0:1, :], mixed.ap(), 1024, gate_d=gbs.ap())
        if getattr(self, "stop", 99) <= 7:
            return
        self.out_proj(mixed.ap(), 2048, w["ab_w_out"].ap()[0], x_ap, h_out_ap, BF16)

    def out_proj(self, mixed_ap, K, Wout, res_ap, out_ap, src_dt):
        P = self.P
        KC = K // 128
        with ExitStack() as st:
            mT = P.sb("mT", [128, KC, S_LEN], BF16, st)
            self.loadT(mixed_ap, K, mT, "mT", gain=None, src_dt=src_dt)
            rs = [P.sb("op_r%d" % i, [128, 512], F32, st) for i in range(3)]
            cnt = [0]

            def sink(t, coff, n, ps, pid):
                i = cnt[0] % 3
                cnt[0] += 1
                P.dma(rs[i][:, 0:n], res_ap[t * 128:(t + 1) * 128, coff:coff + n], w=["op_r%d" % i])
                P.op("dve", lambda e: e.tensor_tensor(out=rs[i][:, 0:n], in0=ps[:, 0:n], in1=rs[i][:, 0:n], op=ALU.add),
                     r=[pid, "op_r%d" % i], w=["op_r%d" % i])
                P.dma(out_ap[t * 128:(t + 1) * 128, coff:coff + n], rs[i][:, 0:n], r=["op_r%d" % i], w=["op_out"], q="act")
            self.linear(mT, "mT", KC, Wout, 0, DM, "tok", sink, slab=(512 if KC <= 16 else 256))
        P.barrier()


class MK2(MK1):
    def peer(self, h_ap, out_ap, gain_ap, Wq, keys, U, V, lid):
        P = self.P
        xT_d = self.scratch("pe_xT%d" % lid, [128, 16, S_LEN], BF16)
        S1_d = self.scratch("pe_S1%d" % lid, [S_LEN, 8, 128], F32)
        S0_d = self.scratch("pe_S0%d" % lid, [S_LEN, 8, 128], F32)
        TH_d = self.scratch("pe_TH%d" % lid, [S_LEN, 8], F32)
        NEG = -1.0e30
        NTT = int(os.environ.get("PEER_NT", "16"))
        with ExitStack() as st:
            xT = P.sb("pe_xT", [128, 16, S_LEN], BF16, st)
            self.loadT(h_ap, DM, xT, "pe_xT", gain=gain_ap)
            P.dma(xT_d.ap(), xT[:], r=["pe_xT"], w=["pe_xT_d"], q="act")
            qT = P.sb("pe_qT", [128, 16, S_LEN], BF16, st)

            def qsink(chunk, tg, ps, pid):
                self.copy(self.alt("act", "dve"), qT[:, chunk, tg * 512:(tg + 1) * 512], ps[:, :], [pid], ["pe_qT"])
            self.linear(xT, "pe_xT", 16, Wq, 0, 2048, "feat", qsink, slab=256)
            k32 = P.sb("pe_k32", [128, 16, 128], F32, st)
            kb = P.sb("pe_kb", [128, 16, 128], BF16, st)
            kT = P.sb("pe_kT", [128, 16, 128], BF16, st)
            ptk = P.ps("pe_ptk", [128, 1024], BF16, st)
            P.dma(k32[:], keys.rearrange("h c k d -> k (h c) d"), w=["pe_k32"])
            self.copy("dve", kb[:], k32[:], ["pe_k32"], ["pe_kb"])
            for half in range(2):
                for kk in range(8):
                    hc = half * 8 + kk
                    P.op("pe", lambda e: e.transpose(out=ptk[:, kk * 128:(kk + 1) * 128], in_=kb[:, hc, :], identity=self.ident[:]),
                         r=["pe_kb", "ident"], w=["pe_ptk"])
                self.copy("act", kT[:, half * 8:(half + 1) * 8, :], ptk[:].rearrange("p (a b) -> p a b", b=128), ["pe_ptk"], ["pe_kT"])
            psS = [P.ps("pe_pss%d" % i, [128, 4, 128], F32, st) for i in range(2)]
            Ssb = [P.sb("pe_S%d" % i, [128, 16, 128], F32, st) for i in range(2)]
            top = P.sb("pe_top", [128, 16, 16], F32, st)
            tmp = P.sb("pe_tmp", [128, 128], F32, st)
            cand = P.sb("pe_cand", [128, 8, 256], F32, st)
            tm2 = P.sb("pe_tm2", [128, 256], F32, st)
            tm3 = P.sb("pe_tm3", [128, 256], F32, st)
            bb = P.sb("pe_bb", [128, 8, 16], F32, st)
            b17 = P.sb("pe_b17", [128, 8, 8], F32, st)
            eb = P.sb("pe_eb", [128, 8, 16], F32, st)
            sc = P.sb("pe_sc", [128, 6, 8], F32, st)
            s0s = [P.sb("pe_s0s%d" % i, [128, 8, 128], F32, st) for i in range(2)]
            ips = 0
            for t in range(NTT):
                Sb = Ssb[t % 2]
                Sid = "pe_S%d" % (t % 2)
                for q4 in range(4):
                    ps = psS[ips % 2]
                    psid = "pe_pss%d" % (ips % 2)
                    ips += 1
                    for kk in range(4):
                        hc = q4 * 4 + kk
                        P.op("pe", lambda e: e.matmul(ps[:, kk, :], lhsT=qT[:, hc, t * 128:(t + 1) * 128], rhs=kT[:, hc, :],
                                                      start=True, stop=True, skip_group_check=True), r=["pe_qT", "pe_kT"], w=[psid])
                    self.copy(self.alt("act", "dve"), Sb[:, q4 * 4:(q4 + 1) * 4, :], ps[:, :, :], [psid], [(Sid, q4)])
                Sall = [(Sid, q4) for q4 in range(4)]
                P.dma(S1_d.ap()[t * 128:(t + 1) * 128, :, :], Sb[:].rearrange("p (h c) k -> p h c k", c=2)[:, :, 1, :], r=Sall, w=["pe_S1_d"], q="act")
                for hc in range(16):
                    P.op("dve", lambda e: e.max(out=top[:, hc, 0:8], in_=Sb[:, hc, :]), r=Sall, w=["pe_top"])
                    P.op("dve", lambda e: e.match_replace(out=tmp[:], in_to_replace=top[:, hc, 0:8], in_values=Sb[:, hc, :], imm_value=NEG),
                         r=Sall + ["pe_top"], w=["pe_tmp"])
                    P.op("dve", lambda e: e.max(out=top[:, hc, 8:16], in_=tmp[:]), r=["pe_tmp"], w=["pe_top"])
                t4 = top[:].rearrange("p (h c) k -> p h c k", c=2)
                P.op("pool", lambda e: e.tensor_tensor(out=cand[:].rearrange("p h (a b) -> p h a b", b=16),
                                                       in0=t4[:, :, 0, :].unsqueeze(3).to_broadcast([128, 8, 16, 16]),
                                                       in1=t4[:, :, 1, :].unsqueeze(2).to_broadcast([128, 8, 16, 16]), op=ALU.add),
                     r=["pe_top"], w=["pe_cand"])
                for h in range(8):
                    P.op("dve", lambda e: e.max(out=bb[:, h, 0:8], in_=cand[:, h, :]), r=["pe_cand"], w=["pe_bb"])
                    P.op("dve", lambda e: e.match_replace(out=tm2[:], in_to_replace=bb[:, h, 0:8], in_values=cand[:, h, :], imm_value=NEG),
                         r=["pe_cand", "pe_bb"], w=["pe_tm2"])
                    P.op("dve", lambda e: e.max(out=bb[:, h, 8:16], in_=tm2[:]), r=["pe_tm2"], w=["pe_bb"])
                P.op("dve", lambda e: e.tensor_tensor(out=eb[:], in0=bb[:], in1=bb[:, :, 0:1].to_broadcast([128, 8, 16]), op=ALU.subtract),
                     r=["pe_bb"], w=["pe_eb"])
                P.op("act", lambda e: e.activation(out=eb[:], in_=eb[:], func=AF.Exp), r=["pe_eb"], w=["pe_eb"])
                P.op("dve", lambda e: e.tensor_reduce(out=sc[:, 0, :], in_=eb[:], axis=AX.X, op=ALU.add), r=["pe_eb"], w=["pe_sc"])
                P.op("act", lambda e: e.activation(out=sc[:, 0, :], in_=sc[:, 0, :], func=AF.Ln), r=["pe_sc"], w=["pe_sc"])
                P.op("dve", lambda e: e.tensor_tensor(out=sc[:, 1, :], in0=sc[:, 0, :], in1=bb[:, :, 0], op=ALU.add), r=["pe_sc", "pe_bb"], w=["pe_sc"])
                P.op("dve", lambda e: e.tensor_tensor(out=sc[:, 2, :], in0=bb[:, :, 15], in1=sc[:, 1, :], op=ALU.subtract), r=["pe_bb", "pe_sc"], w=["pe_sc"])
                P.op("dve", lambda e: e.tensor_scalar(out=sc[:, 3, :], in0=sc[:, 2, :], scalar1=-5.0e-6, scalar2=None, op0=ALU.add),
                     r=["pe_sc"], w=["pe_sc"])
                P.dma(TH_d.ap()[t * 128:(t + 1) * 128, :], sc[:, 3, :], r=["pe_sc"], w=["pe_TH_d"], q="act")
                if "pe_dbg" in self.dbg:
                    if t == 0:
                        self.dbg_bb = self.scratch("pe_dbg", [S_LEN, 8 * 16 + 8 * 8 + 16 * 16], F32)
                    P.dma(self.dbg_bb.ap()[t * 128:(t + 1) * 128, 0:128], bb[:].rearrange("p a b -> p (a b)"), r=["pe_bb"], w=["dbg1"], q="act")
                    P.dma(self.dbg_bb.ap()[t * 128:(t + 1) * 128, 192:448], top[:].rearrange("p a b -> p (a b)"), r=["pe_top"], w=["dbg3"], q="act")
                s0 = s0s[t % 2]
                P.op("pool", lambda e: e.tensor_tensor(out=s0[:], in0=Sb[:].rearrange("p (h c) k -> p h c k", c=2)[:, :, 0, :],
                                                       in1=sc[:, 1, :].unsqueeze(2).to_broadcast([128, 8, 128]), op=ALU.subtract),
                     r=Sall + ["pe_sc"], w=["pe_s0s%d" % (t % 2)])
                P.dma(S0_d.ap()[t * 128:(t + 1) * 128, :, :], s0[:], r=["pe_s0s%d" % (t % 2)], w=["pe_S0_d"], q="act")
        P.barrier()
        if int(os.environ.get("PEER_STOP", "9")) <= 3:
            return
        NTG = int(os.environ.get("PEER_NTG", "4"))
        NEG_ = int(os.environ.get("PEER_NEG", "32"))
        with ExitStack() as st:
            xg = P.sb("pp_xg", [128, 16, 512], BF16, st)
            acc = P.sb("pp_acc", [128, 4, DM], F32, st)
            S1g = P.sb("pp_S1g", [128, 4, 8, 128], F32, st)
            THg = P.sb("pp_THg", [128, 4, 8], F32, st)
            S0e = [P.sb("pp_S0e%d" % i, [128, 4, 8, 4], F32, st) for i in range(2)]
            stg = [P.sb("pp_stg%d" % i, [128, DM], F32, st) for i in range(2)]
            ubf = [P.sb("pp_ubf%d" % i, [128, DM], BF16, st) for i in range(2)]
            uT = [P.sb("pp_uT%d" % i, [128, 16, 128], BF16, st) for i in range(8)]
            vbf = [P.sb("pp_vb%d" % i, [128, DM], BF16, st) for i in range(8)]
            Dt = [P.sb("pp_D%d" % i, [128, 4, 128], F32, st) for i in range(2)]
            Ex = [P.sb("pp_Ex%d" % i, [128, 4, 128], BF16, st) for i in range(2)]
            Ft = [P.sb("pp_F%d" % i, [128, 4, 128], BF16, st) for i in range(3)]
            gA = [P.sb("pp_gA%d" % i, [128, 4, 512], BF16, st) for i in range(2)]
            Gs = [P.sb("pp_Gs%d" % i, [128, 4, 128], F32, st) for i in range(2)]
            GA = [P.sb("pp_GA%d" % i, [128, 4, 128], BF16, st) for i in range(2)]
            hres = [P.sb("pp_hr%d" % i, [128, 512], F32, st) for i in range(2)]
            pV = P.ps("pp_pV", [128, 4, 512], F32, st)
            pG = P.ps("pp_pG", [128, 4, 128], F32, st)
            pA = [P.ps("pp_pA%d" % i, [128, 512], F32, st) for i in range(2)]
            pT = P.ps("pp_pT", [128, 1024], BF16, st)
            istg = 0
            ich = 0
            iD = 0
            iF = 0
            iA = 0
            iG = 0
            for tg in range(NTG):
                P.dma(xg[:], xT_d.ap()[:, :, tg * 512:(tg + 1) * 512], w=["pp_xg"])
                P.dma(S1g[:], S1_d.ap()[tg * 512:(tg + 1) * 512, :, :].rearrange("(a p) h k -> p a h k", p=128), w=["pp_S1g"])
                P.dma(THg[:], TH_d.ap()[tg * 512:(tg + 1) * 512, :].rearrange("(a p) h -> p a h", p=128), w=["pp_THg"])
                for eg in range(NEG_):
                    slots = []
                    for c in range(4):
                        ci = eg * 4 + c
                        sl = ich % 8
                        ich += 1
                        slots.append(sl)
                        sb_ = istg % 2
                        istg += 1
                        P.dma(stg[sb_][:], U[ci * 128:(ci + 1) * 128, :], w=["pp_stg%d" % sb_])
                        ub = ubf[ci % 2]
                        ubid = "pp_ubf%d" % (ci % 2)
                        self.copy("pool", ub[:, 0:1024], stg[sb_][:, 0:1024], ["pp_stg%d" % sb_], [(ubid, 0)])
                        self.copy("act", ub[:, 1024:2048], stg[sb_][:, 1024:2048], ["pp_stg%d" % sb_], [(ubid, 1)])
                        for half in range(2):
                            for kk in range(8):
                                k = half * 8 + kk
                                P.op("pe", lambda e: e.transpose(out=pT[:, kk * 128:(kk + 1) * 128], in_=ub[:, k * 128:(k + 1) * 128],
                                                                 identity=self.ident[:]), r=[(ubid, 0), (ubid, 1), "ident"], w=["pp_pT"])
                            self.copy(self.alt("act", "dve"), uT[sl][:, half * 8:(half + 1) * 8, :],
                                      pT[:].rearrange("p (a b) -> p a b", b=128), ["pp_pT"], [("pp_uT%d" % sl, half)])
                        sb_ = istg % 2
                        istg += 1
                        P.dma(stg[sb_][:], V[ci * 128:(ci + 1) * 128, :], w=["pp_stg%d" % sb_])
                        self.copy("pool", vbf[sl][:, 0:1024], stg[sb_][:, 0:1024], ["pp_stg%d" % sb_], [("pp_vb%d" % sl, 0)])
                        self.copy("dve", vbf[sl][:, 1024:2048], stg[sb_][:, 1024:2048], ["pp_stg%d" % sb_], [("pp_vb%d" % sl, 1)])
                    s0b = eg % 2
                    for tt in range(4):
                        P.dma(S0e[s0b][:, tt, :, :], S0_d.ap()[tg * 512 + tt * 128:tg * 512 + (tt + 1) * 128, :, eg * 4:(eg + 1) * 4],
                              w=["pp_S0e%d" % s0b])
                    gAb = gA[eg % 2]
                    gAid = "pp_gA%d" % (eg % 2)
                    for c in range(4):
                        sl = slots[c]
                        pa = pA[iA % 2]
                        paid = "pp_pA%d" % (iA % 2)
                        iA += 1
                        for k in range(16):
                            P.op("pe", lambda e: e.matmul(pa[:, :], lhsT=uT[sl][:, k, :], rhs=xg[:, k, :], start=(k == 0), stop=(k == 15)),
                                 r=[("pp_uT%d" % sl, 0), ("pp_uT%d" % sl, 1), "pp_xg"], w=[paid])
                        P.op("act", lambda e: e.activation(out=gAb[:, c, :], in_=pa[:, :], func=AF.Gelu_apprx_tanh), r=[paid], w=[(gAid, c)])
                    for tt in range(4):
                        for h in range(8):
                            Db = Dt[iD % 2]
                            Did = "pp_D%d" % (iD % 2)
                            Eb = Ex[iD % 2]
                            Eid = "pp_Ex%d" % (iD % 2)
                            iD += 1
                            Fb = Ft[iF % 3]
                            Fid = "pp_F%d" % (iF % 3)
                            iF += 1
                            P.op("pool", lambda e: e.tensor_tensor(out=Db[:], in0=S1g[:, tt, h, :].unsqueeze(1).to_broadcast([128, 4, 128]),
                                                                   in1=S0e[s0b][:, tt, h, :].unsqueeze(2).to_broadcast([128, 4, 128]), op=ALU.add),
                                 r=["pp_S1g", "pp_S0e%d" % s0b], w=[Did])
                            P.op("act", lambda e: e.activation(out=Eb[:], in_=Db[:], func=AF.Exp), r=[Did], w=[Eid])
                            P.op("dve", lambda e: e.scalar_tensor_tensor(out=Fb[:], in0=Db[:], scalar=THg[:, tt, h:h + 1], in1=Eb[:],
                                                                         op0=ALU.is_ge, op1=ALU.mult), r=[Did, Eid, "pp_THg"], w=[Fid])
                            for c in range(4):
                                P.op("pe", lambda e: e.matmul(pG[:, c, :], lhsT=Fb[:, c, :], rhs=self.ident[:], start=(h == 0 and c == 0),
                                                              stop=(h == 7), skip_group_check=True), r=[Fid, "ident"], w=["pp_pG"])
                        g_ = iG % 2
                        iG += 1
                        self.copy("act", Gs[g_][:], pG[:], ["pp_pG"], ["pp_Gs%d" % g_])
                        P.op("dve", lambda e: e.tensor_tensor(out=GA[g_][:], in0=Gs[g_][:], in1=gAb[:, :, tt * 128:(tt + 1) * 128], op=ALU.mult),
                             r=["pp_Gs%d" % g_] + [(gAid, c) for c in range(4)], w=["pp_GA%d" % g_])
                        for c in range(4):
                            sl = slots[c]
                            for dq in range(4):
                                P.op("pe", lambda e: e.matmul(pV[:, dq, :], lhsT=GA[g_][:, c, :], rhs=vbf[sl][:, dq * 512:(dq + 1) * 512],
                                                              start=(c == 0), stop=(c == 3)),
                                     r=["pp_GA%d" % g_, ("pp_vb%d" % sl, 0), ("pp_vb%d" % sl, 1)], w=[("pp_pV", dq)])
                        for dq in range(4):
                            if eg == 0:
                                self.copy("dve", acc[:, tt, dq * 512:(dq + 1) * 512], pV[:, dq, :], [("pp_pV", dq)], [("pp_acc", tt, dq)])
                            else:
                                P.op("dve", lambda e: e.tensor_tensor(out=acc[:, tt, dq * 512:(dq + 1) * 512], in0=pV[:, dq, :],
                                                                      in1=acc[:, tt, dq * 512:(dq + 1) * 512], op=ALU.add),
                                     r=[("pp_pV", dq), ("pp_acc", tt, dq)], w=[("pp_acc", tt, dq)])
                for tt in range(4):
                    for dq in range(4):
                        i = (tt * 4 + dq) % 2
                        rows = slice(tg * 512 + tt * 128, tg * 512 + (tt + 1) * 128)
                        P.dma(hres[i][:], h_ap[rows, dq * 512:(dq + 1) * 512], w=["pp_hr%d" % i])
                        P.op("pool", lambda e: e.tensor_tensor(out=hres[i][:], in0=hres[i][:], in1=acc[:, tt, dq * 512:(dq + 1) * 512], op=ALU.add),
                             r=["pp_hr%d" % i, ("pp_acc", tt, dq)], w=["pp_hr%d" % i])
                        P.dma(out_ap[rows, dq * 512:(dq + 1) * 512], hres[i][:], r=["pp_hr%d" % i], w=["pp_out"], q="act")
        P.barrier()


def host_consts1():
    c = {}
    s = np.arange(128)[:, None]
    t = np.arange(128)[None, :]
    c["c_tri"] = (s <= t).astype(np.float32)
    c["c_negmT"] = np.where(s <= t, 0.0, -1.0e30).astype(np.float32)
    c["c_negm"] = np.where(t <= s, 0.0, -1.0e30).astype(np.float32)
    c["c_low01"] = (t <= s).astype(np.float32)
    sel = np.zeros((128, 128), np.float32)
    sel[127, :] = 1.0
    c["c_sel127"] = sel
    c["c_onesf"] = np.ones((128, 128), np.float32)
    return c


class MK3(MK2):
    def l1_consts(self, w):
        P = self.P
        self.identf = P.sb("identf", [128, 128], F32)
        self.tri = P.sb("tri", [128, 128], F32)
        self.negmT = P.sb("negmT", [128, 128], F32)
        self.negm = P.sb("negm", [128, 128], F32)
        self.low01 = P.sb("low01", [128, 128], F32)
        self.sel127 = P.sb("sel127", [128, 128], F32)
        self.onesf = P.sb("onesf", [128, 128], F32)
        for t_, nm in ((self.identf, "c_ident"), (self.tri, "c_tri"), (self.negmT, "c_negmT"), (self.negm, "c_negm"),
                       (self.low01, "c_low01"), (self.sel127, "c_sel127"), (self.onesf, "c_onesf")):
            P.dma(t_[:], w[nm].ap(), w=[nm])
        P.barrier()

    def layer1_proj(self, h_ap, w):
        P = self.P
        L = {}
        L["zs"] = self.scratch("zs", [S_LEN, 2048], F32)
        L["xs"] = self.scratch("xs1", [S_LEN, 2048], BF16)
        L["BT"] = self.scratch("BT1", [512, S_LEN], BF16)
        L["CT"] = self.scratch("CT1", [512, S_LEN], BF16)
        L["Btok"] = self.scratch("Btok1", [S_LEN, 512], BF16)
        L["dt"] = self.scratch("dt1", [S_LEN, 32], F32)
        L["dA"] = self.scratch("dA1", [S_LEN, 32], F32)
        L["qT"] = self.scratch("qT1", [1024, S_LEN], BF16)
        L["ckvn"] = self.scratch("ckvn1", [S_LEN, 256], BF16)
        L["ckvnT"] = self.scratch("ckvnT1", [256, S_LEN], BF16)
        L["qiT"] = self.scratch("qiT1", [1024, S_LEN], BF16)
        L["kiT"] = self.scratch("kiT1", [64, S_LEN], BF16)
        L["wi"] = self.scratch("wi1", [S_LEN, 16], F32)
        self.l1 = L
        Win = w["cd_w_in"].ap()[0]
        with ExitStack() as st:
            hT = P.sb("hT1", [128, 16, S_LEN], BF16, st)
            self.loadT(h_ap, DM, hT, "hT1", gain=w["norm_mix"].ap()[1:2, :])
            L1P = os.environ.get("L1P", "zcdqikx")
            with ExitStack() as s2:
                if "z" in L1P: self.linear(hT, "hT1", 16, Win, 0, 2048, "tok", self.make_store_sink(s2, L["zs"].ap(), "tok", F32, func=AF.Silu, tag="szs"))
            with ExitStack() as s2:
                cw = P.sb("cv_w", [128, 4, 24], F32, s2)
                cb = P.sb("cv_b", [128, 24], F32, s2)
                for c4 in range(0, 24, 4):
                    for k in range(4):
                        P.dma(cw[:, k, c4:c4 + 4], w["cd_conv_w"].ap()[0, k, c4 * 128:(c4 + 4) * 128].rearrange("(c p) -> p c", p=128), w=["cv_w"],
                              allow_slow_non_contiguous=True)
                    P.dma(cb[:, c4:c4 + 4], w["cd_conv_b"].ap()[0, c4 * 128:(c4 + 4) * 128].rearrange("(c p) -> p c", p=128), w=["cv_b"],
                          allow_slow_non_contiguous=True)
                rb = [P.sb("cv_r%d" % i, [128, 3 + S_LEN], F32, s2) for i in range(2)]
                ac = [P.sb("cv_a%d" % i, [128, S_LEN], F32, s2) for i in range(2)]
                ob = [P.sb("cv_o%d" % i, [128, S_LEN], BF16, s2) for i in range(2)]
                tb = [P.sb("cv_t%d" % i, [128, 512], BF16, s2) for i in range(2)]
                ptc = [P.ps("cv_p%d" % i, [128, 512], BF16, s2) for i in range(2)]
                for i in range(2):
                    P.op("pool", lambda e: e.memset(rb[i][:, 0:3], 0.0), w=["cv_r%d" % i])
                itc = [0]

                def csink(chunk, tg, ps, pid):
                    i = chunk % 2
                    rid = "cv_r%d" % i
                    self.copy(self.alt("act", "dve"), rb[i][:, 3 + tg * 512:3 + (tg + 1) * 512], ps[:, :], [pid], [(rid, tg)])
                    if tg != 3:
                        return
                    rall = [rid] + [(rid, k) for k in range(4)]
                    aid = "cv_a%d" % i
                    P.op("dve", lambda e: e.tensor_scalar(out=ac[i][:], in0=rb[i][:, 3:3 + S_LEN], scalar1=cw[:, 3, chunk:chunk + 1], scalar2=None, op0=ALU.mult),
                         r=rall + ["cv_w"], w=[aid])
                    for k in range(3):
                        P.op("dve", lambda e: e.scalar_tensor_tensor(out=ac[i][:], in0=rb[i][:, k:k + S_LEN], scalar=cw[:, k, chunk:chunk + 1], in1=ac[i][:],
                                                                     op0=ALU.mult, op1=ALU.add), r=rall + ["cv_w", aid], w=[aid])
                    oid = "cv_o%d" % i
                    P.op("act", lambda e: e.activation(out=ob[i][:], in_=ac[i][:], func=AF.Silu, bias=cb[:, chunk:chunk + 1]), r=[aid, "cv_b"], w=[oid])
                    if chunk >= 16:
                        dst = L["BT"] if chunk < 20 else L["CT"]
                        r0 = (chunk - 16) % 4
                        P.dma(dst.ap()[r0 * 128:(r0 + 1) * 128, :], ob[i][:], r=[oid], w=["cv_d"], q="act")
                    if chunk < 20:
                        for t4 in range(4):
                            j = itc[0] % 2
                            itc[0] += 1
                            for kk in range(4):
                                t = t4 * 4 + kk
                                P.op("pe", lambda e: e.transpose(out=ptc[j][:, kk * 128:(kk + 1) * 128], in_=ob[i][:, t * 128:(t + 1) * 128],
                                                                 identity=self.ident[:]), r=[oid, "ident"], w=["cv_p%d" % j])
                            self.copy(self.alt("act", "dve"), tb[j][:], ptc[j][:], ["cv_p%d" % j], ["cv_t%d" % j])
                            if chunk < 16:
                                dd = L["xs"].ap()[t4 * 512:(t4 + 1) * 512, chunk * 128:(chunk + 1) * 128]
                            else:
                                dd = L["Btok"].ap()[t4 * 512:(t4 + 1) * 512, (chunk - 16) * 128:(chunk - 15) * 128]
                            P.dma(dd.rearrange("(a p) c -> p a c", p=128), tb[j][:].rearrange("p (a c) -> p a c", c=128), r=["cv_t%d" % j], w=["cv_d2"], q="act")
                if "c" in L1P: self.linear(hT, "hT1", 16, Win, 2048, 3072, "feat", csink, slab=256)
            with ExitStack() as s2:
                dbb = P.sb("dt_b", [128, 32], F32, s2)
                aa = P.sb("dt_a", [128, 32], F32, s2)
                one = P.sb("dt_1", [128, 1], F32, s2)
                xx = [P.sb("dt_x%d" % i, [128, 32], F32, s2) for i in range(2)]
                yy = [P.sb("dt_y%d" % i, [128, 32], F32, s2) for i in range(2)]
                zz = [P.sb("dt_z%d" % i, [128, 32], F32, s2) for i in range(2)]
                P.dma(dbb[:], w["cd_dt_bias"].ap()[0:1, :].partition_broadcast(128), w=["dt_b"])
                P.dma(aa[:], w["cd_a_log"].ap()[0:1, :].partition_broadcast(128), w=["dt_a"])
                P.op("act", lambda e: e.activation(out=aa[:], in_=aa[:], func=AF.Exp), r=["dt_a"], w=["dt_a"])
                P.op("pool", lambda e: e.memset(one[:], 1.0), w=["dt_1"])

                def dsink(t, coff, n, ps, pid):
                    i = t % 2
                    P.op("dve", lambda e: e.tensor_tensor(out=xx[i][:], in0=ps[:, 0:32], in1=dbb[:], op=ALU.add), r=[pid, "dt_b"], w=["dt_x%d" % i])
                    P.op("act", lambda e: e.activation(out=yy[i][:], in_=xx[i][:], func=AF.Abs), r=["dt_x%d" % i], w=["dt_y%d" % i])
                    P.op("act", lambda e: e.activation(out=yy[i][:], in_=yy[i][:], func=AF.Exp, scale=-1.0), r=["dt_y%d" % i], w=["dt_y%d" % i])
                    P.op("act", lambda e: e.activation(out=yy[i][:], in_=yy[i][:], func=AF.Ln, bias=one[:, 0:1]), r=["dt_y%d" % i, "dt_1"], w=["dt_y%d" % i])
                    P.op("dve", lambda e: e.scalar_tensor_tensor(out=xx[i][:], in0=xx[i][:], scalar=0.0, in1=yy[i][:], op0=ALU.max, op1=ALU.add),
                         r=["dt_x%d" % i, "dt_y%d" % i], w=["dt_x%d" % i])
                    P.dma(L["dt"].ap()[t * 128:(t + 1) * 128, :], xx[i][:], r=["dt_x%d" % i], w=["dt_d"], q="act")
                    P.op("dve", lambda e: e.scalar_tensor_tensor(out=zz[i][:], in0=xx[i][:], scalar=-1.0, in1=aa[:], op0=ALU.mult, op1=ALU.mult),
                         r=["dt_x%d" % i, "dt_a"], w=["dt_z%d" % i])
                    P.dma(L["dA"].ap()[t * 128:(t + 1) * 128, :], zz[i][:], r=["dt_z%d" % i], w=["dA_d"], q="act")
                if "d" in L1P: self.linear(hT, "hT1", 16, Win, 5120, 32, "tok", dsink, slab=32)
            with ExitStack() as s2:
                if "q" in L1P: self.linear(hT, "hT1", 16, Win, 5152, 1024, "feat", self.make_store_sink(s2, L["qT"].ap(), "feat", BF16, tag="sq1"))
            with ExitStack() as s2:
                if "i" in L1P: self.linear(hT, "hT1", 16, Win, 6432, 1024, "feat", self.make_store_sink(s2, L["qiT"].ap(), "feat", BF16, tag="sqi"))
            with ExitStack() as s2:
                kg = P.sb("kv_g", [128, 256], F32, s2)
                P.dma(kg[:], w["cd_kv_norm"].ap()[0:1, :].partition_broadcast(128), w=["kv_g"])
                jk = P.sb("kv_j", [128, 256], F32, s2)
                ss = P.sb("kv_s", [128, 4], F32, s2)
                kn = [P.sb("kv_n%d" % i, [128, 256], BF16, s2) for i in range(2)]
                kt = [P.sb("kv_t%d" % i, [128, 256], BF16, s2) for i in range(2)]
                pk = [P.ps("kv_p%d" % i, [128, 256], BF16, s2) for i in range(2)]

                def ksink(t, coff, n, ps, pid):
                    i = t % 2
                    P.op("act", lambda e: e.activation(out=jk[:], in_=ps[:, 0:256], func=AF.Square, accum_out=ss[:, i:i + 1]), r=[pid], w=["kv_j", "kv_s%d" % i])
                    self.rstd_from_ss(ss[:, 2 + i:3 + i], ss[:, i:i + 1], 256, "kv_r%d" % i, "kv_s%d" % i)
                    P.op("act", lambda e: e.activation(out=jk[:], in_=ps[:, 0:256], func=AF.Copy, scale=ss[:, 2 + i:3 + i]), r=[pid, "kv_r%d" % i, "kv_j"], w=["kv_j"])
                    P.op("dve", lambda e: e.tensor_tensor(out=kn[i][:], in0=jk[:], in1=kg[:], op=ALU.mult), r=["kv_j", "kv_g"], w=["kv_n%d" % i])
                    P.dma(L["ckvn"].ap()[t * 128:(t + 1) * 128, :], kn[i][:], r=["kv_n%d" % i], w=["ckvn_d"], q="act")
                    for c in range(2):
                        P.op("pe", lambda e: e.transpose(out=pk[i][:, c * 128:(c + 1) * 128], in_=kn[i][:, c * 128:(c + 1) * 128], identity=self.ident[:]),
                             r=["kv_n%d" % i, "ident"], w=["kv_p%d" % i])
                    self.copy("dve", kt[i][:], pk[i][:], ["kv_p%d" % i], ["kv_t%d" % i])
                    P.dma(L["ckvnT"].ap()[:, t * 128:(t + 1) * 128].rearrange("(c p) t -> p c t", p=128), kt[i][:].rearrange("p (c t) -> p c t", t=128),
                          r=["kv_t%d" % i], w=["ckvnT_d"], q="act")
                if "k" in L1P: self.linear(hT, "hT1", 16, Win, 6176, 256, "tok", ksink, slab=256)
            with ExitStack() as s2:
                ki = [P.sb("ki_n%d" % i, [128, 128], BF16, s2) for i in range(2)]
                kit = [P.sb("ki_t%d" % i, [128, 128], BF16, s2) for i in range(2)]
                wi = [P.sb("ki_w%d" % i, [128, 16], F32, s2) for i in range(2)]
                pki = [P.ps("ki_p%d" % i, [128, 128], BF16, s2) for i in range(2)]
                for i in range(2):
                    P.op("pool", lambda e: e.memset(ki[i][:], 0.0), w=["ki_n%d" % i])

                def isink(t, coff, n, ps, pid):
                    i = t % 2
                    self.copy("act", ki[i][:, 0:64], ps[:, 0:64], [pid], ["ki_n%d" % i])
                    self.copy("dve", wi[i][:], ps[:, 64:80], [pid], ["ki_w%d" % i])
                    P.dma(L["wi"].ap()[t * 128:(t + 1) * 128, :], wi[i][:], r=["ki_w%d" % i], w=["wi_d"], q="act")
                    P.op("pe", lambda e: e.transpose(out=pki[i][:], in_=ki[i][:], identity=self.ident[:]), r=["ki_n%d" % i, "ident"], w=["ki_p%d" % i])
                    self.copy("dve", kit[i][:], pki[i][:], ["ki_p%d" % i], ["ki_t%d" % i])
                    P.dma(L["kiT"].ap()[:, t * 128:(t + 1) * 128], kit[i][0:64, :], r=["ki_t%d" % i], w=["kiT_d"], q="act")
                if "x" in L1P: self.linear(hT, "hT1", 16, Win, 7456, 80, "tok", isink, slab=80)
        P.barrier()


class MK4(MK3):
    def mamba_ssd(self, w, mixed_ap):
        P = self.P
        L = self.l1
        with ExitStack() as st:
            CT = P.sb("ss_CT", [128, 4, S_LEN], BF16, st)
            BT = P.sb("ss_BT", [128, 4, S_LEN], BF16, st)
            for g in range(4):
                P.dma(CT[:, g, :], L["CT"].ap()[g * 128:(g + 1) * 128, :], w=["ss_CT"])
                P.dma(BT[:, g, :], L["BT"].ap()[g * 128:(g + 1) * 128, :], w=["ss_BT"])
            Dbc = P.sb("ss_D", [128, 32], F32, st)
            P.dma(Dbc[:], w["cd_d_skip"].ap()[0:1, :].partition_broadcast(128), w=["ss_D"])
            gn = P.sb("ss_gn", [128, 2048], F32, st)
            P.dma(gn[:], w["cd_ssm_norm"].ap()[0:1, :].partition_broadcast(128), w=["ss_gn"])
            state = P.sb("ss_state", [128, 32, 64], F32, st)
            stbf = P.sb("ss_stbf", [128, 32, 64], BF16, st)
            xs = [P.sb("ss_xs%d" % i, [128, 32, 64], BF16, st) for i in range(2)]
            Bk = [P.sb("ss_Bk%d" % i, [128, 512], BF16, st) for i in range(2)]
            zt = [P.sb("ss_z%d" % i, [128, 2048], F32, st) for i in range(2)]
            dtt = [P.sb("ss_dt%d" % i, [128, 32], F32, st) for i in range(2)]
            dAt = [P.sb("ss_dA%d" % i, [128, 32], F32, st) for i in range(2)]
            cs = P.sb("ss_cs", [128, 32], F32, st)
            ncs = P.sb("ss_ncs", [128, 32], F32, st)
            ecs = P.sb("ss_ecs", [128, 32], F32, st)
            clb = P.sb("ss_clb", [128, 32], F32, st)
            dec = P.sb("ss_dec", [128, 32], F32, st)
            ela = P.sb("ss_ela", [128, 32], F32, st)
            xdt = P.sb("ss_xdt", [128, 32, 64], BF16, st)
            xw = P.sb("ss_xw", [128, 32, 64], BF16, st)
            Y = P.sb("ss_Y", [128, 32, 64], F32, st)
            Dg = [P.sb("ss_Dg%d" % i, [128, 128], F32, st) for i in range(2)]
            sg = [P.sb("ss_sg%d" % i, [128, 128], F32, st) for i in range(2)]
            Lt = [P.sb("ss_L%d" % i, [128, 128], F32, st) for i in range(2)]
            Gs = P.sb("ss_Gs", [128, 128], F32, st)
            Wt = [P.sb("ss_W%d" % i, [128, 128], BF16, st) for i in range(2)]
            y1 = [P.sb("ss_y1%d" % i, [128, 64], F32, st) for i in range(2)]
            ssq = P.sb("ss_ssq", [128, 8], F32, st)
            jk = P.sb("ss_jk", [128, 512], F32, st)
            yo = [P.sb("ss_yo%d" % i, [128, 2048], BF16, st) for i in range(2)]
            bk = [P.ps("ss_bk%d" % i, [128, 512], F32, st) for i in range(5)]
            pcs = bk[0][:, 0:64]
            pG = bk[0][:, 128:256]
            pcb = [bk[1][:, 0:128], bk[2][:, 0:128]]
            pyi = [bk[3][:, 0:64], bk[4][:, 0:64]]
            pys = [bk[3][:, 128:192], bk[4][:, 128:192]]
            pdS = P.ps("ss_pdS", [128, 8, 64], F32, st)
            P.op("pool", lambda e: e.memset(state[:], 0.0), w=["ss_state"])
            P.op("pool", lambda e: e.memset(stbf[:], 0.0), w=["ss_stbf"])
            it = 0
            for ck in range(NT):
                b = ck % 2
                rows = slice(ck * 128, (ck + 1) * 128)
                P.dma(xs[b][:].rearrange("p h d -> p (h d)"), L["xs"].ap()[rows, :], w=["ss_xs%d" % b])
                P.dma(Bk[b][:], L["Btok"].ap()[rows, :], w=["ss_Bk%d" % b])
                P.dma(zt[b][:], L["zs"].ap()[rows, :], w=["ss_z%d" % b])
                P.dma(dtt[b][:], L["dt"].ap()[rows, :], w=["ss_dt%d" % b])
                P.dma(dAt[b][:], L["dA"].ap()[rows, :], w=["ss_dA%d" % b])
                P.op("pe", lambda e: e.matmul(pcs[:, 0:32], lhsT=self.tri[:], rhs=dAt[b][:], start=True, stop=True, skip_group_check=True), r=["c_tri", "ss_dA%d" % b], w=["ss_bk0"])
                self.copy("dve", cs[:], pcs[:, 0:32], ["ss_bk0"], ["ss_cs"])
                P.op("pe", lambda e: e.matmul(pcs[:, 32:64], lhsT=self.sel127[:], rhs=cs[:], start=True, stop=True, skip_group_check=True),
                     r=["c_sel127", "ss_cs"], w=["ss_bk0"])
                self.copy("dve", clb[:], pcs[:, 32:64], ["ss_bk0"], ["ss_clb"])
                P.op("dve", lambda e: e.tensor_scalar(out=ncs[:], in0=cs[:], scalar1=-1.0, scalar2=None, op0=ALU.mult), r=["ss_cs"], w=["ss_ncs"])
                P.op("act", lambda e: e.activation(out=ecs[:], in_=cs[:], func=AF.Exp), r=["ss_cs"], w=["ss_ecs"])
                P.op("dve", lambda e: e.tensor_tensor(out=dec[:], in0=clb[:], in1=cs[:], op=ALU.subtract), r=["ss_clb", "ss_cs"], w=["ss_dec"])
                P.op("act", lambda e: e.activation(out=dec[:], in_=dec[:], func=AF.Exp), r=["ss_dec"], w=["ss_dec"])
                P.op("act", lambda e: e.activation(out=ela[:], in_=clb[:], func=AF.Exp), r=["ss_clb"], w=["ss_ela"])
                P.op("dve", lambda e: e.tensor_tensor(out=xdt[:], in0=xs[b][:], in1=dtt[b][:].unsqueeze(2).to_broadcast([128, 32, 64]), op=ALU.mult),
                     r=["ss_xs%d" % b, "ss_dt%d" % b], w=["ss_xdt"])
                P.op("pool", lambda e: e.tensor_tensor(out=xw[:], in0=xdt[:], in1=dec[:].unsqueeze(2).to_broadcast([128, 32, 64]), op=ALU.mult),
                     r=["ss_xdt", "ss_dec"], w=["ss_xw"])
                for g in range(4):
                    P.op("pe", lambda e: e.matmul(pG, lhsT=BT[:, g, ck * 128:(ck + 1) * 128], rhs=CT[:, g, ck * 128:(ck + 1) * 128], start=True, stop=True, skip_group_check=True),
                         r=["ss_BT", "ss_CT"], w=["ss_bk0"])
                    self.copy("act", Gs[:], pG, ["ss_bk0"], ["ss_Gs"])
                    for hh in range(8):
                        h = g * 8 + hh
                        i = it % 2
                        it += 1
                        P.op("dve", lambda e: e.tensor_scalar(out=Dg[i][:], in0=self.identf[:], scalar1=cs[:, h:h + 1], scalar2=None, op0=ALU.mult),
                             r=["c_ident", "ss_cs"], w=["ss_Dg%d" % i])
                        P.op("pe", lambda e: e.matmul(pcb[i], lhsT=self.onesf[:], rhs=Dg[i][:], start=True, stop=True, skip_group_check=True), r=["c_onesf", "ss_Dg%d" % i], w=["ss_bkb%d" % i])
                        P.op("dve", lambda e: e.tensor_tensor(out=sg[i][:], in0=pcb[i], in1=self.negmT[:], op=ALU.add), r=["ss_bkb%d" % i, "c_negmT"], w=["ss_sg%d" % i])
                        P.op("act", lambda e: e.activation(out=Lt[i][:], in_=sg[i][:], func=AF.Exp, bias=ncs[:, h:h + 1]), r=["ss_sg%d" % i, "ss_ncs"], w=["ss_L%d" % i])
                        P.op("pool", lambda e: e.tensor_tensor(out=Wt[i][:], in0=Gs[:], in1=Lt[i][:], op=ALU.mult), r=["ss_Gs", "ss_L%d" % i], w=["ss_W%d" % i])
                        P.op("pe", lambda e: e.matmul(pyi[i], lhsT=Wt[i][:], rhs=xdt[:, h, :], start=True, stop=True, skip_group_check=True), r=["ss_W%d" % i, "ss_xdt"], w=["ss_bky%d" % i])
                        if ck > 0:
                            P.op("pe", lambda e: e.matmul(pys[i], lhsT=CT[:, g, ck * 128:(ck + 1) * 128], rhs=stbf[:, h, :], start=True, stop=True, skip_group_check=True),
                                 r=["ss_CT", ("ss_stbf", h)], w=["ss_bky%d" % i])
                            P.op("dve", lambda e: e.tensor_scalar(out=y1[i][:], in0=pys[i], scalar1=ecs[:, h:h + 1], scalar2=None, op0=ALU.mult),
                                 r=["ss_bky%d" % i, "ss_ecs"], w=["ss_y1%d" % i])
                            P.op("dve", lambda e: e.tensor_tensor(out=Y[:, h, :], in0=pyi[i], in1=y1[i][:], op=ALU.add),
                                 r=["ss_bky%d" % i, "ss_y1%d" % i], w=[("ss_Y", h)])
                        else:
                            self.copy("dve", Y[:, h, :], pyi[i], ["ss_bky%d" % i], [("ss_Y", h)])
                    for hh in range(8):
                        h = g * 8 + hh
                        P.op("pe", lambda e: e.matmul(pdS[:, hh, :], lhsT=Bk[b][:, g * 128:(g + 1) * 128], rhs=xw[:, h, :], start=True, stop=True,
                                                      skip_group_check=True), r=["ss_Bk%d" % b, "ss_xw"], w=["ss_pdS"])
                    for hh in range(8):
                        h = g * 8 + hh
                        P.op("dve", lambda e: e.scalar_tensor_tensor(out=state[:, h, :], in0=state[:, h, :], scalar=ela[:, h:h + 1], in1=pdS[:, hh, :],
                                                                     op0=ALU.mult, op1=ALU.add), r=["ss_pdS", "ss_ela", ("ss_state", h), "ss_state"], w=[("ss_state", h)])
                        self.copy("act", stbf[:, h, :], state[:, h, :], [("ss_state", h)], [("ss_stbf", h)])
                Yall = [("ss_Y", h) for h in range(32)]
                P.op("pool", lambda e: e.tensor_tensor(out=xw[:], in0=xs[b][:], in1=Dbc[:].unsqueeze(2).to_broadcast([128, 32, 64]), op=ALU.mult),
                     r=["ss_xs%d" % b, "ss_D", "ss_xw"], w=["ss_xw"])
                P.op("dve", lambda e: e.tensor_tensor(out=Y[:], in0=Y[:], in1=xw[:], op=ALU.add), r=Yall + ["ss_xw"], w=Yall)
                Yf = Y[:].rearrange("p h d -> p (h d)")
                P.op("dve", lambda e: e.tensor_tensor(out=Yf, in0=Yf, in1=zt[b][:], op=ALU.mult), r=Yall + ["ss_z%d" % b], w=Yall)
                for g in range(4):
                    P.op("act", lambda e: e.activation(out=jk[:], in_=Yf[:, g * 512:(g + 1) * 512], func=AF.Square, accum_out=ssq[:, g:g + 1]),
                         r=Yall, w=["ss_jk", ("ss_ssq", g)])
                    self.rstd_from_ss(ssq[:, 4 + g:5 + g], ssq[:, g:g + 1], 512, ("ss_rs", g), ("ss_ssq", g))
                    P.op("dve", lambda e: e.scalar_tensor_tensor(out=yo[b][:, g * 512:(g + 1) * 512], in0=Yf[:, g * 512:(g + 1) * 512], scalar=ssq[:, 4 + g:5 + g],
                                                                 in1=gn[:, g * 512:(g + 1) * 512], op0=ALU.mult, op1=ALU.mult),
                         r=Yall + [("ss_rs", g), "ss_gn"], w=[("ss_yo%d" % b, g)])
                P.dma(mixed_ap[rows, 0:2048], yo[b][:], r=[("ss_yo%d" % b, g) for g in range(4)], w=["ss_out"], q="act")
        P.barrier()


class MK5(MK4):
    def dsa_select(self):
        P = self.P
        L = self.l1
        MT_d = self.scratch("dsa_MT", [S_LEN, S_LEN], BF16)
        self.MT_d = MT_d
        NEG = -1.0e30
        with ExitStack() as st:
            qi = P.sb("dx_qi", [128, 8, S_LEN], BF16, st)
            ki = P.sb("dx_ki", [128, S_LEN], BF16, st)
            wi = P.sb("dx_wi", [128, NT, 16], F32, st)
            P.dma(qi[:], L["qiT"].ap().rearrange("(c p) t -> p c t", p=128), w=["dx_qi"])
            P.dma(ki[0:64, :], L["kiT"].ap(), w=["dx_ki"])
            P.dma(ki[64:128, :], L["kiT"].ap(), w=["dx_ki"])
            P.dma(wi[:], L["wi"].ap().rearrange("(a p) h -> p a h", p=128), w=["dx_wi"])
            acc = P.sb("dx_acc", [128, S_LEN], F32, st)
            tmp = [P.sb("dx_tmp%d" % i, [128, 512], F32, st) for i in range(2)]
            wk = [P.sb("dx_wk%d" % i, [128, S_LEN], F32, st) for i in range(2)]
            m8 = P.sb("dx_m8", [128, 8], F32, st)
            Mb = P.sb("dx_M", [128, S_LEN], BF16, st)
            mts = [P.sb("dx_mt%d" % i, [128, 512], BF16, st) for i in range(2)]
            pss = [P.ps("dx_ps%d" % i, [128, 512], F32, st) for i in range(3)]
            ptm = [P.ps("dx_pt%d" % i, [128, 512], BF16, st) for i in range(2)]
            ip = 0
            itm = 0
            for i in range(NT):
                W = (i + 1) * 128
                nsg = (W + 511) // 512
                for h in range(16):
                    pb = 64 * (h % 2)
                    for sg in range(nsg):
                        n = min(512, W - sg * 512)
                        ps = pss[ip % 3]
                        pid = "dx_ps%d" % (ip % 3)
                        ip += 1
                        P.op("pe", lambda e: e.matmul(ps[:, 0:n], lhsT=qi[pb:pb + 64, h // 2, i * 128:(i + 1) * 128], rhs=ki[pb:pb + 64, sg * 512:sg * 512 + n],
                                                      start=True, stop=True), r=["dx_qi", "dx_ki"], w=[pid])
                        aid = ("dx_acc", sg)
                        if h == 0:
                            P.op("dve", lambda e: e.tensor_scalar(out=acc[:, sg * 512:sg * 512 + n], in0=ps[:, 0:n], scalar1=0.0, scalar2=wi[:, i, h:h + 1],
                                                                  op0=ALU.max, op1=ALU.mult), r=[pid, "dx_wi"], w=[aid])
                        else:
                            tb = tmp[itm % 2]
                            tid = "dx_tmp%d" % (itm % 2)
                            itm += 1
                            P.op("dve", lambda e: e.tensor_scalar(out=tb[:, 0:n], in0=ps[:, 0:n], scalar1=0.0, scalar2=wi[:, i, h:h + 1],
                                                                  op0=ALU.max, op1=ALU.mult), r=[pid, "dx_wi"], w=[tid])
                            P.op("pool", lambda e: e.tensor_tensor(out=acc[:, sg * 512:sg * 512 + n], in0=acc[:, sg * 512:sg * 512 + n], in1=tb[:, 0:n], op=ALU.add),
                                 r=[tid, aid], w=[aid])
                aall = [("dx_acc", sg) for sg in range(4)]
                P.op("pool", lambda e: e.tensor_tensor(out=acc[:, i * 128:W], in0=acc[:, i * 128:W], in1=self.negm[:], op=ALU.add), r=aall + ["c_negm"], w=aall)
                if W > 256:
                    cur = acc
                    cid = aall
                    for rd in range(32):
                        P.op("dve", lambda e: e.max(out=m8[:], in_=cur[:, 0:W]), r=cid, w=["dx_m8"])
                        if rd < 31:
                            nxt = wk[rd % 2]
                            nid = ["dx_wk%d" % (rd % 2)]
                            P.op("dve", lambda e: e.match_replace(out=nxt[:, 0:W], in_to_replace=m8[:], in_values=cur[:, 0:W], imm_value=NEG),
                                 r=cid + ["dx_m8"], w=nid)
                            cur, cid = nxt, nid
                    P.op("dve", lambda e: e.tensor_scalar(out=Mb[:, 0:W], in0=acc[:, 0:W], scalar1=m8[:, 7:8], scalar2=None, op0=ALU.is_ge),
                         r=aall + ["dx_m8"], w=["dx_M"])
                    P.op("pool", lambda e: e.tensor_tensor(out=Mb[:, i * 128:W], in0=Mb[:, i * 128:W], in1=self.low01[:], op=ALU.mult), r=["dx_M", "c_low01"], w=["dx_M"])
                else:
                    if i > 0:
                        P.op("pool", lambda e: e.memset(Mb[:, 0:i * 128], 1.0), r=aall, w=["dx_M"])
                    self.copy("pool", Mb[:, i * 128:W], self.low01[:], ["c_low01"] + aall, ["dx_M"])
                for j4 in range((i + 4) // 4):
                    nj = min(4, i + 1 - j4 * 4)
                    pt = ptm[j4 % 2]
                    ptid = "dx_pt%d" % (j4 % 2)
                    for jj in range(nj):
                        j = j4 * 4 + jj
                        P.op("pe", lambda e: e.transpose(out=pt[:, jj * 128:(jj + 1) * 128], in_=Mb[:, j * 128:(j + 1) * 128], identity=self.ident[:]),
                             r=["dx_M", "ident"], w=[ptid])
                    ms = mts[j4 % 2]
                    msid = "dx_mt%d" % (j4 % 2)
                    self.copy(self.alt("act", "dve"), ms[:, 0:nj * 128], pt[:, 0:nj * 128], [ptid], [msid])
                    P.dma(MT_d.ap()[j4 * 512:j4 * 512 + nj * 128, i * 128:(i + 1) * 128].rearrange("(a p) c -> p a c", p=128),
                          ms[:, 0:nj * 128].rearrange("p (a c) -> p a c", c=128), r=[msid], w=["dx_MT_d"], q="act")
        P.barrier()

    def dsa_attend(self, w, mixed_ap):
        P = self.P
        L = self.l1
        MT_d = self.MT_d
        SC = 128.0 ** -0.5
        with ExitStack() as st:
            wk32 = P.sb("da_wk32", [128, 8, 256], F32, st)
            wv32 = P.sb("da_wv32", [128, 8, 2, 128], F32, st)
            wuk = P.sb("da_wuk", [128, 8, 256], BF16, st)
            wuv = P.sb("da_wuv", [128, 8, 2, 128], BF16, st)
            P.dma(wk32[:], w["cd_w_uk"].ap()[0].rearrange("h d c -> d h c"), w=["da_wk32"])
            for h in range(8):
                P.dma(wv32[:, h, :, :], w["cd_w_uv"].ap()[0, h].rearrange("(cc p) d -> p cc d", p=128), w=["da_wv32"])
            self.copy("dve", wuk[:], wk32[:], ["da_wk32"], ["da_wuk"])
            self.copy("pool", wuv[:], wv32[:], ["da_wv32"], ["da_wuv"])
            KT = P.sb("da_KT", [128, 2, S_LEN], BF16, st)
            P.dma(KT[:], L["ckvnT"].ap().rearrange("(cc p) t -> p cc t", p=128), w=["da_KT"])
            Vt = P.sb("da_V", [128, NT, 257], BF16, st)
            P.op("pool", lambda e: e.memset(Vt[:, :, 256:257], 1.0), w=["da_V"])
            P.dma(Vt[:, :, 0:256], L["ckvn"].ap().rearrange("(j p) c -> p j c", p=128), w=["da_V"])
            qTh = [P.sb("da_q%d" % i, [128, S_LEN], BF16, st) for i in range(2)]
            QA = [P.sb("da_QA%d" % i, [128, 2, S_LEN], BF16, st) for i in range(2)]
            PTs = [P.sb("da_pt%d" % i, [128, 512], BF16, st) for i in range(3)]
            exn = [P.sb("da_exn%d" % i, [128, 256], F32, st) for i in range(2)]
            exf = [P.sb("da_exf%d" % i, [128, 512], BF16, st) for i in range(2)]
            MTb = [P.sb("da_mt%d" % i, [128, 512], BF16, st) for i in range(3)]
            sm = P.sb("da_sm", [128, 4], F32, st)
            ctx = [P.sb("da_ctx%d" % i, [128, 256], BF16, st) for i in range(2)]
            ctT = [P.sb("da_ctT%d" % i, [128, 256], BF16, st) for i in range(2)]
            og = [P.sb("da_og%d" % i, [128, 128], BF16, st) for i in range(2)]
            pS = [P.ps("da_ps%d" % i, [128, 512], F32, st) for i in range(2)]
            pO = P.ps("da_po", [128, 4, 512], F32, st)
            pM = P.ps("da_pm", [128, 512], F32, st)
            pT = P.ps("da_pT", [128, 1024], BF16, st)
            ipt = 0
            isx = 0
            imt = 0
            ifin = 0
            for h in range(8):
                hb = h % 2
                P.dma(qTh[hb][:], L["qT"].ap()[h * 128:(h + 1) * 128, :], w=["da_q%d" % hb])
                for cc in range(2):
                    for tg in range(4):
                        P.op("pe", lambda e: e.matmul(pM[:, :], lhsT=wuk[:, h, cc * 128:(cc + 1) * 128], rhs=qTh[hb][:, tg * 512:(tg + 1) * 512], start=True, stop=True),
                             r=["da_wuk", "da_q%d" % hb], w=["da_pm"])
                        self.copy(self.alt("act", "dve"), QA[hb][:, cc, tg * 512:(tg + 1) * 512], pM[:, :], ["da_pm"], ["da_QA%d" % hb])
                for g in range(4):
                    for j in range(4 * g + 4):
                        c0 = max(0, j - 4 * g)
                        ps = pS[isx % 2]
                        psid = "da_ps%d" % (isx % 2)
                        isx += 1
                        for cc in range(2):
                            P.op("pe", lambda e: e.matmul(ps[:, c0 * 128:512], lhsT=KT[:, cc, j * 128:(j + 1) * 128],
                                                          rhs=QA[hb][:, cc, g * 512 + c0 * 128:(g + 1) * 512], start=(cc == 0), stop=(cc == 1)),
                                 r=["da_KT", "da_QA%d" % hb], w=[psid])
                        mt = MTb[imt % 3]
                        mtid = "da_mt%d" % (imt % 3)
                        imt += 1
                        P.dma(mt[:, c0 * 128:512], MT_d.ap()[j * 128:(j + 1) * 128, g * 512 + c0 * 128:(g + 1) * 512], w=[mtid])
                        pt = PTs[ipt % 3]
                        ptid = "da_pt%d" % (ipt % 3)
                        ipt += 1
                        near_lo = c0 * 128
                        if j >= 4 * g:
                            near_hi = min(512, near_lo + 256)
                            eb_lo = 0
                        else:
                            near_hi = 128 if j == 4 * g - 1 else 0
                            eb_lo = 128
                        nw = near_hi - near_lo if near_hi > near_lo else 0
                        if nw > 0:
                            exb = exn[ipt % 2]
                            exid = "da_exn%d" % (ipt % 2)
                            P.op("act", lambda e: e.activation(out=exb[:, 0:nw], in_=ps[:, near_lo:near_hi], func=AF.Exp, scale=SC), r=[psid], w=[exid])
                            P.op("dve", lambda e: e.tensor_tensor(out=exb[:, 0:nw], in0=exb[:, 0:nw], in1=self.EB[:, h, eb_lo:eb_lo + nw], op=ALU.mult),
                                 r=[exid, ("EB", h)], w=[exid])
                            P.op("pool", lambda e: e.tensor_tensor(out=pt[:, near_lo:near_hi], in0=exb[:, 0:nw], in1=mt[:, near_lo:near_hi], op=ALU.mult),
                                 r=[exid, mtid], w=[(ptid, 0)])
                        far_lo = max(near_hi, near_lo)
                        if far_lo < 512:
                            efb = exf[ipt % 2]
                            efid = "da_exf%d" % (ipt % 2)
                            P.op("act", lambda e: e.activation(out=efb[:, far_lo:512], in_=ps[:, far_lo:512], func=AF.Exp,
                                                               bias=self.tbl_bc[:, 31 * 8 + h:31 * 8 + h + 1], scale=SC), r=[psid, "tbl_bc"], w=[efid])
                            P.op(self.alt("dve", "pool"), lambda e: e.tensor_tensor(out=pt[:, far_lo:512], in0=efb[:, far_lo:512], in1=mt[:, far_lo:512], op=ALU.mult),
                                 r=[efid, mtid], w=[(ptid, 1)])
                        for il in range(c0, 4):
                            P.op("pe", lambda e: e.matmul(pO[:, il, 0:257], lhsT=pt[:, il * 128:(il + 1) * 128], rhs=Vt[:, j, :],
                                                          start=(j == 0), stop=(j == 4 * g + il)), r=[(ptid, 0), (ptid, 1), "da_V"], w=[("da_po", il)])
                    for il in range(4):
                        qb = 4 * g + il
                        f = ifin % 2
                        ifin += 1
                        P.op("dve", lambda e: e.reciprocal(out=sm[:, f:f + 1], in_=pO[:, il, 256:257]), r=[("da_po", il)], w=[("da_sm", f)])
                        P.op("act", lambda e: e.activation(out=ctx[f][:], in_=pO[:, il, 0:256], func=AF.Copy, scale=sm[:, f:f + 1]),
                             r=[("da_po", il), ("da_sm", f)], w=["da_ctx%d" % f])
                        for cc in range(2):
                            P.op("pe", lambda e: e.transpose(out=pT[:, (f * 2 + cc) * 128:(f * 2 + cc + 1) * 128], in_=ctx[f][:, cc * 128:(cc + 1) * 128],
                                                             identity=self.ident[:]), r=["da_ctx%d" % f, "ident"], w=["da_pT"])
                        self.copy("dve", ctT[f][:], pT[:, f * 256:(f + 1) * 256], ["da_pT"], ["da_ctT%d" % f])
                        for cc in range(2):
                            P.op("pe", lambda e: e.matmul(pM[:, 0:128], lhsT=ctT[f][:, cc * 128:(cc + 1) * 128], rhs=wuv[:, h, cc, :], start=(cc == 0), stop=(cc == 1)),
                                 r=["da_ctT%d" % f, "da_wuv"], w=["da_pm"])
                        self.copy("act", og[f][:], pM[:, 0:128], ["da_pm"], ["da_og%d" % f])
                        P.dma(mixed_ap[qb * 128:(qb + 1) * 128, 2048 + h * 128:2048 + (h + 1) * 128], og[f][:], r=["da_og%d" % f], w=["da_out"], q="act")
        P.barrier()


class MK6(MK5):
    def final_norm(self, h_ap, gain_ap, out_ap):
        P = self.P
        with ExitStack() as st:
            gb = P.sb("fn_g", [128, DM], F32, st)
            P.dma(gb[:], gain_ap.partition_broadcast(128), w=["fn_g"])
            xt = [P.sb("fn_x%d" % i, [128, DM], F32, st) for i in range(2)]
            jk = P.sb("fn_j", [128, DM], BF16, st)
            ss = P.sb("fn_s", [128, 4], F32, st)
            for t in range(NT):
                b = t % 2
                P.dma(xt[b][:], h_ap[t * 128:(t + 1) * 128, :], w=["fn_x%d" % b])
                P.op("act", lambda e: e.activation(out=jk[:], in_=xt[b][:], func=AF.Square, accum_out=ss[:, b:b + 1]), r=["fn_x%d" % b], w=["fn_j", "fn_s%d" % b])
                self.rstd_from_ss(ss[:, 2 + b:3 + b], ss[:, b:b + 1], DM, "fn_r%d" % b, "fn_s%d" % b)
                P.op("dve", lambda e: e.scalar_tensor_tensor(out=xt[b][:], in0=xt[b][:], scalar=ss[:, 2 + b:3 + b], in1=gb[:], op0=ALU.mult, op1=ALU.mult),
                     r=["fn_x%d" % b, "fn_r%d" % b, "fn_g"], w=["fn_x%d" % b])
                P.dma(out_ap[t * 128:(t + 1) * 128, :], xt[b][:], r=["fn_x%d" % b], w=["fn_out"], q="act")
        P.barrier()


W_SHAPES = {
    "rel_table": [32, 8], "ab_w_in": [1, 2048, 6144], "ab_w_out": [1, 2048, 2048], "ab_lambda": [1, 4, 64],
    "ab_a_norm": [1, 128], "ab_b_norm": [1, 128], "cd_w_in": [1, 2048, 7536], "cd_w_out": [1, 3072, 2048],
    "cd_conv_w": [1, 4, 3072], "cd_conv_b": [1, 3072], "cd_dt_bias": [1, 32], "cd_a_log": [1, 32], "cd_d_skip": [1, 32],
    "cd_ssm_norm": [1, 2048], "cd_kv_norm": [1, 256], "cd_w_uk": [1, 8, 128, 256], "cd_w_uv": [1, 8, 256, 128],
    "norm_mix": [2, 2048], "norm_ffn": [2, 2048], "norm_final": [1, 2048],
    "peer_w_q0": [2048, 2048], "peer_w_q1": [2048, 2048], "peer_keys0": [8, 2, 128, 128], "peer_keys1": [8, 2, 128, 128],
    "peer_u0": [16384, 2048], "peer_u1": [16384, 2048], "peer_v0": [16384, 2048], "peer_v1": [16384, 2048],
}


def build_full(dbg=(), upto=99):
    mk = MK6(dbg=dbg)
    P = mk.P
    w = {}
    for k, shp in W_SHAPES.items():
        w[k] = mk.inp(k, shp)
    consts = host_consts()
    consts.update(host_consts1())
    for k, v in consts.items():
        w[k] = mk.inp(k, list(v.shape))
    x = mk.inp("x", [S_LEN, DM])
    out = P.dram("out", [S_LEN, DM], F32, kind="ExternalOutput")
    hA = mk.scratch("hA", [S_LEN, DM], F32)
    hB = mk.scratch("hB", [S_LEN, DM], F32)
    hC = mk.scratch("hC", [S_LEN, DM], F32)
    hD = mk.scratch("hD", [S_LEN, DM], F32)
    mixed1 = mk.scratch("mixed1", [S_LEN, 3072], BF16)
    mk.setup_consts(w["c_ident"])
    mk.build_bias_tables(w["rel_table"], w["c_boh"], w["c_bmask"])
    mk.lam_scalar(w["ab_lambda"])
    mk.l1_consts(w)
    mk.layer0_mixer(x.ap(), hA.ap(), w)
    mk.peer(hA.ap(), hB.ap(), w["norm_ffn"].ap()[0:1, :], w["peer_w_q0"].ap(), w["peer_keys0"].ap(), w["peer_u0"].ap(), w["peer_v0"].ap(), 0)
    mk.layer1_proj(hB.ap(), w)
    mk.mamba_ssd(w, mixed1.ap())
    mk.dsa_select()
    mk.dsa_attend(w, mixed1.ap())
    mk.out_proj(mixed1.ap(), 3072, w["cd_w_out"].ap()[0], hB.ap(), hC.ap(), BF16)
    mk.peer(hC.ap(), hD.ap(), w["norm_ffn"].ap()[1:2, :], w["peer_w_q1"].ap(), w["peer_keys1"].ap(), w["peer_u1"].ap(), w["peer_v1"].ap(), 1)
    mk.final_norm(hD.ap(), w["norm_final"].ap(), out.ap())
    P.finish()
    return mk, consts


def make_in_maps(inputs, consts, n_cores=8):
    shared = {}
    for k, shp in W_SHAPES.items():
        if k.startswith("peer_"):
            base, l = k[:-1], int(k[-1])
            shared[k] = np.ascontiguousarray(np.asarray(inputs[base])[l])
        else:
            shared[k] = np.ascontiguousarray(np.asarray(inputs[k], dtype=np.float32).reshape(shp))
    shared.update(consts)
    xs = np.asarray(inputs["x"], dtype=np.float32)
    maps = []
    for c in range(n_cores):
        m = dict(shared)
        m["x"] = np.ascontiguousarray(xs[c])
        maps.append(m)
    return maps


_CACHE = {}


def kernel(**inputs):
    from concourse.bass_utils import run_bass_kernel_spmd
    if "prog" not in _CACHE:
        _CACHE["prog"] = build_full()
    mk, consts = _CACHE["prog"]
    maps = make_in_maps(inputs, consts, 8)
    res = run_bass_kernel_spmd(mk.P.nc, maps, core_ids=list(range(8)))
    return np.stack([np.asarray(r["out"], dtype=np.float32) for r in res.results], axis=0)
```

```python
from contextlib import ExitStack
import os
import numpy as np
import concourse.bass as bass
import concourse.mybir as mybir

F32 = mybir.dt.float32
BF16 = mybir.dt.bfloat16
I32 = mybir.dt.int32
AF = mybir.ActivationFunctionType
ALU = mybir.AluOpType
AX = mybir.AxisListType

NDMA = 24
COMPUTE = ("pe", "act", "dve", "pool")


class Tok:
    __slots__ = ("key", "val", "clock")

    def __init__(self, key, val, clock):
        self.key, self.val, self.clock = key, val, clock


class Prog:
    def __init__(self):
        self.nc = bass.Bass("TRN2", target_bir_lowering=False)
        nc = self.nc
        self.es = ExitStack()
        self.engs = {"pe": nc.tensor, "act": nc.scalar, "dve": nc.vector,
                     "pool": nc.gpsimd, "sp": nc.sync}
        self.sem = {e: self.es.enter_context(nc.semaphore("s_" + e)) for e in COMPUTE}
        self.cnt = {e: 0 for e in COMPUTE}
        self.dsem = [self.es.enter_context(nc.semaphore("d%d" % i)) for i in range(NDMA)]
        self.dval = [0] * NDMA
        self.dtok = [None] * NDMA
        self.dn = 0
        self.known = {e: {} for e in self.engs}
        self.snap = {e: None for e in self.engs}
        self.last_w = {}
        self.readers = {}
        self.nwaits = 0
        self.ninst = 0
        self.psum_names = set()
        self.dram_in = {}

    def sb(self, name, shape, dt, stack=None):
        self.uid = getattr(self, "uid", 0) + 1
        return (stack or self.es).enter_context(self.nc.sbuf_tensor("%s_u%d" % (name, self.uid), list(shape), dt))

    def ps(self, name, shape, dt=F32, stack=None):
        self.uid = getattr(self, "uid", 0) + 1
        self.psum_names.add(name)
        return (stack or self.es).enter_context(self.nc.psum_tensor("%s_u%d" % (name, self.uid), list(shape), dt))

    def dram(self, name, shape, dt, kind="Internal"):
        return self.nc.dram_tensor(name, list(shape), dt, kind=kind)

    def _snapshot(self, eng):
        s = self.snap[eng]
        if s is None:
            s = dict(self.known[eng])
            self.snap[eng] = s
        return s

    def _semof(self, key):
        return self.sem[key] if isinstance(key, str) else self.dsem[key[1]]

    def _wait(self, eng, toks):
        kn = self.known[eng]
        best = {}
        for t in toks:
            if t is None:
                continue
            if eng == "pe" and t.key == "pe":
                continue
            if kn.get(t.key, 0) >= t.val:
                continue
            if best.get(t.key, (0, None))[0] < t.val:
                best[t.key] = (t.val, t)
        for key, (val, t) in best.items():
            if kn.get(key, 0) >= val:
                continue
            self.engs[eng].wait_ge(self._semof(key), val)
            self.nwaits += 1
            for k2, v2 in t.clock.items():
                if kn.get(k2, 0) < v2:
                    kn[k2] = v2
            kn[key] = val
            self.snap[eng] = None

    def _is_psum(self, b):
        n = b if isinstance(b, str) else b[0]
        return isinstance(n, str) and (n in self.psum_names or n.startswith("ss_bk"))

    def _deps(self, r, w, eng=None):
        need = []
        for b in r:
            t = self.last_w.get(b)
            if t is not None:
                need.append(t)
            if self._is_psum(b):
                need.extend(t2 for t2 in self.readers.get(b, ()) if t2.key != eng)
        for b in w:
            t = self.last_w.get(b)
            if t is not None:
                need.append(t)
            need.extend(self.readers.get(b, ()))
        return need

    def _record(self, tok, r, w):
        for b in w:
            self.last_w[b] = tok
            self.readers[b] = []
        for b in r:
            if b in w:
                continue
            self.readers.setdefault(b, []).append(tok)

    def op(self, eng, fn, r=(), w=()):
        self._wait(eng, self._deps(r, w, eng))
        ins = fn(self.engs[eng])
        self.cnt[eng] += 1
        ins.then_inc(self.sem[eng], 1)
        self.ninst += 1
        tok = Tok(eng, self.cnt[eng], self._snapshot(eng))
        self._record(tok, r, w)
        return tok

    def dma(self, out, in_, r=(), w=(), q="sp", **kw):
        i = self.dn % NDMA
        self.dn += 1
        need = self._deps(r, w)
        need.append(self.dtok[i])
        self._wait(q, need)
        self.dval[i] += 16
        self.engs[q].dma_start(out=out, in_=in_, **kw).then_inc(self.dsem[i], 16)
        self.ninst += 1
        tok = Tok(("d", i), self.dval[i], self._snapshot(q))
        self.dtok[i] = tok
        self._record(tok, r, w)
        return tok

    def barrier(self):
        toks = []
        for e in COMPUTE:
            if self.cnt[e]:
                toks.append(Tok(e, self.cnt[e], {}))
        toks += [t for t in self.dtok if t is not None]
        for e in self.engs:
            self._wait(e, toks)
        self.last_w.clear()
        self.readers.clear()

    def finish(self):
        toks = [t for t in self.dtok if t is not None]
        for e in COMPUTE:
            if self.cnt[e]:
                toks.append(Tok(e, self.cnt[e], {}))
        self._wait("sp", toks)


S_LEN = 2048
DM = 2048
NT = 16
EPS = 1e-6
LAM_INIT0 = 0.8 - 0.6 * float(np.exp(-0.3 * 0))


class MK:
    def __init__(self, dbg=()):
        self.P = Prog()
        self.dbg = set(dbg)
        self.inputs = {}
        self.rr = 0
        P = self.P
        self.ident = P.sb("ident", [128, 128], BF16)
        self.eps_t = P.sb("eps_t", [128, 1], F32)

    def inp(self, name, shape, dt=F32):
        t = self.P.dram(name, shape, dt, kind="ExternalInput")
        self.inputs[name] = t
        return t

    def scratch(self, name, shape, dt):
        kind = "ExternalOutput" if name in self.dbg else "Internal"
        return self.P.dram(name, shape, dt, kind=kind)

    def alt(self, *engs):
        self.rr += 1
        return engs[self.rr % len(engs)]

    def copy(self, eng, out, in_, r, w):
        if eng == "act":
            return self.P.op("act", lambda e: e.activation(out=out, in_=in_, func=AF.Copy), r=r, w=w)
        return self.P.op(eng, lambda e: e.tensor_copy(out=out, in_=in_), r=r, w=w)

    def setup_consts(self, c_ident):
        P = self.P
        with ExitStack() as st:
            idf = P.sb("idf", [128, 128], F32, st)
            P.dma(idf[:], c_ident.ap(), w=["idf"])
            self.copy("dve", self.ident[:], idf[:], ["idf"], ["ident"])
            P.op("pool", lambda e: e.memset(self.eps_t[:], EPS), w=["eps_t"])
            P.barrier()

    def rstd_from_ss(self, rstd, ss, n, rid, sid):
        P = self.P
        P.op("act", lambda e: e.activation(out=rstd, in_=ss, func=AF.Sqrt, bias=self.eps_t[:, 0:1], scale=1.0 / n),
             r=[sid, "eps_t"], w=[rid])
        P.op("dve", lambda e: e.reciprocal(out=rstd, in_=rstd), r=[rid], w=[rid])

    def loadT(self, src, K, dstT, dst_id, gain=None, src_dt=F32):
        P = self.P
        KC = K // 128
        with ExitStack() as st:
            xts = [P.sb("lt_x%d" % i, [128, K], src_dt, st) for i in range(2)]
            need_cast = (gain is not None) or (src_dt != BF16)
            xns = [P.sb("lt_n%d" % i, [128, K], BF16, st) for i in range(2)] if need_cast else None
            pts = [P.ps("lt_p%d" % i, [128, 512], BF16, st) for i in range(2)]
            if gain is not None:
                gb = P.sb("lt_g", [128, K], F32, st)
                junk = P.sb("lt_j", [128, K], BF16, st)
                ss = P.sb("lt_ss", [128, 2], F32, st)
                rs = P.sb("lt_rs", [128, 2], F32, st)
                P.dma(gb[:], gain.partition_broadcast(128), w=["lt_g"])
            ip = 0
            for t in range(NT):
                b = t % 2
                xt = xts[b]
                P.dma(xt[:], src[t * 128:(t + 1) * 128, :], w=["lt_x%d" % b])
                if gain is not None:
                    P.op("act", lambda e: e.activation(out=junk[:], in_=xt[:], func=AF.Square, accum_out=ss[:, b:b + 1]),
                         r=["lt_x%d" % b], w=["lt_j", "lt_ss%d" % b])
                    self.rstd_from_ss(rs[:, b:b + 1], ss[:, b:b + 1], K, "lt_rs%d" % b, "lt_ss%d" % b)
                    P.op("dve", lambda e: e.scalar_tensor_tensor(out=xns[b][:], in0=xt[:], scalar=rs[:, b:b + 1], in1=gb[:],
                                                                 op0=ALU.mult, op1=ALU.mult),
                         r=["lt_x%d" % b, "lt_rs%d" % b, "lt_g"], w=["lt_n%d" % b])
                    xn, xid = xns[b], "lt_n%d" % b
                elif need_cast:
                    self.copy("pool", xns[b][:], xt[:], ["lt_x%d" % b], ["lt_n%d" % b])
                    xn, xid = xns[b], "lt_n%d" % b
                else:
                    xn, xid = xt, "lt_x%d" % b
                for k4 in range(KC // 4):
                    pt = pts[ip % 2]
                    pid = "lt_p%d" % (ip % 2)
                    ip += 1
                    for kk in range(4):
                        k = k4 * 4 + kk
                        P.op("pe", lambda e: e.transpose(out=pt[:, kk * 128:(kk + 1) * 128], in_=xn[:, k * 128:(k + 1) * 128],
                                                         identity=self.ident[:]), r=[xid, "ident"], w=[pid])
                    self.copy(self.alt("act", "dve"), dstT[:, k4 * 4:(k4 + 1) * 4, t * 128:(t + 1) * 128],
                              pt[:].rearrange("p (a b) -> p a b", b=128), [pid], [dst_id])
            P.barrier()

    def linear(self, xT, x_id, KC, W, col0, ncols, mode, sink, slab=512, swap=False, nps=2):
        P = self.P
        Wv = W.rearrange("(kc p) n -> p kc n", p=128)
        with ExitStack() as st:
            w32 = [P.sb("ln_w%d" % i, [128, KC, slab], F32, st) for i in range(2)]
            wb = [P.sb("ln_b%d" % i, [128, KC, slab], BF16, st) for i in range(2)]
            ws = [P.sb("ln_s%d" % i, [128, KC, slab], BF16, st) for i in range(2)] if swap else None
            npsum = nps * (2 if swap else 1)
            pss = [P.ps("ln_p%d" % i, [128, 512], F32, st) for i in range(npsum)]
            ip = 0
            for si, s0 in enumerate(range(col0, col0 + ncols, slab)):
                n = min(slab, col0 + ncols - s0)
                b = si % 2
                P.dma(w32[b][:, :, 0:n], Wv[:, :, s0:s0 + n], w=["ln_w%d" % b])
                half = KC // 2
                self.copy("act", wb[b][:, 0:half, 0:n], w32[b][:, 0:half, 0:n], ["ln_w%d" % b], [("ln_b%d" % b, 0)])
                self.copy("pool", wb[b][:, half:KC, 0:n], w32[b][:, half:KC, 0:n], ["ln_w%d" % b], [("ln_b%d" % b, 1)])
                wid = [("ln_b%d" % b, 0), ("ln_b%d" % b, 1)]
                if swap:
                    self.copy("pool", ws[b][:, :, 0:n:2], w32[b][:, :, 1:n:2], ["ln_w%d" % b], [("ln_s%d" % b, 0)])
                    self.copy("dve", ws[b][:, :, 1:n:2], w32[b][:, :, 0:n:2], ["ln_w%d" % b], [("ln_s%d" % b, 1)])
                    sid = [("ln_s%d" % b, 0), ("ln_s%d" % b, 1)]
                if mode == "tok":
                    for t in range(NT):
                        ps = pss[ip % npsum]
                        pid = "ln_p%d" % (ip % npsum)
                        ip += 1
                        for k in range(KC):
                            P.op("pe", lambda e: e.matmul(ps[:, 0:n], lhsT=xT[:, k, t * 128:(t + 1) * 128], rhs=wb[b][:, k, 0:n],
                                                          start=(k == 0), stop=(k == KC - 1)), r=[x_id] + wid, w=[pid])
                        sink(t, s0 - col0, n, ps, pid)
                else:
                    for c in range(n // 128):
                        for tg in range(4):
                            ps = pss[ip % npsum]
                            pid = "ln_p%d" % (ip % npsum)
                            ip += 1
                            for k in range(KC):
                                P.op("pe", lambda e: e.matmul(ps[:, :], lhsT=wb[b][:, k, c * 128:(c + 1) * 128],
                                                              rhs=xT[:, k, tg * 512:(tg + 1) * 512],
                                                              start=(k == 0), stop=(k == KC - 1)), r=[x_id] + wid, w=[pid])
                            if swap:
                                ps2 = pss[ip % npsum]
                                pid2 = "ln_p%d" % (ip % npsum)
                                ip += 1
                                for k in range(KC):
                                    P.op("pe", lambda e: e.matmul(ps2[:, :], lhsT=ws[b][:, k, c * 128:(c + 1) * 128],
                                                                  rhs=xT[:, k, tg * 512:(tg + 1) * 512],
                                                                  start=(k == 0), stop=(k == KC - 1)), r=[x_id] + sid, w=[pid2])
                                sink((s0 - col0) // 128 + c, tg, ps, pid, ps2, pid2)
                            else:
                                sink((s0 - col0) // 128 + c, tg, ps, pid)
            P.barrier()

    def make_store_sink(self, st, dst, mode, dt, func=None, nstage=3, tag="sk"):
        P = self.P
        stages = [P.sb("%s_st%d" % (tag, i), [128, 512], dt, st) for i in range(nstage)]
        cnt = [0]

        def sink(a, b, *rest):
            i = cnt[0] % nstage
            cnt[0] += 1
            sid = "%s_st%d" % (tag, i)
            if mode == "tok":
                n, ps, pid = rest
                if func is not None:
                    P.op("act", lambda e: e.activation(out=stages[i][:, 0:n], in_=ps[:, 0:n], func=func), r=[pid], w=[sid])
                else:
                    self.copy(self.alt("act", "dve"), stages[i][:, 0:n], ps[:, 0:n], [pid], [sid])
                P.dma(dst[a * 128:(a + 1) * 128, b:b + n], stages[i][:, 0:n], r=[sid], w=[tag + "_d"], q="act")
            else:
                ps, pid = rest
                self.copy(self.alt("act", "dve"), stages[i][:, :], ps[:, :], [pid], [sid])
                P.dma(dst[a * 128:(a + 1) * 128, b * 512:(b + 1) * 512], stages[i][:, :], r=[sid], w=[tag + "_d"], q="act")
        return sink


def host_consts():
    c = {}
    c["c_ident"] = np.eye(128, dtype=np.float32)
    s = np.arange(128)[:, None]
    cidx = np.arange(256)[None, :]
    dist = cidx - s
    n = np.maximum(dist, 0)
    nf = np.maximum(n, 1).astype(np.float32)
    large = 16 + (np.log(nf / np.float32(16)) / np.float32(np.log(128 / 16)) * np.float32(16)).astype(np.int32)
    large = np.minimum(large, 31)
    bucket = np.where(n < 16, n, large)
    oh = np.zeros((128, 32, 256), np.float32)
    for b in range(32):
        oh[:, b, :] = (bucket == b)
    c["c_boh"] = oh
    c["c_bmask"] = (dist >= 0).astype(np.float32)
    pos = np.arange(S_LEN, dtype=np.float32)
    inv = (np.float32(10000.0) ** (-np.arange(0, 64, 2, dtype=np.float32) / np.float32(64))).astype(np.float32)
    ang = pos[:, None] * inv[None, :]
    cos = np.cos(ang).astype(np.float64)
    sin = np.sin(ang).astype(np.float64)
    rq = np.zeros((4, 2, 128, S_LEN), np.float32)
    rk = np.zeros((4, 2, 128, S_LEN), np.float32)
    t64 = np.arange(S_LEN, dtype=np.float64)
    for ch in range(4):
        for p in range(128):
            h = 2 * ch + p // 64
            d = p % 64
            i = d // 2
            sign = -1.0 if d % 2 == 0 else 1.0
            lg = np.log(1.0 - 2.0 ** (-5.0 - h))
            dq = np.exp(t64 * lg) * 64 ** -0.5
            dk = np.exp(-t64 * lg)
            rq[ch, 0, p] = cos[:, i] * dq
            rq[ch, 1, p] = sign * sin[:, i] * dq
            rk[ch, 0, p] = cos[:, i] * dk
            rk[ch, 1, p] = sign * sin[:, i] * dk
    c["c_ropeq"] = rq
    c["c_ropek"] = rk
    return c


class MK0(MK):
    def build_bias_tables(self, rel_table, c_boh, c_bmask):
        P = self.P
        self.tbl_bc = P.sb("tbl_bc", [128, 256], F32)
        self.EB = P.sb("EB", [128, 8, 256], F32)
        self.mask01 = P.sb("mask01", [128, 256], F32)
        with ExitStack() as st:
            oh = P.sb("bt_oh", [128, 32, 256], F32, st)
            P.dma(self.tbl_bc[:], rel_table.ap().rearrange("b h -> (b h)").unsqueeze(0).partition_broadcast(128) if False else
                  rel_table.ap().rearrange("(o b) h -> o (b h)", o=1).partition_broadcast(128), w=["tbl_bc"])
            P.dma(oh[:], c_boh.ap(), w=["bt_oh"])
            P.dma(self.mask01[:], c_bmask.ap(), w=["mask01"])
            for h in range(8):
                for b in range(32):
                    if b == 0:
                        P.op("dve", lambda e: e.tensor_scalar(out=self.EB[:, h, :], in0=oh[:, 0, :], scalar1=self.tbl_bc[:, h:h + 1],
                                                              scalar2=None, op0=ALU.mult), r=["bt_oh", "tbl_bc"], w=[("EB", h)])
                    else:
                        P.op("dve", lambda e: e.scalar_tensor_tensor(out=self.EB[:, h, :], in0=oh[:, b, :],
                                                                     scalar=self.tbl_bc[:, b * 8 + h:b * 8 + h + 1],
                                                                     in1=self.EB[:, h, :], op0=ALU.mult, op1=ALU.add),
                             r=["bt_oh", "tbl_bc", ("EB", h)], w=[("EB", h)])
                P.op("act", lambda e: e.activation(out=self.EB[:, h, :], in_=self.EB[:, h, :], func=AF.Exp), r=[("EB", h)], w=[("EB", h)])
                P.op("dve", lambda e: e.tensor_tensor(out=self.EB[:, h, :], in0=self.EB[:, h, :], in1=self.mask01[:], op=ALU.mult),
                     r=[("EB", h), "mask01"], w=[("EB", h)])
            P.barrier()

    def lam_scalar(self, ab_lambda):
        P = self.P
        self.neglam = P.sb("neglam", [128, 1], F32)
        with ExitStack() as st:
            lp = P.sb("lm_lp", [128, 256], F32, st)
            pr = P.sb("lm_pr", [128, 2, 64], F32, st)
            sm = P.sb("lm_sm", [128, 2], F32, st)
            P.dma(lp[:], ab_lambda.ap().rearrange("o a d -> o (a d)").partition_broadcast(128), w=["lm_lp"])
            P.op("dve", lambda e: e.tensor_tensor(out=pr[:, 0, :], in0=lp[:, 0:64], in1=lp[:, 64:128], op=ALU.mult), r=["lm_lp"], w=["lm_pr"])
            P.op("dve", lambda e: e.tensor_tensor(out=pr[:, 1, :], in0=lp[:, 128:192], in1=lp[:, 192:256], op=ALU.mult), r=["lm_lp", "lm_pr"], w=["lm_pr"])
            P.op("dve", lambda e: e.tensor_reduce(out=sm[:], in_=pr[:], axis=AX.X, op=ALU.add), r=["lm_pr"], w=["lm_sm"])
            P.op("act", lambda e: e.activation(out=sm[:], in_=sm[:], func=AF.Exp), r=["lm_sm"], w=["lm_sm"])
            P.op("dve", lambda e: e.tensor_tensor(out=self.neglam[:], in0=sm[:, 1:2], in1=sm[:, 0:1], op=ALU.subtract), r=["lm_sm"], w=["neglam"])
            P.op("dve", lambda e: e.tensor_scalar(out=self.neglam[:], in0=self.neglam[:], scalar1=-LAM_INIT0, scalar2=None, op0=ALU.add),
                 r=["neglam"], w=["neglam"])
            P.barrier()

    def attention(self, kind, KT_d, QT_d, V_d, nheads, norm_gain, out_d, out_col0, gate_d=None):
        P = self.P
        nmap = 2 if kind == "diff" else 1
        dvp = 129 if (kind == "diff" or os.environ.get("RET_V129", "0") == "1") else 128
        dvo = 129 if kind == "diff" else 128
        with ExitStack() as st:
            KT = [P.sb("at_k%d" % i, [128, S_LEN], BF16, st) for i in range(2)]
            QT = [P.sb("at_q%d" % i, [128, S_LEN], BF16, st) for i in range(2)]
            Vt = [P.sb("at_v%d" % i, [128, NT, dvp], BF16, st) for i in range(2)]
            gn = P.sb("at_gn", [128, 128], F32, st)
            PTs = [P.sb("at_pt%d" % i, [128, 512], BF16, st) for i in range(3)]
            ex = [P.sb("at_ex%d" % i, [128, 256], F32, st) for i in range(2)]
            pS = [P.ps("at_ps%d" % i, [128, 512], F32, st) for i in range(2)]
            pO = [P.ps("at_po%d" % i, [128, 4, 256], F32, st) for i in range(nmap)]
            sm = P.sb("at_sm", [128, 8], F32, st)
            t0 = [P.sb("at_t0%d" % i, [128, 128], F32, st) for i in range(2)]
            junk = P.sb("at_jk", [128, 128], F32, st)
            og = [P.sb("at_og%d" % i, [128, 128], BF16, st) for i in range(2)]
            gt = [P.sb("at_gt%d" % i, [128, 128], F32, st) for i in range(2)] if gate_d is not None else None
            P.dma(gn[:], norm_gain.partition_broadcast(128), w=["at_gn"])
            if kind == "diff":
                P.op("dve", lambda e: e.tensor_scalar(out=gn[:], in0=gn[:], scalar1=1.0 - LAM_INIT0, scalar2=None, op0=ALU.mult),
                     r=["at_gn"], w=["at_gn"])
                for i in range(2):
                    P.op("pool", lambda e: e.memset(Vt[i][:, :, 128:129], 1.0), w=["at_v%d" % i])
            ipt = 0
            isx = 0
            ifin = 0
            for h in range(nheads if kind == "diff" else int(os.environ.get("RET_H", "8"))):
                hb = h % 2
                if kind == "diff":
                    if True:
                        P.dma(KT[hb][:], KT_d[h * 128:(h + 1) * 128, :], w=["at_k%d" % hb])
                        P.dma(QT[hb][:], QT_d[h * 128:(h + 1) * 128, :], w=["at_q%d" % hb])
                    kq_b, kbase = hb, None
                else:
                    if h % 2 == 0:
                        cb = (h // 2) % 2
                        P.dma(KT[cb][:], KT_d[(h // 2) * 128:(h // 2 + 1) * 128, :], w=["at_k%d" % cb])
                        P.dma(QT[cb][:], QT_d[(h // 2) * 128:(h // 2 + 1) * 128, :], w=["at_q%d" % cb])
                    kq_b = (h // 2) % 2
                P.dma(Vt[hb][:, :, 0:128], V_d[:, h * 128:(h + 1) * 128].rearrange("(j p) d -> p j d", p=128), w=["at_v%d" % hb])
                kid, qid, vid = "at_k%d" % kq_b, "at_q%d" % kq_b, "at_v%d" % hb
                for g in range(4):
                    for m in range(nmap):
                        pb = 64 * m if kind == "diff" else 64 * (h % 2)
                        Km = KT[kq_b][pb:pb + 64, :]
                        Qm = QT[kq_b][pb:pb + 64, :]
                        po = pO[m]
                        poid = "at_po%d" % m
                        for j in range(4 * g + 4):
                            c0 = max(0, j - 4 * g)
                            ps = pS[isx % 2]
                            psid = "at_ps%d" % (isx % 2)
                            isx += 1
                            P.op("pe", lambda e: e.matmul(ps[:, c0 * 128:512], lhsT=Km[:, j * 128:(j + 1) * 128],
                                                          rhs=Qm[:, g * 512 + c0 * 128:(g + 1) * 512], start=True, stop=True),
                                 r=[kid, qid], w=[psid])
                            pt = PTs[ipt % 3]
                            ptid = "at_pt%d" % (ipt % 3)
                            ipt += 1
                            near_lo = c0 * 128
                            if j >= 4 * g:
                                near_hi = min(512, near_lo + 256)
                                eb_lo = 0
                            else:
                                near_hi = 128 if j == 4 * g - 1 else 0
                                eb_lo = 128
                            nw = near_hi - near_lo if near_hi > near_lo else 0
                            if kind == "diff":
                                if nw > 0:
                                    exb = ex[ipt % 2]
                                    exid = "at_ex%d" % (ipt % 2)
                                    P.op("act", lambda e: e.activation(out=exb[:, 0:nw], in_=ps[:, near_lo:near_hi], func=AF.Exp, scale=0.125),
                                         r=[psid], w=[exid])
                                    P.op("dve", lambda e: e.tensor_tensor(out=pt[:, near_lo:near_hi], in0=exb[:, 0:nw],
                                                                          in1=self.EB[:, h, eb_lo:eb_lo + nw], op=ALU.mult),
                                         r=[exid, ("EB", h)], w=[(ptid, 0)])
                                far_lo = max(near_hi, near_lo)
                                if far_lo < 512:
                                    P.op("act", lambda e: e.activation(out=pt[:, far_lo:512], in_=ps[:, far_lo:512], func=AF.Exp,
                                                                       bias=self.tbl_bc[:, 31 * 8 + h:31 * 8 + h + 1], scale=0.125),
                                         r=[psid, "tbl_bc"], w=[(ptid, 1)])
                            else:
                                far_lo = near_lo
                                if j >= 4 * g:
                                    if os.environ.get("RET_MASK2", "1") == "1":
                                        exb = ex[ipt % 2]
                                        exid = "at_ex%d" % (ipt % 2)
                                        self.copy("act", exb[:, 0:128], ps[:, near_lo:near_lo + 128], [psid], [exid])
                                        P.op("dve", lambda e: e.tensor_tensor(out=pt[:, near_lo:near_lo + 128], in0=exb[:, 0:128],
                                                                              in1=self.mask01[:, 0:128], op=ALU.mult), r=[exid, "mask01"], w=[(ptid, 0)])
                                    else:
                                        P.op("dve", lambda e: e.tensor_tensor(out=pt[:, near_lo:near_lo + 128], in0=ps[:, near_lo:near_lo + 128],
                                                                              in1=self.mask01[:, 0:128], op=ALU.mult), r=[psid, "mask01"], w=[(ptid, 0)])
                                    far_lo = near_lo + 128
                                if far_lo < 512:
                                    self.copy(self.alt("act", "dve"), pt[:, far_lo:512], ps[:, far_lo:512], [psid], [(ptid, 1)])
                            for il in range(c0, 4):
                                P.op("pe", lambda e: e.matmul(po[:, il, 0:dvo], lhsT=pt[:, il * 128:(il + 1) * 128], rhs=Vt[hb][:, j, 0:dvo],
                                                              start=(j == 0 and il % 2 == 0), stop=(j == 4 * g + il),
                                                              skip_group_check=True), r=[(ptid, 0), (ptid, 1), vid], w=[poid])
                    for il in range(4 if int(os.environ.get("RET_FIN", "1")) or kind == "diff" else 0):
                        qb = 4 * g + il
                        f = ifin % 2
                        ifin += 1
                        tt, ttid = t0[f], "at_t0%d" % f
                        if kind == "diff":
                            P.op("dve", lambda e: e.reciprocal(out=sm[:, 0:1], in_=pO[0][:, il, 128:129]), r=["at_po0"], w=["at_sm"])
                            P.op("dve", lambda e: e.reciprocal(out=sm[:, 1:2], in_=pO[1][:, il, 128:129]), r=["at_po1", "at_sm"], w=["at_sm"])
                            P.op("dve", lambda e: e.tensor_tensor(out=sm[:, 2:3], in0=sm[:, 1:2], in1=self.neglam[:], op=ALU.mult),
                                 r=["at_sm", "neglam"], w=["at_sm"])
                            P.op("act", lambda e: e.activation(out=tt[:], in_=pO[0][:, il, 0:128], func=AF.Copy, scale=sm[:, 0:1]),
                                 r=["at_po0", "at_sm"], w=[ttid])
                            P.op("dve", lambda e: e.scalar_tensor_tensor(out=tt[:], in0=pO[1][:, il, 0:128], scalar=sm[:, 2:3], in1=tt[:],
                                                                         op0=ALU.mult, op1=ALU.add), r=["at_po1", "at_sm", ttid], w=[ttid])
                        else:
                            self.copy("act", tt[:], pO[0][:, il, 0:128], ["at_po0"], [ttid])
                        P.op("act", lambda e: e.activation(out=junk[:], in_=tt[:], func=AF.Square, accum_out=sm[:, 4:5]),
                             r=[ttid, "at_sm"], w=["at_jk", "at_sm"])
                        self.rstd_from_ss(sm[:, 5:6], sm[:, 4:5], 128, "at_sm", "at_sm")
                        if gate_d is None:
                            P.op("dve", lambda e: e.scalar_tensor_tensor(out=og[f][:], in0=tt[:], scalar=sm[:, 5:6], in1=gn[:],
                                                                         op0=ALU.mult, op1=ALU.mult), r=[ttid, "at_sm", "at_gn"], w=["at_og%d" % f])
                        else:
                            P.dma(gt[f][:], gate_d[qb * 128:(qb + 1) * 128, h * 128:(h + 1) * 128], w=["at_gt%d" % f])
                            P.op("dve", lambda e: e.scalar_tensor_tensor(out=tt[:], in0=tt[:], scalar=sm[:, 5:6], in1=gn[:],
                                                                         op0=ALU.mult, op1=ALU.mult), r=[ttid, "at_sm", "at_gn"], w=[ttid])
                            P.op("dve", lambda e: e.tensor_tensor(out=og[f][:], in0=tt[:], in1=gt[f][:], op=ALU.mult),
                                 r=[ttid, "at_gt%d" % f], w=["at_og%d" % f])
                        P.dma(out_d[qb * 128:(qb + 1) * 128, out_col0 + h * 128:out_col0 + (h + 1) * 128], og[f][:],
                              r=["at_og%d" % f], w=["at_out"], q="act")
            P.barrier()


class MK1(MK0):
    def layer0_mixer(self, x_ap, h_out_ap, w):
        P = self.P
        qaT = self.scratch("qaT", [1024, S_LEN], BF16)
        kaT = self.scratch("kaT", [1024, S_LEN], BF16)
        va = self.scratch("va", [S_LEN, 1024], BF16)
        qbT = self.scratch("qbT", [512, S_LEN], BF16)
        kbT = self.scratch("kbT", [512, S_LEN], BF16)
        vb = self.scratch("vb", [S_LEN, 1024], BF16)
        gbs = self.scratch("gbs", [S_LEN, 1024], F32)
        mixed = self.scratch("mixed0", [S_LEN, 2048], BF16)
        Win = w["ab_w_in"].ap()[0]
        with ExitStack() as st:
            hT = P.sb("hT", [128, 16, S_LEN], BF16, st)
            self.loadT(x_ap, DM, hT, "hT", gain=w["norm_mix"].ap()[0:1, :])
            if getattr(self, "stop", 99) <= 1:
                return
            with ExitStack() as s2:
                self.linear(hT, "hT", 16, Win, 0, 1024, "feat", self.make_store_sink(s2, qaT.ap(), "feat", BF16, tag="sqa"))
            if getattr(self, "stop", 99) <= 2:
                return
            with ExitStack() as s2:
                self.linear(hT, "hT", 16, Win, 1024, 1024, "feat", self.make_store_sink(s2, kaT.ap(), "feat", BF16, tag="ska"))
            with ExitStack() as s2:
                self.linear(hT, "hT", 16, Win, 2048, 1024, "tok", self.make_store_sink(s2, va.ap(), "tok", BF16, tag="sva"))
            if getattr(self, "stop", 99) <= 3:
                return
            for (c0, dst, tab, tg_) in ((3072, qbT, w["c_ropeq"], "sqb"), (3584, kbT, w["c_ropek"], "skb")):
                with ExitStack() as s2:
                    cs = [P.sb("%s_c%d" % (tg_, i), [128, 2, 512], F32, s2) for i in range(2)]
                    t1 = [P.sb("%s_a%d" % (tg_, i), [128, 512], F32, s2) for i in range(2)]
                    t2 = [P.sb("%s_b%d" % (tg_, i), [128, 512], F32, s2) for i in range(2)]
                    so = [P.sb("%s_o%d" % (tg_, i), [128, 512], BF16, s2) for i in range(2)]
                    cnt = [0]

                    def rsink(chunk, tg, ps, pid, ps2, pid2, dst=dst, tab=tab, tg_=tg_, cs=cs, t1=t1, t2=t2, so=so, cnt=cnt):
                        i = cnt[0] % 2
                        cnt[0] += 1
                        P.dma(cs[i][:], tab.ap()[chunk, :, :, tg * 512:(tg + 1) * 512].rearrange("a p t -> p a t"), w=["%s_c%d" % (tg_, i)])
                        P.op("dve", lambda e: e.tensor_tensor(out=t1[i][:], in0=ps[:, :], in1=cs[i][:, 0, :], op=ALU.mult),
                             r=[pid, "%s_c%d" % (tg_, i)], w=["%s_a%d" % (tg_, i)])
                        P.op("dve", lambda e: e.tensor_tensor(out=t2[i][:], in0=ps2[:, :], in1=cs[i][:, 1, :], op=ALU.mult),
                             r=[pid2, "%s_c%d" % (tg_, i)], w=["%s_b%d" % (tg_, i)])
                        P.op("pool", lambda e: e.tensor_tensor(out=so[i][:], in0=t1[i][:], in1=t2[i][:], op=ALU.add),
                             r=["%s_a%d" % (tg_, i), "%s_b%d" % (tg_, i)], w=["%s_o%d" % (tg_, i)])
                        P.dma(dst.ap()[chunk * 128:(chunk + 1) * 128, tg * 512:(tg + 1) * 512], so[i][:], r=["%s_o%d" % (tg_, i)], w=[tg_ + "_d"], q="act")
                    self.linear(hT, "hT", 16, Win, c0, 512, "feat", rsink, slab=256, swap=True)
            with ExitStack() as s2:
                self.linear(hT, "hT", 16, Win, 4096, 1024, "tok", self.make_store_sink(s2, vb.ap(), "tok", BF16, tag="svb"))
            with ExitStack() as s2:
                self.linear(hT, "hT", 16, Win, 5120, 1024, "tok", self.make_store_sink(s2, gbs.ap(), "tok", F32, func=AF.Silu, tag="sgb"))
        P.barrier()
        if getattr(self, "stop", 99) <= 4:
            return
        if getattr(self, "stop", 99) != 7:
            self.attention("diff", kaT.ap(), qaT.ap(), va.ap(), 8, w["ab_a_norm"].ap()[0:1, :], mixed.ap(), 0)
        if getattr(self, "stop", 99) <= 5:
            return
        if os.environ.get("RET_DATA", "b") == "a":
            kbT, qbT, vb = kaT, qaT, va
        self.attention("ret", kbT.ap(), qbT.ap(), vb.ap(), 8, w["ab_b_norm"].ap()[0:1, :], mixed.ap(), 1024, gate_d=gbs.ap())
        if getattr(self, "stop", 99) <= 7:
            return
        self.out_proj(mixed.ap(), 2048, w["ab_w_out"].ap()[0], x_ap, h_out_ap, BF16)

    def out_proj(self, mixed_ap, K, Wout, res_ap, out_ap, src_dt):
        P = self.P
        KC = K // 128
        with ExitStack() as st:
            mT = P.sb("mT", [128, KC, S_LEN], BF16, st)
            self.loadT(mixed_ap, K, mT, "mT", gain=None, src_dt=src_dt)
            rs = [P.sb("op_r%d" % i, [128, 512], F32, st) for i in range(3)]
            cnt = [0]

            def sink(t, coff, n, ps, pid):
                i = cnt[0] % 3
                cnt[0] += 1
                P.dma(rs[i][:, 0:n], res_ap[t * 128:(t + 1) * 128, coff:coff + n], w=["op_r%d" % i])
                P.op("dve", lambda e: e.tensor_tensor(out=rs[i][:, 0:n], in0=ps[:, 0:n], in1=rs[i][:, 0:n], op=ALU.add),
                     r=[pid, "op_r%d" % i], w=["op_r%d" % i])
                P.dma(out_ap[t * 128:(t + 1) * 128, coff:coff + n], rs[i][:, 0:n], r=["op_r%d" % i], w=["op_out"], q="act")
            self.linear(mT, "mT", KC, Wout, 0, DM, "tok", sink, slab=(512 if KC <= 16 else 256))
        P.barrier()


class MK2(MK1):
    def peer(self, h_ap, out_ap, gain_ap, Wq, keys, U, V, lid):
        P = self.P
        xT_d = self.scratch("pe_xT%d" % lid, [128, 16, S_LEN], BF16)
        S1_d = self.scratch("pe_S1%d" % lid, [S_LEN, 8, 128], F32)
        S0_d = self.scratch("pe_S0%d" % lid, [S_LEN, 8, 128], F32)
        TH_d = self.scratch("pe_TH%d" % lid, [S_LEN, 8], F32)
        NEG = -1.0e30
        NTT = int(os.environ.get("PEER_NT", "16"))
        with ExitStack() as st:
            xT = P.sb("pe_xT", [128, 16, S_LEN], BF16, st)
            self.loadT(h_ap, DM, xT, "pe_xT", gain=gain_ap)
            P.dma(xT_d.ap(), xT[:], r=["pe_xT"], w=["pe_xT_d"], q="act")
            qT = P.sb("pe_qT", [128, 16, S_LEN], BF16, st)

            def qsink(chunk, tg, ps, pid):
                self.copy(self.alt("act", "dve"), qT[:, chunk, tg * 512:(tg + 1) * 512], ps[:, :], [pid], ["pe_qT"])
            self.linear(xT, "pe_xT", 16, Wq, 0, 2048, "feat", qsink, slab=256)
            k32 = P.sb("pe_k32", [128, 16, 128], F32, st)
            kb = P.sb("pe_kb", [128, 16, 128], BF16, st)
            kT = P.sb("pe_kT", [128, 16, 128], BF16, st)
            ptk = P.ps("pe_ptk", [128, 1024], BF16, st)
            P.dma(k32[:], keys.rearrange("h c k d -> k (h c) d"), w=["pe_k32"])
            self.copy("dve", kb[:], k32[:], ["pe_k32"], ["pe_kb"])
            for half in range(2):
                for kk in range(8):
                    hc = half * 8 + kk
                    P.op("pe", lambda e: e.transpose(out=ptk[:, kk * 128:(kk + 1) * 128], in_=kb[:, hc, :], identity=self.ident[:]),
                         r=["pe_kb", "ident"], w=["pe_ptk"])
                self.copy("act", kT[:, half * 8:(half + 1) * 8, :], ptk[:].rearrange("p (a b) -> p a b", b=128), ["pe_ptk"], ["pe_kT"])
            psS = [P.ps("pe_pss%d" % i, [128, 4, 128], F32, st) for i in range(2)]
            Ssb = [P.sb("pe_S%d" % i, [128, 16, 128], F32, st) for i in range(2)]
            top = P.sb("pe_top", [128, 16, 16], F32, st)
            tmp = P.sb("pe_tmp", [128, 128], F32, st)
            cand = P.sb("pe_cand", [128, 8, 256], F32, st)
            tm2 = P.sb("pe_tm2", [128, 256], F32, st)
            tm3 = P.sb("pe_tm3", [128, 256], F32, st)
            bb = P.sb("pe_bb", [128, 8, 16], F32, st)
            b17 = P.sb("pe_b17", [128, 8, 8], F32, st)
            eb = P.sb("pe_eb", [128, 8, 16], F32, st)
            sc = P.sb("pe_sc", [128, 6, 8], F32, st)
            s0s = [P.sb("pe_s0s%d" % i, [128, 8, 128], F32, st) for i in range(2)]
            ips = 0
            for t in range(NTT):
                Sb = Ssb[t % 2]
                Sid = "pe_S%d" % (t % 2)
                for q4 in range(4):
                    ps = psS[ips % 2]
                    psid = "pe_pss%d" % (ips % 2)
                    ips += 1
                    for kk in range(4):
                        hc = q4 * 4 + kk
                        P.op("pe", lambda e: e.matmul(ps[:, kk, :], lhsT=qT[:, hc, t * 128:(t + 1) * 128], rhs=kT[:, hc, :],
                                                      start=True, stop=True, skip_group_check=True), r=["pe_qT", "pe_kT"], w=[psid])
                    self.copy(self.alt("act", "dve"), Sb[:, q4 * 4:(q4 + 1) * 4, :], ps[:, :, :], [psid], [(Sid, q4)])
                Sall = [(Sid, q4) for q4 in range(4)]
                P.dma(S1_d.ap()[t * 128:(t + 1) * 128, :, :], Sb[:].rearrange("p (h c) k -> p h c k", c=2)[:, :, 1, :], r=Sall, w=["pe_S1_d"], q="act")
                for hc in range(16):
                    P.op("dve", lambda e: e.max(out=top[:, hc, 0:8], in_=Sb[:, hc, :]), r=Sall, w=["pe_top"])
                    P.op("dve", lambda e: e.match_replace(out=tmp[:], in_to_replace=top[:, hc, 0:8], in_values=Sb[:, hc, :], imm_value=NEG),
                         r=Sall + ["pe_top"], w=["pe_tmp"])
                    P.op("dve", lambda e: e.max(out=top[:, hc, 8:16], in_=tmp[:]), r=["pe_tmp"], w=["pe_top"])
                t4 = top[:].rearrange("p (h c) k -> p h c k", c=2)
                P.op("pool", lambda e: e.tensor_tensor(out=cand[:].rearrange("p h (a b) -> p h a b", b=16),
                                                       in0=t4[:, :, 0, :].unsqueeze(3).to_broadcast([128, 8, 16, 16]),
                                                       in1=t4[:, :, 1, :].unsqueeze(2).to_broadcast([128, 8, 16, 16]), op=ALU.add),
                     r=["pe_top"], w=["pe_cand"])
                for h in range(8):
                    P.op("dve", lambda e: e.max(out=bb[:, h, 0:8], in_=cand[:, h, :]), r=["pe_cand"], w=["pe_bb"])
                    P.op("dve", lambda e: e.match_replace(out=tm2[:], in_to_replace=bb[:, h, 0:8], in_values=cand[:, h, :], imm_value=NEG),
                         r=["pe_cand", "pe_bb"], w=["pe_tm2"])
                    P.op("dve", lambda e: e.max(out=bb[:, h, 8:16], in_=tm2[:]), r=["pe_tm2"], w=["pe_bb"])
                P.op("dve", lambda e: e.tensor_tensor(out=eb[:], in0=bb[:], in1=bb[:, :, 0:1].to_broadcast([128, 8, 16]), op=ALU.subtract),
                     r=["pe_bb"], w=["pe_eb"])
                P.op("act", lambda e: e.activation(out=eb[:], in_=eb[:], func=AF.Exp), r=["pe_eb"], w=["pe_eb"])
                P.op("dve", lambda e: e.tensor_reduce(out=sc[:, 0, :], in_=eb[:], axis=AX.X, op=ALU.add), r=["pe_eb"], w=["pe_sc"])
                P.op("act", lambda e: e.activation(out=sc[:, 0, :], in_=sc[:, 0, :], func=AF.Ln), r=["pe_sc"], w=["pe_sc"])
                P.op("dve", lambda e: e.tensor_tensor(out=sc[:, 1, :], in0=sc[:, 0, :], in1=bb[:, :, 0], op=ALU.add), r=["pe_sc", "pe_bb"], w=["pe_sc"])
                P.op("dve", lambda e: e.tensor_tensor(out=sc[:, 2, :], in0=bb[:, :, 15], in1=sc[:, 1, :], op=ALU.subtract), r=["pe_bb", "pe_sc"], w=["pe_sc"])
                P.op("dve", lambda e: e.tensor_scalar(out=sc[:, 3, :], in0=sc[:, 2, :], scalar1=-5.0e-6, scalar2=None, op0=ALU.add),
                     r=["pe_sc"], w=["pe_sc"])
                P.op("act", lambda e: e.activation(out=sc[:, 4, :], in_=sc[:, 3, :], func=AF.Exp), r=["pe_sc"], w=["pe_sc"])
                P.op("dve", lambda e: e.tensor_tensor(out=sc[:, 5, :], in0=sc[:, 1, :], in1=sc[:, 3, :], op=ALU.add), r=["pe_sc"], w=["pe_sc"])
                P.dma(TH_d.ap()[t * 128:(t + 1) * 128, :], sc[:, 4, :], r=["pe_sc"], w=["pe_TH_d"], q="act")
                if "pe_dbg" in self.dbg:
                    if t == 0:
                        self.dbg_bb = self.scratch("pe_dbg", [S_LEN, 8 * 16 + 8 * 8 + 16 * 16], F32)
                    P.dma(self.dbg_bb.ap()[t * 128:(t + 1) * 128, 0:128], bb[:].rearrange("p a b -> p (a b)"), r=["pe_bb"], w=["dbg1"], q="act")
                    P.dma(self.dbg_bb.ap()[t * 128:(t + 1) * 128, 192:448], top[:].rearrange("p a b -> p (a b)"), r=["pe_top"], w=["dbg3"], q="act")
                s0 = s0s[t % 2]
                P.op("pool", lambda e: e.tensor_tensor(out=s0[:], in0=Sb[:].rearrange("p (h c) k -> p h c k", c=2)[:, :, 0, :],
                                                       in1=sc[:, 5, :].unsqueeze(2).to_broadcast([128, 8, 128]), op=ALU.subtract),
                     r=Sall + ["pe_sc"], w=["pe_s0s%d" % (t % 2)])
                P.dma(S0_d.ap()[t * 128:(t + 1) * 128, :, :], s0[:], r=["pe_s0s%d" % (t % 2)], w=["pe_S0_d"], q="act")
        P.barrier()
        if int(os.environ.get("PEER_STOP", "9")) <= 3:
            return
        NTG = int(os.environ.get("PEER_NTG", "4"))
        NEG_ = int(os.environ.get("PEER_NEG", "32"))
        with ExitStack() as st:
            xg = P.sb("pp_xg", [128, 16, 512], BF16, st)
            acc = P.sb("pp_acc", [128, 4, DM], F32, st)
            S1g = P.sb("pp_S1g", [128, 4, 8, 128], F32, st)
            THg = P.sb("pp_THg", [128, 4, 8], F32, st)
            S0e = [P.sb("pp_S0e%d" % i, [128, 4, 8, 4], F32, st) for i in range(2)]
            stg = [P.sb("pp_stg%d" % i, [128, DM], F32, st) for i in range(2)]
            ubf = [P.sb("pp_ubf%d" % i, [128, DM], BF16, st) for i in range(2)]
            uT = [P.sb("pp_uT%d" % i, [128, 16, 128], BF16, st) for i in range(6)]
            vbf = [P.sb("pp_vb%d" % i, [128, DM], BF16, st) for i in range(6)]
            Dt = [P.sb("pp_D%d" % i, [128, 4, 4, 128], F32, st) for i in range(2)]
            Ex = [P.sb("pp_Ex%d" % i, [128, 4, 4, 128], BF16, st) for i in range(2)]
            Ft = [P.sb("pp_F%d" % i, [128, 4, 4, 128], BF16, st) for i in range(2)]
            dgt = P.sb("pp_dg", [128, 32, 128], BF16, st)
            gA = [P.sb("pp_gA%d" % i, [128, 4, 512], BF16, st) for i in range(2)]
            Gs = [P.sb("pp_Gs%d" % i, [128, 4, 128], F32, st) for i in range(2)]
            GA = [P.sb("pp_GA%d" % i, [128, 4, 128], BF16, st) for i in range(2)]
            hres = [stg[0][:, 0:512], stg[0][:, 512:1024]]
            pV = P.ps("pp_pV", [128, 4, 512], F32, st)
            pG = P.ps("pp_pG", [128, 4, 128], F32, st)
            pA = [P.ps("pp_pA%d" % i, [128, 512], F32, st) for i in range(2)]
            pT = P.ps("pp_pT", [128, 1024], BF16, st)
            istg = 0
            ich = 0
            conv_done = set()
            iD = 0
            iF = 0
            iA = 0
            iG = 0
            for tg in range(NTG):
                P.dma(xg[:], xT_d.ap()[:, :, tg * 512:(tg + 1) * 512], w=["pp_xg"])
                P.dma(S1g[:], S1_d.ap()[tg * 512:(tg + 1) * 512, :, :].rearrange("(a p) h k -> p a h k", p=128), w=["pp_S1g"])
                P.dma(THg[:], TH_d.ap()[tg * 512:(tg + 1) * 512, :].rearrange("(a p) h -> p a h", p=128), w=["pp_THg"])
                for tt in range(4):
                    for h in range(8):
                        P.op("dve", lambda e: e.tensor_scalar(out=dgt[:, tt * 8 + h, :], in0=self.ident[:], scalar1=THg[:, tt, h:h + 1], scalar2=None, op0=ALU.mult),
                             r=["ident", "pp_THg"], w=["pp_dg"])
                for eg in range(NEG_):
                    G_ = tg * NEG_ + eg
                    slots = [(G_ * 4 + c) % 6 for c in range(4)]
                    for gidx in list(range(G_ * 4, G_ * 4 + 4)) + [G_ * 4 + 4, G_ * 4 + 5]:
                        if gidx in conv_done or gidx >= NTG * NEG_ * 4:
                            continue
                        conv_done.add(gidx)
                        ci = gidx % (NEG_ * 4)
                        sl = gidx % 6
                        sb_ = istg % 2
                        istg += 1
                        P.dma(stg[sb_][:], U[ci * 128:(ci + 1) * 128, :], w=["pp_stg%d" % sb_])
                        ub = ubf[gidx % 2]
                        ubid = "pp_ubf%d" % (gidx % 2)
                        self.copy("act", ub[:, 0:1024], stg[sb_][:, 0:1024], ["pp_stg%d" % sb_], [(ubid, 0)])
                        self.copy("act", ub[:, 1024:2048], stg[sb_][:, 1024:2048], ["pp_stg%d" % sb_], [(ubid, 1)])
                        for half in range(2):
                            for kk in range(8):
                                k = half * 8 + kk
                                P.op("pe", lambda e: e.transpose(out=pT[:, kk * 128:(kk + 1) * 128], in_=ub[:, k * 128:(k + 1) * 128],
                                                                 identity=self.ident[:]), r=[(ubid, 0), (ubid, 1), "ident"], w=["pp_pT"])
                            self.copy(self.alt("act", "dve"), uT[sl][:, half * 8:(half + 1) * 8, :],
                                      pT[:].rearrange("p (a b) -> p a b", b=128), ["pp_pT"], [("pp_uT%d" % sl, half)])
                        sb_ = istg % 2
                        istg += 1
                        P.dma(stg[sb_][:], V[ci * 128:(ci + 1) * 128, :], w=["pp_stg%d" % sb_])
                        self.copy("act", vbf[sl][:, 0:1024], stg[sb_][:, 0:1024], ["pp_stg%d" % sb_], [("pp_vb%d" % sl, 0)])
                        self.copy("dve", vbf[sl][:, 1024:2048], stg[sb_][:, 1024:2048], ["pp_stg%d" % sb_], [("pp_vb%d" % sl, 1)])
                    s0b = eg % 2
                    for tt in range(4):
                        P.dma(S0e[s0b][:, tt, :, :], S0_d.ap()[tg * 512 + tt * 128:tg * 512 + (tt + 1) * 128, :, eg * 4:(eg + 1) * 4],
                              w=["pp_S0e%d" % s0b])
                    gAb = gA[eg % 2]
                    gAid = "pp_gA%d" % (eg % 2)
                    for c in range(4):
                        sl = slots[c]
                        pa = pA[iA % 2]
                        paid = "pp_pA%d" % (iA % 2)
                        iA += 1
                        for k in range(16):
                            P.op("pe", lambda e: e.matmul(pa[:, :], lhsT=uT[sl][:, k, :], rhs=xg[:, k, :], start=(k == 0), stop=(k == 15)),
                                 r=[("pp_uT%d" % sl, 0), ("pp_uT%d" % sl, 1), "pp_xg"], w=[paid])
                        P.op("act", lambda e: e.activation(out=gAb[:, c, :], in_=pa[:, :], func=AF.Gelu_apprx_tanh), r=[paid], w=[(gAid, c)])
                    for tt in range(4):
                        for hq in range(2):
                            Db = Dt[iD % 2]
                            Did = "pp_D%d" % (iD % 2)
                            Eb = Ex[iD % 2]
                            Eid = "pp_Ex%d" % (iD % 2)
                            Fb = Ft[iD % 2]
                            Fid = "pp_F%d" % (iD % 2)
                            iD += 1
                            hs = slice(hq * 4, hq * 4 + 4)
                            P.op("pool", lambda e: e.tensor_tensor(out=Db[:], in0=S1g[:, tt, hs, :].unsqueeze(2).to_broadcast([128, 4, 4, 128]),
                                                                   in1=S0e[s0b][:, tt, hs, :].unsqueeze(3).to_broadcast([128, 4, 4, 128]), op=ALU.add),
                                 r=["pp_S1g", "pp_S0e%d" % s0b], w=[Did])
                            P.op("act", lambda e: e.activation(out=Eb[:], in_=Db[:], func=AF.Exp), r=[Did], w=[Eid])
                            P.op("dve", lambda e: e.scalar_tensor_tensor(out=Fb[:], in0=Db[:], scalar=0.0, in1=Eb[:], op0=ALU.is_ge, op1=ALU.mult),
                                 r=[Did, Eid], w=[Fid])
                            for hh in range(4):
                                h = hq * 4 + hh
                                for c in range(4):
                                    P.op("pe", lambda e: e.matmul(pG[:, c, :], lhsT=Fb[:, hh, c, :], rhs=dgt[:, tt * 8 + h, :], start=(h == 0 and c == 0),
                                                                  stop=(h == 7), skip_group_check=True), r=[Fid, "pp_dg"], w=["pp_pG"])
                        g_ = iG % 2
                        iG += 1
                        self.copy("act", Gs[g_][:], pG[:], ["pp_pG"], ["pp_Gs%d" % g_])
                        P.op("dve", lambda e: e.tensor_tensor(out=GA[g_][:], in0=Gs[g_][:], in1=gAb[:, :, tt * 128:(tt + 1) * 128], op=ALU.mult),
                             r=["pp_Gs%d" % g_] + [(gAid, c) for c in range(4)], w=["pp_GA%d" % g_])
                        for c in range(4):
                            sl = slots[c]
                            for dq in range(4):
                                P.op("pe", lambda e: e.matmul(pV[:, dq, :], lhsT=GA[g_][:, c, :], rhs=vbf[sl][:, dq * 512:(dq + 1) * 512],
                                                              start=(c == 0), stop=(c == 3)),
                                     r=["pp_GA%d" % g_, ("pp_vb%d" % sl, 0), ("pp_vb%d" % sl, 1)], w=[("pp_pV", dq)])
                        for dq in range(4):
                            if eg == 0:
                                self.copy("dve", acc[:, tt, dq * 512:(dq + 1) * 512], pV[:, dq, :], [("pp_pV", dq)], [("pp_acc", tt, dq)])
                            else:
                                P.op("dve", lambda e: e.tensor_tensor(out=acc[:, tt, dq * 512:(dq + 1) * 512], in0=pV[:, dq, :],
                                                                      in1=acc[:, tt, dq * 512:(dq + 1) * 512], op=ALU.add),
                                     r=[("pp_pV", dq), ("pp_acc", tt, dq)], w=[("pp_acc", tt, dq)])
                for tt in range(4):
                    for dq in range(4):
                        i = (tt * 4 + dq) % 2
                        rows = slice(tg * 512 + tt * 128, tg * 512 + (tt + 1) * 128)
                        P.dma(hres[i], h_ap[rows, dq * 512:(dq + 1) * 512], r=["pp_stg0"], w=["pp_hr%d" % i, "pp_stg0"])
                        P.op("pool", lambda e: e.tensor_tensor(out=hres[i], in0=hres[i], in1=acc[:, tt, dq * 512:(dq + 1) * 512], op=ALU.add),
                             r=["pp_hr%d" % i, ("pp_acc", tt, dq), "pp_stg0"], w=["pp_hr%d" % i])
                        P.dma(out_ap[rows, dq * 512:(dq + 1) * 512], hres[i], r=["pp_hr%d" % i, "pp_stg0"], w=["pp_out"], q="act")
        P.barrier()


def host_consts1():
    c = {}
    s = np.arange(128)[:, None]
    t = np.arange(128)[None, :]
    c["c_tri"] = (s <= t).astype(np.float32)
    c["c_negmT"] = np.where(s <= t, 0.0, -1.0e30).astype(np.float32)
    c["c_negm"] = np.where(t <= s, 0.0, -1.0e30).astype(np.float32)
    c["c_low01"] = (t <= s).astype(np.float32)
    sel = np.zeros((128, 128), np.float32)
    sel[127, :] = 1.0
    c["c_sel127"] = sel
    c["c_onesf"] = np.ones((128, 128), np.float32)
    return c


class MK3(MK2):
    def l1_consts(self, w):
        P = self.P
        self.identf = P.sb("identf", [128, 128], F32)
        self.tri = P.sb("tri", [128, 128], F32)
        self.negmT = P.sb("negmT", [128, 128], F32)
        self.negm = P.sb("negm", [128, 128], F32)
        self.low01 = P.sb("low01", [128, 128], F32)
        self.sel127 = P.sb("sel127", [128, 128], F32)
        self.onesf = P.sb("onesf", [128, 128], F32)
        for t_, nm in ((self.identf, "c_ident"), (self.tri, "c_tri"), (self.negmT, "c_negmT"), (self.negm, "c_negm"),
                       (self.low01, "c_low01"), (self.sel127, "c_sel127"), (self.onesf, "c_onesf")):
            P.dma(t_[:], w[nm].ap(), w=[nm])
        P.barrier()

    def layer1_proj(self, h_ap, w):
        P = self.P
        L = {}
        L["zs"] = self.scratch("zs", [S_LEN, 2048], F32)
        L["xs"] = self.scratch("xs1", [S_LEN, 2048], BF16)
        L["BT"] = self.scratch("BT1", [512, S_LEN], BF16)
        L["CT"] = self.scratch("CT1", [512, S_LEN], BF16)
        L["Btok"] = self.scratch("Btok1", [S_LEN, 512], BF16)
        L["dt"] = self.scratch("dt1", [S_LEN, 32], F32)
        L["dA"] = self.scratch("dA1", [S_LEN, 32], F32)
        L["qT"] = self.scratch("qT1", [1024, S_LEN], BF16)
        L["ckvn"] = self.scratch("ckvn1", [S_LEN, 256], BF16)
        L["ckvnT"] = self.scratch("ckvnT1", [256, S_LEN], BF16)
        L["qiT"] = self.scratch("qiT1", [1024, S_LEN], BF16)
        L["kiT"] = self.scratch("kiT1", [64, S_LEN], BF16)
        L["wi"] = self.scratch("wi1", [S_LEN, 16], F32)
        self.l1 = L
        Win = w["cd_w_in"].ap()[0]
        with ExitStack() as st:
            hT = P.sb("hT1", [128, 16, S_LEN], BF16, st)
            self.loadT(h_ap, DM, hT, "hT1", gain=w["norm_mix"].ap()[1:2, :])
            L1P = os.environ.get("L1P", "zcdqikx")
            with ExitStack() as s2:
                if "z" in L1P: self.linear(hT, "hT1", 16, Win, 0, 2048, "tok", self.make_store_sink(s2, L["zs"].ap(), "tok", F32, func=AF.Silu, tag="szs"))
            with ExitStack() as s2:
                cw = P.sb("cv_w", [128, 4, 24], F32, s2)
                cb = P.sb("cv_b", [128, 24], F32, s2)
                for c4 in range(0, 24, 4):
                    for k in range(4):
                        P.dma(cw[:, k, c4:c4 + 4], w["cd_conv_w"].ap()[0, k, c4 * 128:(c4 + 4) * 128].rearrange("(c p) -> p c", p=128), w=["cv_w"],
                              allow_slow_non_contiguous=True)
                    P.dma(cb[:, c4:c4 + 4], w["cd_conv_b"].ap()[0, c4 * 128:(c4 + 4) * 128].rearrange("(c p) -> p c", p=128), w=["cv_b"],
                          allow_slow_non_contiguous=True)
                rb = [P.sb("cv_r%d" % i, [128, 3 + S_LEN], F32, s2) for i in range(2)]
                ac = [P.sb("cv_a%d" % i, [128, S_LEN], F32, s2) for i in range(2)]
                ob = [P.sb("cv_o%d" % i, [128, S_LEN], BF16, s2) for i in range(2)]
                tb = [P.sb("cv_t%d" % i, [128, 512], BF16, s2) for i in range(2)]
                ptc = [P.ps("cv_p%d" % i, [128, 512], BF16, s2) for i in range(2)]
                for i in range(2):
                    P.op("pool", lambda e: e.memset(rb[i][:, 0:3], 0.0), w=["cv_r%d" % i])
                itc = [0]

                def csink(chunk, tg, ps, pid):
                    i = chunk % 2
                    rid = "cv_r%d" % i
                    self.copy(self.alt("act", "dve"), rb[i][:, 3 + tg * 512:3 + (tg + 1) * 512], ps[:, :], [pid], [(rid, tg)])
                    if tg != 3:
                        return
                    rall = [rid] + [(rid, k) for k in range(4)]
                    aid = "cv_a%d" % i
                    P.op("dve", lambda e: e.tensor_scalar(out=ac[i][:], in0=rb[i][:, 3:3 + S_LEN], scalar1=cw[:, 3, chunk:chunk + 1], scalar2=None, op0=ALU.mult),
                         r=rall + ["cv_w"], w=[aid])
                    for k in range(3):
                        P.op("dve", lambda e: e.scalar_tensor_tensor(out=ac[i][:], in0=rb[i][:, k:k + S_LEN], scalar=cw[:, k, chunk:chunk + 1], in1=ac[i][:],
                                                                     op0=ALU.mult, op1=ALU.add), r=rall + ["cv_w", aid], w=[aid])
                    oid = "cv_o%d" % i
                    P.op("act", lambda e: e.activation(out=ob[i][:], in_=ac[i][:], func=AF.Silu, bias=cb[:, chunk:chunk + 1]), r=[aid, "cv_b"], w=[oid])
                    if chunk >= 16:
                        dst = L["BT"] if chunk < 20 else L["CT"]
                        r0 = (chunk - 16) % 4
                        P.dma(dst.ap()[r0 * 128:(r0 + 1) * 128, :], ob[i][:], r=[oid], w=["cv_d"], q="act")
                    if chunk < 20:
                        for t4 in range(4):
                            j = itc[0] % 2
                            itc[0] += 1
                            for kk in range(4):
                                t = t4 * 4 + kk
                                P.op("pe", lambda e: e.transpose(out=ptc[j][:, kk * 128:(kk + 1) * 128], in_=ob[i][:, t * 128:(t + 1) * 128],
                                                                 identity=self.ident[:]), r=[oid, "ident"], w=["cv_p%d" % j])
                            self.copy(self.alt("act", "dve"), tb[j][:], ptc[j][:], ["cv_p%d" % j], ["cv_t%d" % j])
                            if chunk < 16:
                                dd = L["xs"].ap()[t4 * 512:(t4 + 1) * 512, chunk * 128:(chunk + 1) * 128]
                            else:
                                dd = L["Btok"].ap()[t4 * 512:(t4 + 1) * 512, (chunk - 16) * 128:(chunk - 15) * 128]
                            P.dma(dd.rearrange("(a p) c -> p a c", p=128), tb[j][:].rearrange("p (a c) -> p a c", c=128), r=["cv_t%d" % j], w=["cv_d2"], q="act")
                if "c" in L1P: self.linear(hT, "hT1", 16, Win, 2048, 3072, "feat", csink, slab=256)
            with ExitStack() as s2:
                dbb = P.sb("dt_b", [128, 32], F32, s2)
                aa = P.sb("dt_a", [128, 32], F32, s2)
                one = P.sb("dt_1", [128, 1], F32, s2)
                xx = [P.sb("dt_x%d" % i, [128, 32], F32, s2) for i in range(2)]
                yy = [P.sb("dt_y%d" % i, [128, 32], F32, s2) for i in range(2)]
                zz = [P.sb("dt_z%d" % i, [128, 32], F32, s2) for i in range(2)]
                P.dma(dbb[:], w["cd_dt_bias"].ap()[0:1, :].partition_broadcast(128), w=["dt_b"])
                P.dma(aa[:], w["cd_a_log"].ap()[0:1, :].partition_broadcast(128), w=["dt_a"])
                P.op("act", lambda e: e.activation(out=aa[:], in_=aa[:], func=AF.Exp), r=["dt_a"], w=["dt_a"])
                P.op("pool", lambda e: e.memset(one[:], 1.0), w=["dt_1"])

                def dsink(t, coff, n, ps, pid):
                    i = t % 2
                    P.op("dve", lambda e: e.tensor_tensor(out=xx[i][:], in0=ps[:, 0:32], in1=dbb[:], op=ALU.add), r=[pid, "dt_b"], w=["dt_x%d" % i])
                    P.op("act", lambda e: e.activation(out=yy[i][:], in_=xx[i][:], func=AF.Abs), r=["dt_x%d" % i], w=["dt_y%d" % i])
                    P.op("act", lambda e: e.activation(out=yy[i][:], in_=yy[i][:], func=AF.Exp, scale=-1.0), r=["dt_y%d" % i], w=["dt_y%d" % i])
                    P.op("act", lambda e: e.activation(out=yy[i][:], in_=yy[i][:], func=AF.Ln, bias=one[:, 0:1]), r=["dt_y%d" % i, "dt_1"], w=["dt_y%d" % i])
                    P.op("dve", lambda e: e.scalar_tensor_tensor(out=xx[i][:], in0=xx[i][:], scalar=0.0, in1=yy[i][:], op0=ALU.max, op1=ALU.add),
                         r=["dt_x%d" % i, "dt_y%d" % i], w=["dt_x%d" % i])
                    P.dma(L["dt"].ap()[t * 128:(t + 1) * 128, :], xx[i][:], r=["dt_x%d" % i], w=["dt_d"], q="act")
                    P.op("dve", lambda e: e.scalar_tensor_tensor(out=zz[i][:], in0=xx[i][:], scalar=-1.0, in1=aa[:], op0=ALU.mult, op1=ALU.mult),
                         r=["dt_x%d" % i, "dt_a"], w=["dt_z%d" % i])
                    P.dma(L["dA"].ap()[t * 128:(t + 1) * 128, :], zz[i][:], r=["dt_z%d" % i], w=["dA_d"], q="act")
                if "d" in L1P: self.linear(hT, "hT1", 16, Win, 5120, 32, "tok", dsink, slab=32)
            with ExitStack() as s2:
                if "q" in L1P: self.linear(hT, "hT1", 16, Win, 5152, 1024, "feat", self.make_store_sink(s2, L["qT"].ap(), "feat", BF16, tag="sq1"))
            with ExitStack() as s2:
                if "i" in L1P: self.linear(hT, "hT1", 16, Win, 6432, 1024, "feat", self.make_store_sink(s2, L["qiT"].ap(), "feat", BF16, tag="sqi"))
            with ExitStack() as s2:
                kg = P.sb("kv_g", [128, 256], F32, s2)
                P.dma(kg[:], w["cd_kv_norm"].ap()[0:1, :].partition_broadcast(128), w=["kv_g"])
                jk = P.sb("kv_j", [128, 256], F32, s2)
                ss = P.sb("kv_s", [128, 4], F32, s2)
                kn = [P.sb("kv_n%d" % i, [128, 256], BF16, s2) for i in range(2)]
                kt = [P.sb("kv_t%d" % i, [128, 256], BF16, s2) for i in range(2)]
                pk = [P.ps("kv_p%d" % i, [128, 256], BF16, s2) for i in range(2)]

                def ksink(t, coff, n, ps, pid):
                    i = t % 2
                    P.op("act", lambda e: e.activation(out=jk[:], in_=ps[:, 0:256], func=AF.Square, accum_out=ss[:, i:i + 1]), r=[pid], w=["kv_j", "kv_s%d" % i])
                    self.rstd_from_ss(ss[:, 2 + i:3 + i], ss[:, i:i + 1], 256, "kv_r%d" % i, "kv_s%d" % i)
                    P.op("act", lambda e: e.activation(out=jk[:], in_=ps[:, 0:256], func=AF.Copy, scale=ss[:, 2 + i:3 + i]), r=[pid, "kv_r%d" % i, "kv_j"], w=["kv_j"])
                    P.op("dve", lambda e: e.tensor_tensor(out=kn[i][:], in0=jk[:], in1=kg[:], op=ALU.mult), r=["kv_j", "kv_g"], w=["kv_n%d" % i])
                    P.dma(L["ckvn"].ap()[t * 128:(t + 1) * 128, :], kn[i][:], r=["kv_n%d" % i], w=["ckvn_d"], q="act")
                    for c in range(2):
                        P.op("pe", lambda e: e.transpose(out=pk[i][:, c * 128:(c + 1) * 128], in_=kn[i][:, c * 128:(c + 1) * 128], identity=self.ident[:]),
                             r=["kv_n%d" % i, "ident"], w=["kv_p%d" % i])
                    self.copy("dve", kt[i][:], pk[i][:], ["kv_p%d" % i], ["kv_t%d" % i])
                    P.dma(L["ckvnT"].ap()[:, t * 128:(t + 1) * 128].rearrange("(c p) t -> p c t", p=128), kt[i][:].rearrange("p (c t) -> p c t", t=128),
                          r=["kv_t%d" % i], w=["ckvnT_d"], q="act")
                if "k" in L1P: self.linear(hT, "hT1", 16, Win, 6176, 256, "tok", ksink, slab=256)
            with ExitStack() as s2:
                ki = [P.sb("ki_n%d" % i, [128, 128], BF16, s2) for i in range(2)]
                kit = [P.sb("ki_t%d" % i, [128, 128], BF16, s2) for i in range(2)]
                wi = [P.sb("ki_w%d" % i, [128, 16], F32, s2) for i in range(2)]
                pki = [P.ps("ki_p%d" % i, [128, 128], BF16, s2) for i in range(2)]
                for i in range(2):
                    P.op("pool", lambda e: e.memset(ki[i][:], 0.0), w=["ki_n%d" % i])

                def isink(t, coff, n, ps, pid):
                    i = t % 2
                    self.copy("act", ki[i][:, 0:64], ps[:, 0:64], [pid], ["ki_n%d" % i])
                    self.copy("dve", wi[i][:], ps[:, 64:80], [pid], ["ki_w%d" % i])
                    P.dma(L["wi"].ap()[t * 128:(t + 1) * 128, :], wi[i][:], r=["ki_w%d" % i], w=["wi_d"], q="act")
                    P.op("pe", lambda e: e.transpose(out=pki[i][:], in_=ki[i][:], identity=self.ident[:]), r=["ki_n%d" % i, "ident"], w=["ki_p%d" % i])
                    self.copy("dve", kit[i][:], pki[i][:], ["ki_p%d" % i], ["ki_t%d" % i])
                    P.dma(L["kiT"].ap()[:, t * 128:(t + 1) * 128], kit[i][0:64, :], r=["ki_t%d" % i], w=["kiT_d"], q="act")
                if "x" in L1P: self.linear(hT, "hT1", 16, Win, 7456, 80, "tok", isink, slab=80)
        P.barrier()


class MK4(MK3):
    def mamba_ssd(self, w, mixed_ap):
        P = self.P
        L = self.l1
        with ExitStack() as st:
            CT = P.sb("ss_CT", [128, 4, S_LEN], BF16, st)
            BT = P.sb("ss_BT", [128, 4, S_LEN], BF16, st)
            for g in range(4):
                P.dma(CT[:, g, :], L["CT"].ap()[g * 128:(g + 1) * 128, :], w=["ss_CT"])
                P.dma(BT[:, g, :], L["BT"].ap()[g * 128:(g + 1) * 128, :], w=["ss_BT"])
            Dbc = P.sb("ss_D", [128, 32], F32, st)
            P.dma(Dbc[:], w["cd_d_skip"].ap()[0:1, :].partition_broadcast(128), w=["ss_D"])
            gn = P.sb("ss_gn", [128, 2048], F32, st)
            P.dma(gn[:], w["cd_ssm_norm"].ap()[0:1, :].partition_broadcast(128), w=["ss_gn"])
            state = P.sb("ss_state", [128, 32, 64], F32, st)
            stbf = P.sb("ss_stbf", [128, 32, 64], BF16, st)
            xs = [P.sb("ss_xs%d" % i, [128, 32, 64], BF16, st) for i in range(2)]
            Bk = [P.sb("ss_Bk%d" % i, [128, 512], BF16, st) for i in range(2)]
            zt = [P.sb("ss_z%d" % i, [128, 2048], F32, st) for i in range(2)]
            dtt = [P.sb("ss_dt%d" % i, [128, 32], F32, st) for i in range(2)]
            dAt = [P.sb("ss_dA%d" % i, [128, 32], F32, st) for i in range(2)]
            cs = P.sb("ss_cs", [128, 32], F32, st)
            ncs = P.sb("ss_ncs", [128, 32], F32, st)
            ecs = P.sb("ss_ecs", [128, 32], F32, st)
            clb = P.sb("ss_clb", [128, 32], F32, st)
            dec = P.sb("ss_dec", [128, 32], F32, st)
            ela = P.sb("ss_ela", [128, 32], F32, st)
            xdt = P.sb("ss_xdt", [128, 32, 64], BF16, st)
            xw = P.sb("ss_xw", [128, 32, 64], BF16, st)
            Y = P.sb("ss_Y", [128, 32, 64], F32, st)
            Dg = [P.sb("ss_Dg%d" % i, [128, 128], F32, st) for i in range(2)]
            sg = [P.sb("ss_sg%d" % i, [128, 128], F32, st) for i in range(2)]
            Lt = [P.sb("ss_L%d" % i, [128, 128], F32, st) for i in range(2)]
            Gs = P.sb("ss_Gs", [128, 128], F32, st)
            Wt = [P.sb("ss_W%d" % i, [128, 128], BF16, st) for i in range(2)]
            y1 = [P.sb("ss_y1%d" % i, [128, 64], F32, st) for i in range(2)]
            ssq = P.sb("ss_ssq", [128, 8], F32, st)
            jk = P.sb("ss_jk", [128, 512], F32, st)
            yo = [P.sb("ss_yo%d" % i, [128, 2048], BF16, st) for i in range(2)]
            bk = [P.ps("ss_bk%d" % i, [128, 512], F32, st) for i in range(5)]
            pcs = bk[0][:, 0:64]
            pG = bk[0][:, 128:256]
            pcb = [bk[1][:, 0:128], bk[2][:, 0:128]]
            pyi = [bk[3][:, 0:64], bk[4][:, 0:64]]
            pys = [bk[3][:, 128:192], bk[4][:, 128:192]]
            pdS = P.ps("ss_pdS", [128, 8, 64], F32, st)
            P.op("pool", lambda e: e.memset(state[:], 0.0), w=["ss_state"])
            P.op("pool", lambda e: e.memset(stbf[:], 0.0), w=["ss_stbf"])
            it = 0
            for ck in range(NT):
                b = ck % 2
                rows = slice(ck * 128, (ck + 1) * 128)
                P.dma(xs[b][:].rearrange("p h d -> p (h d)"), L["xs"].ap()[rows, :], w=["ss_xs%d" % b])
                P.dma(Bk[b][:], L["Btok"].ap()[rows, :], w=["ss_Bk%d" % b])
                P.dma(zt[b][:], L["zs"].ap()[rows, :], w=["ss_z%d" % b])
                P.dma(dtt[b][:], L["dt"].ap()[rows, :], w=["ss_dt%d" % b])
                P.dma(dAt[b][:], L["dA"].ap()[rows, :], w=["ss_dA%d" % b])
                P.op("pe", lambda e: e.matmul(pcs[:, 0:32], lhsT=self.tri[:], rhs=dAt[b][:], start=True, stop=True, skip_group_check=True), r=["c_tri", "ss_dA%d" % b], w=["ss_bk0"])
                self.copy("dve", cs[:], pcs[:, 0:32], ["ss_bk0"], ["ss_cs"])
                P.op("pe", lambda e: e.matmul(pcs[:, 32:64], lhsT=self.sel127[:], rhs=cs[:], start=True, stop=True, skip_group_check=True),
                     r=["c_sel127", "ss_cs"], w=["ss_bk0"])
                self.copy("dve", clb[:], pcs[:, 32:64], ["ss_bk0"], ["ss_clb"])
                P.op("dve", lambda e: e.tensor_scalar(out=ncs[:], in0=cs[:], scalar1=-1.0, scalar2=None, op0=ALU.mult), r=["ss_cs"], w=["ss_ncs"])
                P.op("act", lambda e: e.activation(out=ecs[:], in_=cs[:], func=AF.Exp), r=["ss_cs"], w=["ss_ecs"])
                P.op("dve", lambda e: e.tensor_tensor(out=dec[:], in0=clb[:], in1=cs[:], op=ALU.subtract), r=["ss_clb", "ss_cs"], w=["ss_dec"])
                P.op("act", lambda e: e.activation(out=dec[:], in_=dec[:], func=AF.Exp), r=["ss_dec"], w=["ss_dec"])
                P.op("act", lambda e: e.activation(out=ela[:], in_=clb[:], func=AF.Exp), r=["ss_clb"], w=["ss_ela"])
                P.op("dve", lambda e: e.tensor_tensor(out=xdt[:], in0=xs[b][:], in1=dtt[b][:].unsqueeze(2).to_broadcast([128, 32, 64]), op=ALU.mult),
                     r=["ss_xs%d" % b, "ss_dt%d" % b], w=["ss_xdt"])
                P.op("pool", lambda e: e.tensor_tensor(out=xw[:], in0=xdt[:], in1=dec[:].unsqueeze(2).to_broadcast([128, 32, 64]), op=ALU.mult),
                     r=["ss_xdt", "ss_dec"], w=["ss_xw"])
                for g in range(4):
                    P.op("pe", lambda e: e.matmul(pG, lhsT=BT[:, g, ck * 128:(ck + 1) * 128], rhs=CT[:, g, ck * 128:(ck + 1) * 128], start=True, stop=True, skip_group_check=True),
                         r=["ss_BT", "ss_CT"], w=["ss_bk0"])
                    self.copy("act", Gs[:], pG, ["ss_bk0"], ["ss_Gs"])
                    for hh in range(8):
                        h = g * 8 + hh
                        i = it % 2
                        it += 1
                        P.op("dve", lambda e: e.tensor_scalar(out=Dg[i][:], in0=self.identf[:], scalar1=cs[:, h:h + 1], scalar2=None, op0=ALU.mult),
                             r=["c_ident", "ss_cs"], w=["ss_Dg%d" % i])
                        P.op("pe", lambda e: e.matmul(pcb[i], lhsT=self.onesf[:], rhs=Dg[i][:], start=True, stop=True, skip_group_check=True), r=["c_onesf", "ss_Dg%d" % i], w=["ss_bkb%d" % i])
                        P.op("dve", lambda e: e.tensor_tensor(out=sg[i][:], in0=pcb[i], in1=self.negmT[:], op=ALU.add), r=["ss_bkb%d" % i, "c_negmT"], w=["ss_sg%d" % i])
                        P.op("act", lambda e: e.activation(out=Lt[i][:], in_=sg[i][:], func=AF.Exp, bias=ncs[:, h:h + 1]), r=["ss_sg%d" % i, "ss_ncs"], w=["ss_L%d" % i])
                        P.op("pool", lambda e: e.tensor_tensor(out=Wt[i][:], in0=Gs[:], in1=Lt[i][:], op=ALU.mult), r=["ss_Gs", "ss_L%d" % i], w=["ss_W%d" % i])
                        P.op("pe", lambda e: e.matmul(pyi[i], lhsT=Wt[i][:], rhs=xdt[:, h, :], start=True, stop=True, skip_group_check=True), r=["ss_W%d" % i, "ss_xdt"], w=["ss_bky%d" % i])
                        if ck > 0:
                            P.op("pe", lambda e: e.matmul(pys[i], lhsT=CT[:, g, ck * 128:(ck + 1) * 128], rhs=stbf[:, h, :], start=True, stop=True, skip_group_check=True),
                                 r=["ss_CT", ("ss_stbf", h)], w=["ss_bky%d" % i])
                            P.op("dve", lambda e: e.tensor_scalar(out=y1[i][:], in0=pys[i], scalar1=ecs[:, h:h + 1], scalar2=None, op0=ALU.mult),
                                 r=["ss_bky%d" % i, "ss_ecs"], w=["ss_y1%d" % i])
                            P.op("dve", lambda e: e.tensor_tensor(out=Y[:, h, :], in0=pyi[i], in1=y1[i][:], op=ALU.add),
                                 r=["ss_bky%d" % i, "ss_y1%d" % i], w=[("ss_Y", h)])
                        else:
                            self.copy("dve", Y[:, h, :], pyi[i], ["ss_bky%d" % i], [("ss_Y", h)])
                    for hh in range(8):
                        h = g * 8 + hh
                        P.op("pe", lambda e: e.matmul(pdS[:, hh, :], lhsT=Bk[b][:, g * 128:(g + 1) * 128], rhs=xw[:, h, :], start=True, stop=True,
                                                      skip_group_check=True), r=["ss_Bk%d" % b, "ss_xw"], w=["ss_pdS"])
                    for hh in range(8):
                        h = g * 8 + hh
                        P.op("dve", lambda e: e.scalar_tensor_tensor(out=state[:, h, :], in0=state[:, h, :], scalar=ela[:, h:h + 1], in1=pdS[:, hh, :],
                                                                     op0=ALU.mult, op1=ALU.add), r=["ss_pdS", "ss_ela", ("ss_state", h), "ss_state"], w=[("ss_state", h)])
                        self.copy("act", stbf[:, h, :], state[:, h, :], [("ss_state", h)], [("ss_stbf", h)])
                Yall = [("ss_Y", h) for h in range(32)]
                P.op("pool", lambda e: e.tensor_tensor(out=xw[:], in0=xs[b][:], in1=Dbc[:].unsqueeze(2).to_broadcast([128, 32, 64]), op=ALU.mult),
                     r=["ss_xs%d" % b, "ss_D", "ss_xw"], w=["ss_xw"])
                P.op("dve", lambda e: e.tensor_tensor(out=Y[:], in0=Y[:], in1=xw[:], op=ALU.add), r=Yall + ["ss_xw"], w=Yall)
                Yf = Y[:].rearrange("p h d -> p (h d)")
                P.op("dve", lambda e: e.tensor_tensor(out=Yf, in0=Yf, in1=zt[b][:], op=ALU.mult), r=Yall + ["ss_z%d" % b], w=Yall)
                for g in range(4):
                    P.op("act", lambda e: e.activation(out=jk[:], in_=Yf[:, g * 512:(g + 1) * 512], func=AF.Square, accum_out=ssq[:, g:g + 1]),
                         r=Yall, w=["ss_jk", ("ss_ssq", g)])
                    self.rstd_from_ss(ssq[:, 4 + g:5 + g], ssq[:, g:g + 1], 512, ("ss_rs", g), ("ss_ssq", g))
                    P.op("dve", lambda e: e.scalar_tensor_tensor(out=yo[b][:, g * 512:(g + 1) * 512], in0=Yf[:, g * 512:(g + 1) * 512], scalar=ssq[:, 4 + g:5 + g],
                                                                 in1=gn[:, g * 512:(g + 1) * 512], op0=ALU.mult, op1=ALU.mult),
                         r=Yall + [("ss_rs", g), "ss_gn"], w=[("ss_yo%d" % b, g)])
                P.dma(mixed_ap[rows, 0:2048], yo[b][:], r=[("ss_yo%d" % b, g) for g in range(4)], w=["ss_out"], q="act")
        P.barrier()


class MK5(MK4):
    def dsa_select(self):
        P = self.P
        L = self.l1
        MT_d = self.scratch("dsa_MT", [S_LEN, S_LEN], BF16)
        self.MT_d = MT_d
        NEG = -1.0e30
        with ExitStack() as st:
            qi = P.sb("dx_qi", [128, 8, S_LEN], BF16, st)
            ki = P.sb("dx_ki", [128, S_LEN], BF16, st)
            wi = P.sb("dx_wi", [128, NT, 16], F32, st)
            P.dma(qi[:], L["qiT"].ap().rearrange("(c p) t -> p c t", p=128), w=["dx_qi"])
            P.dma(ki[0:64, :], L["kiT"].ap(), w=["dx_ki"])
            P.dma(ki[64:128, :], L["kiT"].ap(), w=["dx_ki"])
            P.dma(wi[:], L["wi"].ap().rearrange("(a p) h -> p a h", p=128), w=["dx_wi"])
            acc = P.sb("dx_acc", [128, S_LEN], F32, st)
            tmp = [P.sb("dx_tmp%d" % i, [128, 512], F32, st) for i in range(2)]
            wk = [P.sb("dx_wk%d" % i, [128, S_LEN], F32, st) for i in range(2)]
            m8 = P.sb("dx_m8", [128, 8], F32, st)
            Mb = P.sb("dx_M", [128, S_LEN], BF16, st)
            mts = [P.sb("dx_mt%d" % i, [128, 512], BF16, st) for i in range(2)]
            pss = [P.ps("dx_ps%d" % i, [128, 512], F32, st) for i in range(3)]
            ptm = [P.ps("dx_pt%d" % i, [128, 512], BF16, st) for i in range(2)]
            ip = 0
            itm = 0
            for i in range(NT):
                W = (i + 1) * 128
                nsg = (W + 511) // 512
                for h in range(16):
                    pb = 64 * (h % 2)
                    for sg in range(nsg):
                        n = min(512, W - sg * 512)
                        ps = pss[ip % 3]
                        pid = "dx_ps%d" % (ip % 3)
                        ip += 1
                        P.op("pe", lambda e: e.matmul(ps[:, 0:n], lhsT=qi[pb:pb + 64, h // 2, i * 128:(i + 1) * 128], rhs=ki[pb:pb + 64, sg * 512:sg * 512 + n],
                                                      start=True, stop=True), r=["dx_qi", "dx_ki"], w=[pid])
                        aid = ("dx_acc", sg)
                        if h == 0:
                            P.op("dve", lambda e: e.tensor_scalar(out=acc[:, sg * 512:sg * 512 + n], in0=ps[:, 0:n], scalar1=0.0, scalar2=wi[:, i, h:h + 1],
                                                                  op0=ALU.max, op1=ALU.mult), r=[pid, "dx_wi"], w=[aid])
                        else:
                            tb = tmp[itm % 2]
                            tid = "dx_tmp%d" % (itm % 2)
                            itm += 1
                            P.op("dve", lambda e: e.tensor_scalar(out=tb[:, 0:n], in0=ps[:, 0:n], scalar1=0.0, scalar2=wi[:, i, h:h + 1],
                                                                  op0=ALU.max, op1=ALU.mult), r=[pid, "dx_wi"], w=[tid])
                            P.op("pool", lambda e: e.tensor_tensor(out=acc[:, sg * 512:sg * 512 + n], in0=acc[:, sg * 512:sg * 512 + n], in1=tb[:, 0:n], op=ALU.add),
                                 r=[tid, aid], w=[aid])
                aall = [("dx_acc", sg) for sg in range(4)]
                P.op("pool", lambda e: e.tensor_tensor(out=acc[:, i * 128:W], in0=acc[:, i * 128:W], in1=self.negm[:], op=ALU.add), r=aall + ["c_negm"], w=aall)
                if W > 256:
                    cur = acc
                    cid = aall
                    for rd in range(32):
                        P.op("dve", lambda e: e.max(out=m8[:], in_=cur[:, 0:W]), r=cid, w=["dx_m8"])
                        if rd < 31:
                            nxt = wk[rd % 2]
                            nid = ["dx_wk%d" % (rd % 2)]
                            P.op("dve", lambda e: e.match_replace(out=nxt[:, 0:W], in_to_replace=m8[:], in_values=cur[:, 0:W], imm_value=NEG),
                                 r=cid + ["dx_m8"], w=nid)
                            cur, cid = nxt, nid
                    P.op("dve", lambda e: e.tensor_scalar(out=Mb[:, 0:W], in0=acc[:, 0:W], scalar1=m8[:, 7:8], scalar2=None, op0=ALU.is_ge),
                         r=aall + ["dx_m8"], w=["dx_M"])
                    P.op("pool", lambda e: e.tensor_tensor(out=Mb[:, i * 128:W], in0=Mb[:, i * 128:W], in1=self.low01[:], op=ALU.mult), r=["dx_M", "c_low01"], w=["dx_M"])
                else:
                    if i > 0:
                        P.op("pool", lambda e: e.memset(Mb[:, 0:i * 128], 1.0), r=aall, w=["dx_M"])
                    self.copy("pool", Mb[:, i * 128:W], self.low01[:], ["c_low01"] + aall, ["dx_M"])
                for j4 in range((i + 4) // 4):
                    nj = min(4, i + 1 - j4 * 4)
                    pt = ptm[j4 % 2]
                    ptid = "dx_pt%d" % (j4 % 2)
                    for jj in range(nj):
                        j = j4 * 4 + jj
                        P.op("pe", lambda e: e.transpose(out=pt[:, jj * 128:(jj + 1) * 128], in_=Mb[:, j * 128:(j + 1) * 128], identity=self.ident[:]),
                             r=["dx_M", "ident"], w=[ptid])
                    ms = mts[j4 % 2]
                    msid = "dx_mt%d" % (j4 % 2)
                    self.copy(self.alt("act", "dve"), ms[:, 0:nj * 128], pt[:, 0:nj * 128], [ptid], [msid])
                    P.dma(MT_d.ap()[j4 * 512:j4 * 512 + nj * 128, i * 128:(i + 1) * 128].rearrange("(a p) c -> p a c", p=128),
                          ms[:, 0:nj * 128].rearrange("p (a c) -> p a c", c=128), r=[msid], w=["dx_MT_d"], q="act")
        P.barrier()

    def dsa_attend(self, w, mixed_ap):
        P = self.P
        L = self.l1
        MT_d = self.MT_d
        SC = 128.0 ** -0.5
        with ExitStack() as st:
            wk32 = P.sb("da_wk32", [128, 8, 256], F32, st)
            wv32 = P.sb("da_wv32", [128, 8, 2, 128], F32, st)
            wuk = P.sb("da_wuk", [128, 8, 256], BF16, st)
            wuv = P.sb("da_wuv", [128, 8, 2, 128], BF16, st)
            P.dma(wk32[:], w["cd_w_uk"].ap()[0].rearrange("h d c -> d h c"), w=["da_wk32"])
            for h in range(8):
                P.dma(wv32[:, h, :, :], w["cd_w_uv"].ap()[0, h].rearrange("(cc p) d -> p cc d", p=128), w=["da_wv32"])
            self.copy("dve", wuk[:], wk32[:], ["da_wk32"], ["da_wuk"])
            self.copy("pool", wuv[:], wv32[:], ["da_wv32"], ["da_wuv"])
            KT = P.sb("da_KT", [128, 2, S_LEN], BF16, st)
            P.dma(KT[:], L["ckvnT"].ap().rearrange("(cc p) t -> p cc t", p=128), w=["da_KT"])
            Vt = P.sb("da_V", [128, NT, 257], BF16, st)
            P.op("pool", lambda e: e.memset(Vt[:, :, 256:257], 1.0), w=["da_V"])
            P.dma(Vt[:, :, 0:256], L["ckvn"].ap().rearrange("(j p) c -> p j c", p=128), w=["da_V"])
            qTh = [P.sb("da_q%d" % i, [128, S_LEN], BF16, st) for i in range(2)]
            QA = [P.sb("da_QA%d" % i, [128, 2, S_LEN], BF16, st) for i in range(2)]
            PTs = [P.sb("da_pt%d" % i, [128, 512], BF16, st) for i in range(3)]
            exn = [P.sb("da_exn%d" % i, [128, 256], F32, st) for i in range(2)]
            exf = [P.sb("da_exf%d" % i, [128, 512], BF16, st) for i in range(2)]
            MTb = [P.sb("da_mt%d" % i, [128, 512], BF16, st) for i in range(3)]
            sm = P.sb("da_sm", [128, 4], F32, st)
            ctx = [P.sb("da_ctx%d" % i, [128, 256], BF16, st) for i in range(2)]
            ctT = [P.sb("da_ctT%d" % i, [128, 256], BF16, st) for i in range(2)]
            og = [P.sb("da_og%d" % i, [128, 128], BF16, st) for i in range(2)]
            pS = [P.ps("da_ps%d" % i, [128, 512], F32, st) for i in range(2)]
            pO = P.ps("da_po", [128, 4, 512], F32, st)
            pM = P.ps("da_pm", [128, 512], F32, st)
            pT = P.ps("da_pT", [128, 1024], BF16, st)
            ipt = 0
            isx = 0
            imt = 0
            ifin = 0
            for h in range(8):
                hb = h % 2
                P.dma(qTh[hb][:], L["qT"].ap()[h * 128:(h + 1) * 128, :], w=["da_q%d" % hb])
                for cc in range(2):
                    for tg in range(4):
                        P.op("pe", lambda e: e.matmul(pM[:, :], lhsT=wuk[:, h, cc * 128:(cc + 1) * 128], rhs=qTh[hb][:, tg * 512:(tg + 1) * 512], start=True, stop=True),
                             r=["da_wuk", "da_q%d" % hb], w=["da_pm"])
                        self.copy(self.alt("act", "dve"), QA[hb][:, cc, tg * 512:(tg + 1) * 512], pM[:, :], ["da_pm"], ["da_QA%d" % hb])
                for g in range(4):
                    for j in range(4 * g + 4):
                        c0 = max(0, j - 4 * g)
                        ps = pS[isx % 2]
                        psid = "da_ps%d" % (isx % 2)
                        isx += 1
                        for cc in range(2):
                            P.op("pe", lambda e: e.matmul(ps[:, c0 * 128:512], lhsT=KT[:, cc, j * 128:(j + 1) * 128],
                                                          rhs=QA[hb][:, cc, g * 512 + c0 * 128:(g + 1) * 512], start=(cc == 0), stop=(cc == 1)),
                                 r=["da_KT", "da_QA%d" % hb], w=[psid])
                        mt = MTb[imt % 3]
                        mtid = "da_mt%d" % (imt % 3)
                        imt += 1
                        P.dma(mt[:, c0 * 128:512], MT_d.ap()[j * 128:(j + 1) * 128, g * 512 + c0 * 128:(g + 1) * 512], w=[mtid])
                        pt = PTs[ipt % 3]
                        ptid = "da_pt%d" % (ipt % 3)
                        ipt += 1
                        near_lo = c0 * 128
                        if j >= 4 * g:
                            near_hi = min(512, near_lo + 256)
                            eb_lo = 0
                        else:
                            near_hi = 128 if j == 4 * g - 1 else 0
                            eb_lo = 128
                        nw = near_hi - near_lo if near_hi > near_lo else 0
                        if nw > 0:
                            exb = exn[ipt % 2]
                            exid = "da_exn%d" % (ipt % 2)
                            P.op("act", lambda e: e.activation(out=exb[:, 0:nw], in_=ps[:, near_lo:near_hi], func=AF.Exp, scale=SC), r=[psid], w=[exid])
                            P.op("dve", lambda e: e.tensor_tensor(out=exb[:, 0:nw], in0=exb[:, 0:nw], in1=self.EB[:, h, eb_lo:eb_lo + nw], op=ALU.mult),
                                 r=[exid, ("EB", h)], w=[exid])
                            P.op("pool", lambda e: e.tensor_tensor(out=pt[:, near_lo:near_hi], in0=exb[:, 0:nw], in1=mt[:, near_lo:near_hi], op=ALU.mult),
                                 r=[exid, mtid], w=[(ptid, 0)])
                        far_lo = max(near_hi, near_lo)
                        if far_lo < 512:
                            efb = exf[ipt % 2]
                            efid = "da_exf%d" % (ipt % 2)
                            P.op("act", lambda e: e.activation(out=efb[:, far_lo:512], in_=ps[:, far_lo:512], func=AF.Exp,
                                                               bias=self.tbl_bc[:, 31 * 8 + h:31 * 8 + h + 1], scale=SC), r=[psid, "tbl_bc"], w=[efid])
                            P.op(self.alt("dve", "pool"), lambda e: e.tensor_tensor(out=pt[:, far_lo:512], in0=efb[:, far_lo:512], in1=mt[:, far_lo:512], op=ALU.mult),
                                 r=[efid, mtid], w=[(ptid, 1)])
                        for il in range(c0, 4):
                            P.op("pe", lambda e: e.matmul(pO[:, il, 0:257], lhsT=pt[:, il * 128:(il + 1) * 128], rhs=Vt[:, j, :],
                                                          start=(j == 0), stop=(j == 4 * g + il)), r=[(ptid, 0), (ptid, 1), "da_V"], w=[("da_po", il)])
                    for il in range(4):
                        qb = 4 * g + il
                        f = ifin % 2
                        ifin += 1
                        P.op("dve", lambda e: e.reciprocal(out=sm[:, f:f + 1], in_=pO[:, il, 256:257]), r=[("da_po", il)], w=[("da_sm", f)])
                        P.op("act", lambda e: e.activation(out=ctx[f][:], in_=pO[:, il, 0:256], func=AF.Copy, scale=sm[:, f:f + 1]),
                             r=[("da_po", il), ("da_sm", f)], w=["da_ctx%d" % f])
                        for cc in range(2):
                            P.op("pe", lambda e: e.transpose(out=pT[:, (f * 2 + cc) * 128:(f * 2 + cc + 1) * 128], in_=ctx[f][:, cc * 128:(cc + 1) * 128],
                                                             identity=self.ident[:]), r=["da_ctx%d" % f, "ident"], w=["da_pT"])
                        self.copy("dve", ctT[f][:], pT[:, f * 256:(f + 1) * 256], ["da_pT"], ["da_ctT%d" % f])
                        for cc in range(2):
                            P.op("pe", lambda e: e.matmul(pM[:, 0:128], lhsT=ctT[f][:, cc * 128:(cc + 1) * 128], rhs=wuv[:, h, cc, :], start=(cc == 0), stop=(cc == 1)),
                                 r=["da_ctT%d" % f, "da_wuv"], w=["da_pm"])
                        self.copy("act", og[f][:], pM[:, 0:128], ["da_pm"], ["da_og%d" % f])
                        P.dma(mixed_ap[qb * 128:(qb + 1) * 128, 2048 + h * 128:2048 + (h + 1) * 128], og[f][:], r=["da_og%d" % f], w=["da_out"], q="act")
        P.barrier()


class MK6(MK5):
    def final_norm(self, h_ap, gain_ap, out_ap):
        P = self.P
        with ExitStack() as st:
            gb = P.sb("fn_g", [128, DM], F32, st)
            P.dma(gb[:], gain_ap.partition_broadcast(128), w=["fn_g"])
            xt = [P.sb("fn_x%d" % i, [128, DM], F32, st) for i in range(2)]
            jk = P.sb("fn_j", [128, DM], BF16, st)
            ss = P.sb("fn_s", [128, 4], F32, st)
            for t in range(NT):
                b = t % 2
                P.dma(xt[b][:], h_ap[t * 128:(t + 1) * 128, :], w=["fn_x%d" % b])
                P.op("act", lambda e: e.activation(out=jk[:], in_=xt[b][:], func=AF.Square, accum_out=ss[:, b:b + 1]), r=["fn_x%d" % b], w=["fn_j", "fn_s%d" % b])
                self.rstd_from_ss(ss[:, 2 + b:3 + b], ss[:, b:b + 1], DM, "fn_r%d" % b, "fn_s%d" % b)
                P.op("dve", lambda e: e.scalar_tensor_tensor(out=xt[b][:], in0=xt[b][:], scalar=ss[:, 2 + b:3 + b], in1=gb[:], op0=ALU.mult, op1=ALU.mult),
                     r=["fn_x%d" % b, "fn_r%d" % b, "fn_g"], w=["fn_x%d" % b])
                P.dma(out_ap[t * 128:(t + 1) * 128, :], xt[b][:], r=["fn_x%d" % b], w=["fn_out"], q="act")
        P.barrier()


W_SHAPES = {
    "rel_table": [32, 8], "ab_w_in": [1, 2048, 6144], "ab_w_out": [1, 2048, 2048], "ab_lambda": [1, 4, 64],
    "ab_a_norm": [1, 128], "ab_b_norm": [1, 128], "cd_w_in": [1, 2048, 7536], "cd_w_out": [1, 3072, 2048],
    "cd_conv_w": [1, 4, 3072], "cd_conv_b": [1, 3072], "cd_dt_bias": [1, 32], "cd_a_log": [1, 32], "cd_d_skip": [1, 32],
    "cd_ssm_norm": [1, 2048], "cd_kv_norm": [1, 256], "cd_w_uk": [1, 8, 128, 256], "cd_w_uv": [1, 8, 256, 128],
    "norm_mix": [2, 2048], "norm_ffn": [2, 2048], "norm_final": [1, 2048],
    "peer_w_q0": [2048, 2048], "peer_w_q1": [2048, 2048], "peer_keys0": [8, 2, 128, 128], "peer_keys1": [8, 2, 128, 128],
    "peer_u0": [16384, 2048], "peer_u1": [16384, 2048], "peer_v0": [16384, 2048], "peer_v1": [16384, 2048],
}


def build_full(dbg=(), upto=99):
    mk = MK6(dbg=dbg)
    P = mk.P
    w = {}
    for k, shp in W_SHAPES.items():
        w[k] = mk.inp(k, shp)
    consts = host_consts()
    consts.update(host_consts1())
    for k, v in consts.items():
        w[k] = mk.inp(k, list(v.shape))
    x = mk.inp("x", [S_LEN, DM])
    out = P.dram("out", [S_LEN, DM], F32, kind="ExternalOutput")
    hA = mk.scratch("hA", [S_LEN, DM], F32)
    hB = mk.scratch("hB", [S_LEN, DM], F32)
    hC = mk.scratch("hC", [S_LEN, DM], F32)
    hD = mk.scratch("hD", [S_LEN, DM], F32)
    mixed1 = mk.scratch("mixed1", [S_LEN, 3072], BF16)
    mk.setup_consts(w["c_ident"])
    mk.build_bias_tables(w["rel_table"], w["c_boh"], w["c_bmask"])
    mk.lam_scalar(w["ab_lambda"])
    mk.l1_consts(w)
    mk.layer0_mixer(x.ap(), hA.ap(), w)
    mk.peer(hA.ap(), hB.ap(), w["norm_ffn"].ap()[0:1, :], w["peer_w_q0"].ap(), w["peer_keys0"].ap(), w["peer_u0"].ap(), w["peer_v0"].ap(), 0)
    mk.layer1_proj(hB.ap(), w)
    mk.mamba_ssd(w, mixed1.ap())
    mk.dsa_select()
    mk.dsa_attend(w, mixed1.ap())
    mk.out_proj(mixed1.ap(), 3072, w["cd_w_out"].ap()[0], hB.ap(), hC.ap(), BF16)
    mk.peer(hC.ap(), hD.ap(), w["norm_ffn"].ap()[1:2, :], w["peer_w_q1"].ap(), w["peer_keys1"].ap(), w["peer_u1"].ap(), w["peer_v1"].ap(), 1)
    mk.final_norm(hD.ap(), w["norm_final"].ap(), out.ap())
    P.finish()
    return mk, consts


def make_in_maps(inputs, consts, n_cores=8):
    shared = {}
    peer_src = {"peer_w_q": inputs["peer_w_q"], "peer_keys": inputs["peer_keys"], "peer_u": inputs["peer_u"], "peer_v": inputs["peer_v"]}
    for k, shp in W_SHAPES.items():
        if k.startswith("peer_"):
            base, l = k[:-1], int(k[-1])
            shared[k] = np.ascontiguousarray(np.asarray(peer_src[base])[l])
        else:
            shared[k] = np.ascontiguousarray(np.asarray(inputs[k], dtype=np.float32).reshape(shp))
    shared.update(consts)
    xs = np.asarray(inputs["x"], dtype=np.float32)
    maps = []
    for c in range(n_cores):
        m = dict(shared)
        m["x"] = np.ascontiguousarray(xs[c])
        maps.append(m)
    return maps


_CACHE = {}


def kernel(**inputs):
    from concourse.bass_utils import run_bass_kernel_spmd
    if "prog" not in _CACHE:
        _CACHE["prog"] = build_full()
    mk, consts = _CACHE["prog"]
    maps = make_in_maps(inputs, consts, 8)
    res = run_bass_kernel_spmd(mk.P.nc, maps, core_ids=list(range(8)))
    return np.stack([np.asarray(r["out"], dtype=np.float32) for r in res.results], axis=0)
```
